# Optimizing a Trainium2 kernel written in Bass

```python
import jax, jax.numpy as jnp
from jax import lax
import numpy as np

D_MODEL = 1024
BATCH = 16
SEQ = 2048
DEPTH = 4

N_MIXERS = 3
HEAD_DIM = 64
N_HEADS = D_MODEL // HEAD_DIM
MOBA_BLOCK = 256
MOBA_TOPK = 3
MOBA_QCHUNK = 64
SWA_WINDOW = 128
SWA_KV_HEADS = 4
SB_QBLOCK = 128
N_GROUPS = 4
EXPERTS_PER_GROUP = 8
N_EXPERTS = N_GROUPS * EXPERTS_PER_GROUP
TOPK_IN_GROUP = 2
D_EXPERT = 512
MOE_BLOCK = 256
PLE_DIM = 256
DEEPNORM_ALPHA = (2.0 * DEPTH) ** 0.25
DEEPNORM_BETA = (8.0 * DEPTH) ** -0.25
LN_EPS = 1e-5

kernel_name = 'hybrid_moba_swa_stickbreak_hmoe_deepnorm'


def _n_layers_of(mixer):
    return len(range(mixer, DEPTH, N_MIXERS))


def _alibi_slopes(n_heads):
    return jnp.asarray(np.array([2.0 ** (-8.0 * (h + 1) / n_heads) for h in range(n_heads)], dtype=np.float32))


def layer_norm(x, g, b):
    xf = x.astype(jnp.float32)
    mu = xf.mean(-1, keepdims=True)
    var = jnp.square(xf - mu).mean(-1, keepdims=True)
    y = (xf - mu) * lax.rsqrt(var + LN_EPS) * g.astype(jnp.float32) + b.astype(jnp.float32)
    return y.astype(x.dtype)


def moba_attention(q, k, v):
    bsz, seq, n_h, hd = q.shape
    s_pad = -(-seq // MOBA_BLOCK) * MOBA_BLOCK
    padw = ((0, 0), (0, s_pad - seq), (0, 0), (0, 0))
    qh = jnp.pad(q, padw).transpose(0, 2, 1, 3)
    kb = jnp.pad(k, padw).transpose(0, 2, 1, 3).reshape(bsz, n_h, -1, MOBA_BLOCK, hd)
    vb = jnp.pad(v, padw).transpose(0, 2, 1, 3).reshape(bsz, n_h, -1, MOBA_BLOCK, hd)
    n_blk = s_pad // MOBA_BLOCK
    k_sel = min(MOBA_TOPK, n_blk)
    scale = hd ** -0.5
    slopes = _alibi_slopes(n_h)
    q_blk = jnp.arange(s_pad) // MOBA_BLOCK
    k_mean = kb.astype(jnp.float32).mean(axis=3)
    gate = jnp.einsum('bhqd,bhnd->bhqn', qh.astype(jnp.float32), k_mean)
    gate = jnp.where(jnp.arange(n_blk)[None, :] < q_blk[:, None], gate, -jnp.inf)
    _, sel = lax.top_k(gate, k_sel)
    sel_valid = sel < q_blk[None, None, :, None]
    n_chunk = s_pad // MOBA_QCHUNK
    h_idx = jnp.arange(n_h)[:, None, None]
    blk_off = jnp.arange(MOBA_BLOCK)

    def chunk(idx):
        b = idx // n_chunk
        q0 = (idx % n_chunk) * MOBA_QCHUNK
        own = q0 // MOBA_BLOCK
        qc = lax.dynamic_slice_in_dim(qh[b], q0, MOBA_QCHUNK, axis=1)
        sel_c = lax.dynamic_slice_in_dim(sel[b], q0, MOBA_QCHUNK, axis=1)
        val_c = lax.dynamic_slice_in_dim(sel_valid[b], q0, MOBA_QCHUNK, axis=1)
        t_pos = q0 + jnp.arange(MOBA_QCHUNK)
        k_own = kb[b, :, own]
        v_own = vb[b, :, own]
        d_own = t_pos[:, None] - (own * MOBA_BLOCK + blk_off)[None, :]
        l_own = jnp.einsum('hqd,hkd->hqk', qc, k_own, preferred_element_type=jnp.float32) * scale - slopes[:, None, None] * d_own
        l_own = jnp.where(d_own >= 0, l_own, -jnp.inf)
        k_g = kb[b, h_idx, sel_c]
        v_g = vb[b, h_idx, sel_c]
        d_sel = t_pos[None, :, None, None] - (sel_c[..., None] * MOBA_BLOCK + blk_off)
        l_sel = jnp.einsum('hqd,hqnkd->hqnk', qc, k_g, preferred_element_type=jnp.float32) * scale - slopes[:, None, None, None] * d_sel
        l_sel = jnp.where(val_c[..., None], l_sel, -jnp.inf)
        logits = jnp.concatenate([l_own, l_sel.reshape(n_h, MOBA_QCHUNK, k_sel * MOBA_BLOCK)], axis=-1)
        w = jax.nn.softmax(logits, axis=-1).astype(v.dtype)
        w_sel = w[..., MOBA_BLOCK:].reshape(n_h, MOBA_QCHUNK, k_sel, MOBA_BLOCK)
        return jnp.einsum('hqk,hkd->hqd', w[..., :MOBA_BLOCK], v_own) + jnp.einsum('hqnk,hqnkd->hqd', w_sel, v_g)

    out = lax.map(chunk, jnp.arange(bsz * n_chunk))
    out = out.reshape(bsz, n_chunk, n_h, MOBA_QCHUNK, hd).transpose(0, 1, 3, 2, 4).reshape(bsz, s_pad, n_h, hd)
    return out[:, :seq]


def swa_gqa_attention(q, k, v, sinks):
    bsz, seq, n_h, hd = q.shape
    n_kv = k.shape[2]
    grp = n_h // n_kv
    win = SWA_WINDOW
    n_b = seq // win
    qb = q.reshape(bsz, n_b, win, n_kv, grp, hd)
    kb = k.reshape(bsz, n_b, win, n_kv, hd)
    vb = v.reshape(bsz, n_b, win, n_kv, hd)
    shift = ((0, 0), (1, 0), (0, 0), (0, 0), (0, 0))
    k_band = jnp.concatenate([jnp.pad(kb, shift)[:, :-1], kb], axis=2)
    v_band = jnp.concatenate([jnp.pad(vb, shift)[:, :-1], vb], axis=2)
    logits = jnp.einsum('bnqkgd,bnskd->bkgnqs', qb, k_band, preferred_element_type=jnp.float32) * (hd ** -0.5)
    dist = (jnp.arange(win)[:, None] + win) - jnp.arange(2 * win)[None, :]
    first = (jnp.arange(n_b) == 0)[:, None, None] & (jnp.arange(2 * win) < win)[None, None, :]
    allowed = ((dist >= 0) & (dist < win))[None] & ~first
    slopes = _alibi_slopes(n_h).reshape(1, n_kv, grp, 1, 1, 1)
    logits = jnp.where(allowed, logits - slopes * dist, -jnp.inf)
    sink = jnp.broadcast_to(sinks.astype(jnp.float32).reshape(1, n_kv, grp, 1, 1, 1), logits.shape[:-1] + (1,))
    w = jax.nn.softmax(jnp.concatenate([logits, sink], axis=-1), axis=-1)[..., :-1]
    o = jnp.einsum('bkgnqs,bnskd->bnqkgd', w.astype(v.dtype), v_band)
    return o.reshape(bsz, seq, n_h, hd)


def stick_breaking_attention(q, k, v):
    bsz, seq, n_h, hd = q.shape
    n_b = seq // SB_QBLOCK
    qh = q.transpose(0, 2, 1, 3)
    kh = k.transpose(0, 2, 1, 3)
    vh = v.transpose(0, 2, 1, 3)
    s_pos = jnp.arange(seq)
    scale = hd ** -0.5

    def block(c):
        q0 = c * SB_QBLOCK
        qc = lax.dynamic_slice_in_dim(qh, q0, SB_QBLOCK, axis=2)
        z = jnp.einsum('bhqd,bhsd->bhqs', qc, kh, preferred_element_type=jnp.float32) * scale
        strict = s_pos[None, :] < (q0 + jnp.arange(SB_QBLOCK))[:, None]
        log_keep = jnp.where(strict, jax.nn.log_sigmoid(-z), 0.0)
        after = lax.cumsum(log_keep, axis=3, reverse=True) - log_keep
        a = jnp.where(strict, jnp.exp(jax.nn.log_sigmoid(z) + after), 0.0)
        return jnp.einsum('bhqs,bhsd->bhqd', a.astype(vh.dtype), vh)

    out = lax.map(block, jnp.arange(n_b))
    return out.transpose(1, 0, 3, 2, 4).reshape(bsz, seq, n_h, hd)


def hierarchical_moe(x, w_grp, b_grp, w_exp, b_exp, w_e_gate, w_e_up, w_e_down):
    bsz, seq, d = x.shape
    xt = x.reshape(-1, d)
    n_tok = xt.shape[0]
    grp_prob = jax.nn.softmax((xt @ w_grp).astype(jnp.float32) + b_grp.astype(jnp.float32), axis=-1)
    g_p, g_idx = lax.top_k(grp_prob, 1)
    e_logits = ((xt @ w_exp).astype(jnp.float32) + b_exp.astype(jnp.float32)).reshape(n_tok, N_GROUPS, EXPERTS_PER_GROUP)
    in_grp = jnp.take_along_axis(e_logits, g_idx[:, :, None], axis=1)[:, 0]
    e_p, e_loc = lax.top_k(jax.nn.softmax(in_grp, axis=-1), TOPK_IN_GROUP)
    gate = g_p * (e_p / e_p.sum(-1, keepdims=True))
    e_glob = g_idx * EXPERTS_PER_GROUP + e_loc
    n_asg = n_tok * TOPK_IN_GROUP
    flat_e = e_glob.reshape(-1)
    flat_w = gate.reshape(-1)
    flat_tok = jnp.repeat(jnp.arange(n_tok), TOPK_IN_GROUP)
    order = jnp.argsort(flat_e)
    se = flat_e[order]
    counts = jnp.bincount(flat_e, length=N_EXPERTS)
    start = jnp.cumsum(counts) - counts
    pcounts = (counts + MOE_BLOCK - 1) // MOE_BLOCK * MOE_BLOCK
    pend = jnp.cumsum(pcounts)
    dest = (pend - pcounts)[se] + jnp.arange(n_asg) - start[se]
    n_pad = -(-n_asg // MOE_BLOCK) * MOE_BLOCK + N_EXPERTS * MOE_BLOCK
    n_blocks = n_pad // MOE_BLOCK
    tok_pad = jnp.zeros((n_pad,), jnp.int32).at[dest].set(flat_tok[order])
    w_pad = jnp.zeros((n_pad,), jnp.float32).at[dest].set(flat_w[order])
    block_e = jnp.minimum(jnp.searchsorted(pend, jnp.arange(n_blocks) * MOE_BLOCK, side='right'), N_EXPERTS - 1)

    def expert_block(args):
        xb, e = args
        h = jax.nn.silu(xb @ w_e_gate[e]) * (xb @ w_e_up[e])
        return h @ w_e_down[e]

    y = lax.map(expert_block, (xt[tok_pad].reshape(n_blocks, MOE_BLOCK, d), block_e))
    y = y.reshape(n_pad, d) * w_pad[:, None].astype(y.dtype)
    out = jnp.zeros_like(xt).at[tok_pad].add(y)
    return out.reshape(bsz, seq, d)


def setup_inputs(seed: int = 0) -> dict:
    key = jax.random.key(seed)
    ks = jax.random.split(key, 32)
    f32 = jnp.float32
    d = D_MODEL
    n_a, n_b, n_c = _n_layers_of(0), _n_layers_of(1), _n_layers_of(2)
    kv_w = SWA_KV_HEADS * HEAD_DIM

    def nrm(k, shape, scale):
        return jax.random.normal(k, shape, f32) * scale

    return {
        'x': nrm(ks[0], (BATCH, SEQ, d), 1.0),
        'p': nrm(ks[1], (DEPTH, BATCH, SEQ, PLE_DIM), 1.0),
        'w_qkv_a': nrm(ks[2], (n_a, d, 3 * d), d ** -0.5),
        'w_o_a': nrm(ks[3], (n_a, d, d), d ** -0.5 * DEEPNORM_BETA),
        'w_qkv_b': nrm(ks[4], (n_b, d, d + 2 * kv_w), d ** -0.5),
        'w_o_b': nrm(ks[5], (n_b, d, d), d ** -0.5 * DEEPNORM_BETA),
        'sinks_b': nrm(ks[6], (n_b, N_HEADS), 0.5),
        'w_qkv_c': nrm(ks[7], (n_c, d, 3 * d), d ** -0.5),
        'w_o_c': nrm(ks[8], (n_c, d, d), d ** -0.5 * DEEPNORM_BETA),
        'ln1_g': 1.0 + nrm(ks[9], (DEPTH, d), 0.02),
        'ln1_b': nrm(ks[10], (DEPTH, d), 0.02),
        'ln2_g': 1.0 + nrm(ks[11], (DEPTH, d), 0.02),
        'ln2_b': nrm(ks[12], (DEPTH, d), 0.02),
        'w_grp': nrm(ks[13], (DEPTH, d, N_GROUPS), d ** -0.5),
        'b_grp': nrm(ks[14], (DEPTH, N_GROUPS), 0.01),
        'w_exp': nrm(ks[15], (DEPTH, d, N_EXPERTS), d ** -0.5),
        'b_exp': nrm(ks[16], (DEPTH, N_EXPERTS), 0.01),
        'w_e_gate': nrm(ks[17], (DEPTH, N_EXPERTS, d, D_EXPERT), d ** -0.5),
        'w_e_up': nrm(ks[18], (DEPTH, N_EXPERTS, d, D_EXPERT), d ** -0.5),
        'w_e_down': nrm(ks[19], (DEPTH, N_EXPERTS, D_EXPERT, d), D_EXPERT ** -0.5 * DEEPNORM_BETA),
        'w_ple_gate': nrm(ks[20], (DEPTH, d, d), d ** -0.5),
        'w_ple_proj': nrm(ks[21], (DEPTH, PLE_DIM, d), PLE_DIM ** -0.5),
    }


def reference(x, p, w_qkv_a, w_o_a, w_qkv_b, w_o_b, sinks_b, w_qkv_c, w_o_c,
              ln1_g, ln1_b, ln2_g, ln2_b, w_grp, b_grp, w_exp, b_exp,
              w_e_gate, w_e_up, w_e_down, w_ple_gate, w_ple_proj):
    bsz, seq, d = x.shape
    kv_w = SWA_KV_HEADS * HEAD_DIM
    for i in range(DEPTH):
        mixer, j = i % N_MIXERS, i // N_MIXERS
        if mixer == 0:
            q, k, v = jnp.split(x @ w_qkv_a[j], 3, axis=-1)
            o = moba_attention(q.reshape(bsz, seq, N_HEADS, HEAD_DIM), k.reshape(bsz, seq, N_HEADS, HEAD_DIM),
                               v.reshape(bsz, seq, N_HEADS, HEAD_DIM))
            h = o.reshape(bsz, seq, d) @ w_o_a[j]
        elif mixer == 1:
            qkv = x @ w_qkv_b[j]
            q = qkv[..., :d].reshape(bsz, seq, N_HEADS, HEAD_DIM)
            k = qkv[..., d:d + kv_w].reshape(bsz, seq, SWA_KV_HEADS, HEAD_DIM)
            v = qkv[..., d + kv_w:].reshape(bsz, seq, SWA_KV_HEADS, HEAD_DIM)
            h = swa_gqa_attention(q, k, v, sinks_b[j]).reshape(bsz, seq, d) @ w_o_b[j]
        else:
            q, k, v = jnp.split(x @ w_qkv_c[j], 3, axis=-1)
            o = stick_breaking_attention(q.reshape(bsz, seq, N_HEADS, HEAD_DIM), k.reshape(bsz, seq, N_HEADS, HEAD_DIM),
                                         v.reshape(bsz, seq, N_HEADS, HEAD_DIM))
            h = o.reshape(bsz, seq, d) @ w_o_c[j]
        x = layer_norm(DEEPNORM_ALPHA * x + h, ln1_g[i], ln1_b[i])
        m = hierarchical_moe(x, w_grp[i], b_grp[i], w_exp[i], b_exp[i], w_e_gate[i], w_e_up[i], w_e_down[i])
        x = layer_norm(DEEPNORM_ALPHA * x + m, ln2_g[i], ln2_b[i])
        ple_gate = jax.nn.sigmoid((x @ w_ple_gate[i]).astype(jnp.float32)).astype(x.dtype)
        x = x + ple_gate * (p[i] @ w_ple_proj[i])
    return x
```

```python
import numpy as np
import concourse.bass as bass
import concourse.mybir as mybir
from concourse.bass_utils import run_bass_kernel_spmd

F32 = mybir.dt.float32
F32R = mybir.dt.float32r
I32 = mybir.dt.int32
ALU = mybir.AluOpType
AF = mybir.ActivationFunctionType
AX = mybir.AxisListType

USE_F32R = True
MMDT = F32R if USE_F32R else F32

NCORES = 8
D = 1024
SEQ = 2048
NSEQ = 2
T = NSEQ * SEQ
DEPTH = 4
NH = 16
HD = 64
NEXP = 32
DEXP = 512
CAP = 384
PLE = 256
ALPHA = (2.0 * DEPTH) ** 0.25
EPS = 1e-5
BIG = 30000.0
SLOPES = [2.0 ** (-8.0 * (h + 1) / NH) for h in range(NH)]
XS_ROWS = NEXP * CAP + 128


class Eng:
    def __init__(self, name, h, sem):
        self.name, self.h, self.sem = name, h, sem
        self.count = 0
        self.known = {}


class Buf:
    __slots__ = ("name", "writers", "readers")

    def __init__(self, name):
        self.name = name
        self.writers = {}
        self.readers = {}


class Ctx:
    def __init__(self, nc, es):
        self.nc = nc
        self.es = es
        mk = lambda n, h: Eng(n, h, es.enter_context(nc.semaphore("s_" + n)))
        self.pe = mk("pe", nc.tensor)
        self.act = mk("act", nc.scalar)
        self.dve = mk("dve", nc.vector)
        self.pool = mk("pool", nc.gpsimd)
        self.sp = mk("sp", nc.sync)
        self.engs = [self.pe, self.act, self.dve, self.pool, self.sp]
        self.sems = {e.name: e.sem for e in self.engs}
        self.ndma = 40
        self.dsem = []
        for i in range(self.ndma):
            s = es.enter_context(nc.semaphore("s_dma%d" % i))
            self.dsem.append(s)
            self.sems["d%d" % i] = s
        self.dval = [0] * self.ndma
        self.dnext = 0
        self.FW, self.RW = 17408, 34816
        self.arena = es.enter_context(nc.sbuf_tensor("arena_f", [128, self.FW], F32))
        self.arena_r = es.enter_context(nc.sbuf_tensor("arena_r", [128, self.RW], MMDT))
        self.atop = 0
        self.rtop = 0
        self.psum = []
        for i in range(8):
            t = es.enter_context(nc.psum_tensor("psb%d" % i, [128, 512], F32))
            self.psum.append((t, Buf("ps%d" % i)))
        self.nbuf = 0

    def alloc(self, ncols, dt=F32, name=None):
        if dt == MMDT and USE_F32R:
            a = self.arena_r[:, self.rtop:self.rtop + ncols]
            self.rtop += ncols
            assert self.rtop <= self.RW, "SBUF R arena overflow %d" % self.rtop
        else:
            a = self.arena[:, self.atop:self.atop + ncols]
            self.atop += ncols
            assert self.atop <= self.FW, "SBUF F arena overflow %d" % self.atop
            if dt != F32:
                a = a.bitcast(dt)
        self.nbuf += 1
        return a, Buf(name or ("b%d" % self.nbuf))

    def wait(self, E, key, val):
        if E.known.get(key, 0) >= val:
            return
        E.h.wait_ge(self.sems[key], val)
        E.known[key] = val

    def _deps(self, E, reads, writes):
        evs = {}
        for b in reads:
            for k, v in b.writers.items():
                evs[k] = max(evs.get(k, 0), v)
        for b in writes:
            for k, v in b.readers.items():
                if k != E.name:
                    evs[k] = max(evs.get(k, 0), v)
            for k, v in b.writers.items():
                if k != E.name:
                    evs[k] = max(evs.get(k, 0), v)
        for k, v in evs.items():
            self.wait(E, k, v)

    def _record(self, key, val, reads, writes):
        for b in reads:
            b.readers[key] = max(b.readers.get(key, 0), val)
        for b in writes:
            if b.readers:
                b.readers = {}
                b.writers = {key: val}
            else:
                b.writers[key] = max(b.writers.get(key, 0), val)

    def op(self, E, fname, *args, reads=(), writes=(), signal=True, **kw):
        self._deps(E, reads, writes)
        ins = getattr(E.h, fname)(*args, **kw)
        if signal:
            E.count += 1
            ins.then_inc(E.sem, 1)
            self._record(E.name, E.count, reads, writes)
        else:
            self._record(E.name, E.count + 1, reads, writes)
        return ins

    def dma(self, out, in_, reads=(), writes=(), q=None, indirect=None, **kw):
        Q = q or self.sp
        self._deps(Q, reads, writes)
        i = self.dnext
        self.dnext = (self.dnext + 1) % self.ndma
        key = "d%d" % i
        self.wait(Q, key, self.dval[i])
        if indirect is None:
            ins = Q.h.dma_start(out=out, in_=in_, **kw)
        else:
            ins = Q.h.indirect_dma_start(out=out, in_=in_, **indirect)
        self.dval[i] += 16
        ins.then_inc(self.dsem[i], 16)
        self._record(key, self.dval[i], reads, writes)

    def barrier(self):
        for E in self.engs:
            for F in self.engs:
                if F is not E and F.count > 0:
                    self.wait(E, F.name, F.count)
            for i in range(self.ndma):
                if self.dval[i] > 0:
                    self.wait(E, "d%d" % i, self.dval[i])

    def phase_reset(self, keep):
        self.barrier()
        self.atop, self.rtop = keep

    def mm(self, out, outbuf, parts, start=True, stop=True):
        n = len(parts)
        for i, (l, r, rb) in enumerate(parts):
            last = (i == n - 1)
            self.op(self.pe, "matmul", out, l, r, start=(start and i == 0), stop=(stop and last),
                    reads=rb, writes=[outbuf], signal=last)

    def transpose(self, out, outbuf, in_, inbufs, ident, signal=True):
        self.op(self.pe, "transpose", out, in_, ident, reads=inbufs, writes=[outbuf], signal=signal)


def build(nlayers=DEPTH, debug=None):
    from contextlib import ExitStack
    nc = bass.Bass("TRN2", target_bir_lowering=False)
    dr = lambda n, s, kind="ExternalInput", dt=F32: nc.dram_tensor(n, list(s), dt, kind=kind).ap()
    x_in = dr("x", [T, D])
    p_in = dr("p", [DEPTH, T, PLE])
    w_qkv_a = dr("w_qkv_a", [2, D, 3 * D]); w_o_a = dr("w_o_a", [2, D, D])
    w_qkv_b = dr("w_qkv_b", [1, D, D + 512]); w_o_b = dr("w_o_b", [1, D, D])
    sinks_b = dr("sinks_b", [1, NH])
    w_qkv_c = dr("w_qkv_c", [1, D, 3 * D]); w_o_c = dr("w_o_c", [1, D, D])
    ln1_g = dr("ln1_g", [DEPTH, D]); ln1_b = dr("ln1_b", [DEPTH, D])
    ln2_g = dr("ln2_g", [DEPTH, D]); ln2_b = dr("ln2_b", [DEPTH, D])
    w_rt = dr("w_rt", [DEPTH, D, 36]); b_rt = dr("b_rt", [DEPTH, 36])
    w_e_gate = dr("w_e_gate", [DEPTH, NEXP, D, DEXP]); w_e_up = dr("w_e_up", [DEPTH, NEXP, D, DEXP])
    w_e_down = dr("w_e_down", [DEPTH, NEXP, DEXP, D])
    w_ple_gate = dr("w_ple_gate", [DEPTH, D, D]); w_ple_proj = dr("w_ple_proj", [DEPTH, PLE, D])
    cst = dr("cst", [128, 6, 128])
    cmask = dr("cmask", [9, 128, 512])
    cbase = dr("cbase", [128, 512])
    ceoff = dr("ceoff", [128, NEXP])
    out = dr("out", [T, D], kind="ExternalOutput")
    X = dr("Xs", [T, D], kind="Internal"); X1 = dr("X1s", [T, D], kind="Internal")
    QT = dr("QTs", [D, T], kind="Internal"); KT = dr("KTs", [D, T], kind="Internal")
    V = dr("Vs", [T, D], kind="Internal"); OT = dr("OTs", [D, T], kind="Internal")
    XS = dr("XSs", [XS_ROWS, D], kind="Internal"); YS = dr("YSs", [XS_ROWS, D], kind="Internal")
    dbg = {}
    if debug:
        for nme, shp in debug.items():
            dbg[nme] = dr("dbg_" + nme, shp, kind="ExternalOutput")
    dX, dX1, dQT, dKT, dV, dOT, dXS, dYS = [Buf(n) for n in ("X", "X1", "QT", "KT", "V", "OT", "XS", "YS")]
    dIN = Buf("in")

    with ExitStack() as es:
        c = Ctx(nc, es)
        pe, act, dve, pool, sp = c.pe, c.act, c.dve, c.pool, c.sp
        wq = pool if USE_F32R else sp
        PS = c.psum
        bc_reg = nc.gpsimd.to_reg(XS_ROWS - 1)

        ident, b_const = c.alloc(128)
        ones_r, _ = c.alloc(128, MMDT)
        tri_f, _ = c.alloc(128)
        ustr_r, _ = c.alloc(128, MMDT)
        ones_f, _ = c.alloc(128)
        cb_f, _ = c.alloc(512)
        eoff, _ = c.alloc(NEXP)
        rt, b_rtab = c.alloc(32 * 4)
        ridx, b_ridx = c.alloc(32 * 2, I32)
        cnt_b, b_cnt = c.alloc(NEXP)
        c.dma(ident, cst[:, 0, :], writes=[b_const])
        c.dma(ones_f, cst[:, 1, :], writes=[b_const])
        c.dma(tri_f, cst[:, 2, :], writes=[b_const])
        c.dma(ones_r, cst[:, 1, :], writes=[b_const], q=wq)
        c.dma(ustr_r, cst[:, 3, :], writes=[b_const], q=wq)
        c.dma(cb_f, cbase, writes=[b_const])
        c.dma(eoff, ceoff, writes=[b_const])
        KEEP = (c.atop, c.rtop)
        rt3 = rt.rearrange("p (t k) -> p t k", k=4)
        ridx3 = ridx.rearrange("p (t k) -> p t k", k=2)

        for i in range(8):
            c.dma(X[i * 512:(i + 1) * 512, :], x_in[i * 512:(i + 1) * 512, :], reads=[dIN], writes=[dX])

        state = {"ev": 0}

        def evac(out_ap, outb, in_ap, inb, extra_reads=()):
            state["ev"] ^= 1
            if state["ev"]:
                c.op(act, "copy", out_ap, in_ap, reads=[inb, *extra_reads], writes=[outb])
            else:
                c.op(dve, "tensor_copy", out_ap, in_ap, reads=[inb, *extra_reads], writes=[outb])

        def layer_norm(y, yb, gB, bB, gbuf, outt, outb, sc):
            st, stb = sc["st"]
            mv, mvb = sc["mv"]
            c.op(dve, "bn_stats", st[:, 0:6], y[:, 0:512], reads=[yb], writes=[stb])
            c.op(dve, "bn_stats", st[:, 6:12], y[:, 512:1024], reads=[yb], writes=[stb])
            c.op(dve, "bn_aggr", mv[:, 0:2], st[:, 0:12], reads=[stb], writes=[mvb])
            c.op(dve, "tensor_scalar", mv[:, 2:3], mv[:, 1:2], EPS, None, op0=ALU.add, reads=[mvb], writes=[mvb])
            c.op(act, "activation", mv[:, 2:3], mv[:, 2:3], AF.Sqrt, reads=[mvb], writes=[mvb])
            c.op(dve, "reciprocal", mv[:, 2:3], mv[:, 2:3], reads=[mvb], writes=[mvb])
            c.op(dve, "scalar_tensor_tensor", mv[:, 3:4], mv[:, 0:1], -1.0, mv[:, 2:3], op0=ALU.mult, op1=ALU.mult,
                 reads=[mvb], writes=[mvb])
            c.op(act, "activation", y, y, AF.Identity, bias=mv[:, 3:4], scale=mv[:, 2:3], reads=[yb, mvb], writes=[yb])
            c.op(dve, "tensor_tensor", y, y, gB, op=ALU.mult, reads=[yb, gbuf], writes=[yb])
            c.op(pool, "tensor_tensor", outt, y, bB, op=ALU.add, reads=[yb, gbuf], writes=[outb])

        for L in range(nlayers):
            mixer, j = L % 3, L // 3
            if mixer == 0:
                Wqkv, Wo, NQ, NKV = w_qkv_a[j], w_o_a[j], 1024, 1024
            elif mixer == 1:
                Wqkv, Wo, NQ, NKV = w_qkv_b[j], w_o_b[j], 1024, 256
            else:
                Wqkv, Wo, NQ, NKV = w_qkv_c[j], w_o_c[j], 1024, 1024
            NQKV = NQ + 2 * NKV

            c.phase_reset(KEEP)
            Wt, Wb = c.alloc(8 * NQKV, MMDT)
            W3 = Wt.rearrange("p (c n) -> p c n", n=NQKV)
            for cc in range(8):
                for n0 in range(0, NQKV, 512):
                    c.dma(W3[:, cc, n0:n0 + 512], Wqkv[cc * 128:(cc + 1) * 128, n0:n0 + 512], reads=[dIN], writes=[Wb], q=wq)
            xr_ring = [c.alloc(4 * 1024) for _ in range(2)]
            xT_ring = [c.alloc(8 * 512, MMDT) for _ in range(2)]
            stg_ring = [c.alloc(512) for _ in range(4)]
            sti = 0
            pso = 0
            for tb in range(T // 512):
                xr, xrb = xr_ring[tb % 2]
                xT, xTb = xT_ring[tb % 2]
                xr3 = xr.rearrange("p (j d) -> p j d", d=1024)
                xT3 = xT.rearrange("p (c t) -> p c t", t=512)
                c.dma(xr3, X[tb * 512:(tb + 1) * 512, :].rearrange("(j p) d -> p j d", p=128), reads=[dX], writes=[xrb])
                for cc in range(8):
                    pt, pb = PS[cc % 2]
                    for jj in range(4):
                        c.transpose(pt[:, jj * 128:(jj + 1) * 128], pb, xr3[:, jj, cc * 128:(cc + 1) * 128], [xrb], ident,
                                    signal=(jj == 3))
                    evac(xT3[:, cc, :], xTb, pt[:, :], pb)
                for fc in range((NQ + NKV) // 128):
                    pt, pb = PS[2 + pso % 4]; pso += 1
                    c.mm(pt[:, :], pb, [(W3[:, cc, fc * 128:(fc + 1) * 128], xT3[:, cc, :], [Wb, xTb]) for cc in range(8)])
                    sg, sgb = stg_ring[sti % 4]; sti += 1
                    evac(sg, sgb, pt[:, :], pb)
                    if fc < NQ // 128:
                        c.dma(QT[fc * 128:(fc + 1) * 128, tb * 512:(tb + 1) * 512], sg, reads=[sgb], writes=[dQT])
                    else:
                        f2 = fc - NQ // 128
                        c.dma(KT[f2 * 128:(f2 + 1) * 128, tb * 512:(tb + 1) * 512], sg, reads=[sgb], writes=[dKT])
                for jj in range(4):
                    for v0 in range(0, NKV, 512):
                        nv = min(512, NKV - v0)
                        pt, pb = PS[2 + pso % 4]; pso += 1
                        c.mm(pt[:, 0:nv], pb, [(xT3[:, cc, jj * 128:(jj + 1) * 128], W3[:, cc, NQ + NKV + v0:NQ + NKV + v0 + nv], [Wb, xTb])
                                               for cc in range(8)])
                        sg, sgb = stg_ring[sti % 4]; sti += 1
                        evac(sg[:, 0:nv], sgb, pt[:, 0:nv], pb)
                        r0 = tb * 512 + jj * 128
                        c.dma(V[r0:r0 + 128, v0:v0 + nv], sg[:, 0:nv], reads=[sgb], writes=[dV])
            if debug and ("QT%d" % L) in dbg:
                c.dma(dbg["QT%d" % L], QT, reads=[dQT], writes=[dIN])
                c.dma(dbg["KT%d" % L], KT, reads=[dKT], writes=[dIN])
                c.dma(dbg["V%d" % L], V, reads=[dV], writes=[dIN])

            c.phase_reset(KEEP)
            ld_ring = []
            for _ in range(2):
                qt_, qb_ = c.alloc(SEQ, MMDT)
                kt_, kb_ = c.alloc(SEQ, MMDT)
                va_, vb_ = c.alloc(16 * 2 * 128, MMDT)
                ld_ring.append((qt_, qb_, kt_, kb_, va_, vb_))
                va4 = va_.rearrange("p (k h m) -> p k h m", h=2, m=128)
                c.op(dve, "tensor_copy", va_.rearrange("p (a b) -> p a b", b=128), ones_f.unsqueeze(1).broadcast_to([128, 32, 128]),
                     reads=[b_const], writes=[vb_])
            ot_ring = [c.alloc(SEQ, MMDT) for _ in range(2)]
            tmp_ring = [c.alloc(512) for _ in range(3)]
            P_ring = [c.alloc(512, MMDT) for _ in range(3)]
            bq_ring = [c.alloc(512) for _ in range(2)]
            rec_ring = [c.alloc(512) for _ in range(2)]
            nmask = 0
            if mixer == 0:
                masks = [c.alloc(512) for _ in range(4)]
                for i, (m, mb) in enumerate(masks):
                    c.dma(m, cmask[i], writes=[mb])
                selb_ring = [c.alloc(512) for _ in range(8)]
                km, kmb = c.alloc(8)
                kmr, kmrb = c.alloc(8, MMDT)
                kmrep, kmrepb = c.alloc(8 * 128, MMDT)
                kmrep3 = kmrep.rearrange("p (n m) -> p n m", m=128)
                gsb_ring = [c.alloc(8) for _ in range(2)]
                top_ring = [c.alloc(12) for _ in range(2)]
                diag_ring = [c.alloc(128, MMDT) for _ in range(2)]
                thr_ring = [c.alloc(512) for _ in range(2)]
            elif mixer == 1:
                masks = [c.alloc(512) for _ in range(5)]
                for i, (m, mb) in enumerate(masks):
                    c.dma(m, cmask[4 + i], writes=[mb])
                esk, eskb = c.alloc(NH)
                c.dma(esk, sinks_b[0:1, :].broadcast_to([128, NH]), reads=[dIN], writes=[eskb])
                c.op(act, "activation", esk, esk, AF.Exp, reads=[eskb], writes=[eskb])
            else:
                sp_ring = [c.alloc(512, MMDT) for _ in range(3)]
                e_ring = [c.alloc(512) for _ in range(2)]
                racc_ring = [c.alloc(512, MMDT) for _ in range(2)]
                t3_ring = [c.alloc(512) for _ in range(2)]
            cnt = {"tmp": 0, "P": 0, "S": 0, "acc": 0, "bq": 0, "rec": 0, "sel": 0, "g": 0, "sp": 0, "e": 0, "r": 0, "t3": 0}

            def nxt(ring, key):
                r = ring[cnt[key] % len(ring)]
                cnt[key] += 1
                return r

            it = 0
            for s in range(NSEQ):
                for hp in range(8):
                    qt_, qb_, kt_, kb_, va_, vb_ = ld_ring[it % 2]
                    ot_, ob_ = ot_ring[it % 2]
                    it += 1
                    va4 = va_.rearrange("p (k h m) -> p k h m", h=2, m=128)
                    ts0 = s * SEQ
                    c.dma(qt_, QT[hp * 128:(hp + 1) * 128, ts0:ts0 + SEQ], reads=[dQT], writes=[qb_], q=wq)
                    if mixer == 1:
                        g = hp // 2
                        for hh in range(2):
                            c.dma(kt_[hh * 64:(hh + 1) * 64, :], KT[g * 64:(g + 1) * 64, ts0:ts0 + SEQ], reads=[dKT], writes=[kb_], q=wq)
                            c.dma(va4[:, :, hh, 0:64], V[ts0:ts0 + SEQ, g * 64:(g + 1) * 64].rearrange("(k p) d -> p k d", p=128),
                                  reads=[dV], writes=[vb_], q=wq)
                    else:
                        c.dma(kt_, KT[hp * 128:(hp + 1) * 128, ts0:ts0 + SEQ], reads=[dKT], writes=[kb_], q=wq)
                        for hh in range(2):
                            f0 = hp * 128 + hh * 64
                            c.dma(va4[:, :, hh, 0:64], V[ts0:ts0 + SEQ, f0:f0 + 64].rearrange("(k p) d -> p k d", p=128),
                                  reads=[dV], writes=[vb_], q=wq)
                    if mixer == 0:
                        c.op(dve, "tensor_reduce", km, kt_.bitcast(F32).rearrange("p (n k) -> p n k", k=256), axis=AX.X, op=ALU.add,
                             reads=[kb_], writes=[kmb])
                        c.op(dve, "tensor_scalar", km, km, 1.0 / 256.0, None, op0=ALU.mult, reads=[kmb], writes=[kmb])
                        c.op(pool, "tensor_copy", kmr, km, reads=[kmb], writes=[kmrb])
                        for n in range(8):
                            c.op(pool, "tensor_scalar", kmrep3[:, n, :], ones_f, km[:, n:n + 1], None, op0=ALU.mult,
                                 reads=[kmb, b_const], writes=[kmrepb])
                    for hh in range(2):
                        h = hp * 2 + hh
                        hs = slice(hh * 64, (hh + 1) * 64)
                        slope = SLOPES[h]
                        if mixer in (0, 1):
                            bq, bqb = nxt(bq_ring, "bq")
                            c.op(pool, "tensor_scalar", bq, cb_f, slope, None, op0=ALU.mult, reads=[b_const], writes=[bqb])
                        for qc in range(4):
                            qs = slice(qc * 512, (qc + 1) * 512)
                            if mixer in (0, 1):
                                selb = {}
                                if mixer == 0 and qc >= 2:
                                    thr, thrb = nxt(thr_ring, "g")
                                    pth, pthb = PS[6]
                                    for jq in range(4):
                                        qtile = qc * 4 + jq
                                        blk = qtile // 2
                                        q128 = slice(qtile * 128, (qtile + 1) * 128)
                                        pg, pgb = PS[7]
                                        c.mm(pg[:, 0:8], pgb, [(qt_[hs, q128], kmr[hs, 0:8], [qb_, kmrb])])
                                        gs, gsb = nxt(gsb_ring, "sel")
                                        c.op(dve, "memset", gs, -BIG, writes=[gsb])
                                        c.op(dve, "tensor_copy", gs[:, 0:blk], pg[:, 0:blk], reads=[pgb], writes=[gsb])
                                        tp, tpb = nxt(top_ring, "e")
                                        c.op(dve, "max", tp[:, 0:8], gs, reads=[gsb], writes=[tpb])
                                        c.op(dve, "tensor_tensor", tp[:, 8:9], tp[:, 2:3], tp[:, 3:4], op=ALU.add, reads=[tpb], writes=[tpb])
                                        c.op(dve, "tensor_scalar", tp[:, 9:10], tp[:, 8:9], 0.5, None, op0=ALU.mult, reads=[tpb], writes=[tpb])
                                        dg, dgb = nxt(diag_ring, "r")
                                        c.op(dve, "tensor_scalar", dg, ident, tp[:, 9:10], None, op0=ALU.mult, reads=[tpb, b_const], writes=[dgb])
                                        c.mm(pth[:, jq * 128:(jq + 1) * 128], pthb, [(ones_r, dg, [dgb, b_const])])
                                    c.op(act, "copy", thr, pth[:, :], reads=[pthb], writes=[thrb])
                                    for n in range(2 * qc + 1):
                                        pg, pgb = PS[7]
                                        c.mm(pg[:, :], pgb, [(kmrep3[hs, n, :], qt_[hs, qs], [kmrepb, qb_])])
                                        sb_, sbb = nxt(selb_ring, "sp")
                                        c.op(dve, "tensor_tensor", sb_, pg[:, :], thr, op=ALU.is_ge, reads=[pgb, thrb], writes=[sbb])
                                        c.op(pool, "tensor_scalar", sb_, sb_, 1.0, BIG, op0=ALU.subtract, op1=ALU.mult, reads=[sbb], writes=[sbb])
                                        if n == 2 * qc:
                                            c.op(pool, "memset", sb_[:, 0:256], 0.0, writes=[sbb])
                                        selb[n] = (sb_, sbb)
                                if mixer == 0:
                                    kts = list(range(0, 4 * qc + 4))
                                else:
                                    kts = list(range(max(0, 4 * qc - 1), 4 * qc + 4))
                                acc, accb = PS[4 + cnt["acc"] % 2]; cnt["acc"] += 1
                                for ki, kt in enumerate(kts):
                                    ks = slice(kt * 128, (kt + 1) * 128)
                                    pS, pSb = PS[cnt["S"] % 4]; cnt["S"] += 1
                                    c.mm(pS[:, :], pSb, [(kt_[hs, ks], qt_[hs, qs], [kb_, qb_])])
                                    tm, tmb = nxt(tmp_ring, "tmp")
                                    c.op(dve, "scalar_tensor_tensor", tm, pS[:, :], 0.125, bq, op0=ALU.mult, op1=ALU.add,
                                         reads=[pSb, bqb], writes=[tmb])
                                    if mixer == 0:
                                        mi = kt - 4 * qc
                                        mk = masks[mi] if mi >= 0 else None
                                    else:
                                        mk = masks[kt - 4 * qc + 1]
                                    if mk is not None:
                                        c.op(pool, "tensor_tensor", tm, tm, mk[0], op=ALU.add, reads=[tmb, mk[1]], writes=[tmb])
                                    n = kt // 2
                                    if n in selb:
                                        c.op(pool, "tensor_tensor", tm, tm, selb[n][0], op=ALU.add, reads=[tmb, selb[n][1]], writes=[tmb])
                                    Pt, Pb = nxt(P_ring, "P")
                                    c.op(act, "activation", Pt, tm, AF.Exp, bias=float(-slope * (qc * 512 - kt * 128)), scale=1.0,
                                         reads=[tmb], writes=[Pb])
                                    c.mm(acc[:, :], accb, [(va4[:, kt, hh, :], Pt, [vb_, Pb])], start=(ki == 0), stop=(ki == len(kts) - 1))
                                rc, rcb = nxt(rec_ring, "rec")
                                if mixer == 1:
                                    c.op(dve, "tensor_scalar", rc[64:128, :], acc[64:128, :], esk[64:128, h:h + 1], None, op0=ALU.add,
                                         reads=[accb, eskb], writes=[rcb])
                                    c.op(dve, "reciprocal", rc[64:128, :], rc[64:128, :], reads=[rcb], writes=[rcb])
                                else:
                                    c.op(dve, "reciprocal", rc[64:128, :], acc[64:128, :], reads=[accb], writes=[rcb])
                                c.op(dve, "tensor_tensor", ot_[hs, qs], acc[0:64, :], rc[64:128, :], op=ALU.mult, reads=[accb, rcb], writes=[ob_])
                            else:
                                acc, accb = PS[4 + cnt["acc"] % 2]; cnt["acc"] += 1
                                kts = list(range(4 * qc + 3, -1, -1))
                                racc_prev = None
                                for ki, kt in enumerate(kts):
                                    ks = slice(kt * 128, (kt + 1) * 128)
                                    diag = kt >= 4 * qc
                                    base = qc * 512 - kt * 128
                                    pS, pSb = PS[cnt["S"] % 2]; cnt["S"] += 1
                                    c.mm(pS[:, :], pSb, [(kt_[hs, ks], qt_[hs, qs], [kb_, qb_])])
                                    et, etb = nxt(e_ring, "e")
                                    c.op(act, "activation", et, pS[:, :], AF.Exp, scale=0.125, reads=[pSb], writes=[etb])
                                    spt, spb = nxt(sp_ring, "sp")
                                    c.op(act, "activation", spt, et, AF.Ln, bias=1.0, scale=1.0, reads=[etb], writes=[spb])
                                    if diag:
                                        c.op(pool, "affine_select", spt, spt, pattern=[[1, 512]], compare_op=ALU.is_gt, fill=0.0,
                                             base=base, channel_multiplier=-1, reads=[spb], writes=[spb])
                                    pa, pab = PS[2 + cnt["r"] % 2]; cnt["r"] += 1
                                    parts = [(ustr_r, spt, [spb, b_const])]
                                    if racc_prev is not None:
                                        parts.append((ones_r, racc_prev[0], [racc_prev[1], b_const]))
                                    c.mm(pa[:, :], pab, parts)
                                    tm, tmb = nxt(tmp_ring, "tmp")
                                    c.op(dve, "scalar_tensor_tensor", tm, pS[:, :], 0.125, spt.bitcast(F32), op0=ALU.mult, op1=ALU.subtract,
                                         reads=[pSb, spb], writes=[tmb])
                                    t3, t3b = nxt(t3_ring, "t3")
                                    c.op(dve, "tensor_tensor", t3, tm, pa[:, :], op=ALU.subtract, reads=[tmb, pab], writes=[t3b])
                                    Pt, Pb = nxt(P_ring, "P")
                                    c.op(act, "activation", Pt, t3, AF.Exp, reads=[t3b], writes=[Pb])
                                    if diag:
                                        c.op(pool, "affine_select", Pt, Pt, pattern=[[1, 512]], compare_op=ALU.is_gt, fill=0.0,
                                             base=base, channel_multiplier=-1, reads=[Pb], writes=[Pb])
                                    c.mm(acc[:, :], accb, [(va4[:, kt, hh, :], Pt, [vb_, Pb])], start=(ki == 0), stop=(ki == len(kts) - 1))
                                    if ki < len(kts) - 1:
                                        rn, rnb = nxt(racc_ring, "bq")
                                        if racc_prev is None:
                                            c.op(pool, "tensor_copy", rn, spt, reads=[spb], writes=[rnb])
                                        else:
                                            c.op(pool, "tensor_tensor", rn, racc_prev[0].bitcast(F32), spt.bitcast(F32), op=ALU.add,
                                                 reads=[racc_prev[1], spb], writes=[rnb])
                                        racc_prev = (rn, rnb)
                                c.op(act, "copy", ot_[hs, qs], acc[0:64, :], reads=[accb], writes=[ob_])
                    c.dma(OT[hp * 128:(hp + 1) * 128, ts0:ts0 + SEQ], ot_.bitcast(F32), reads=[ob_], writes=[dOT])
            if debug and ("OT%d" % L) in dbg:
                c.dma(dbg["OT%d" % L], OT, reads=[dOT], writes=[dIN])

            c.phase_reset(KEEP)
            Wot, Wob = c.alloc(8 * D, MMDT)
            Wo3 = Wot.rearrange("p (c n) -> p c n", n=D)
            for cc in range(8):
                for n0 in range(0, D, 512):
                    c.dma(Wo3[:, cc, n0:n0 + 512], Wo[cc * 128:(cc + 1) * 128, n0:n0 + 512], reads=[dIN], writes=[Wob], q=wq)
            Wr, Wrb = c.alloc(8 * 36)
            Wr3 = Wr.rearrange("p (c n) -> p c n", n=36)
            c.dma(Wr3, w_rt[L].rearrange("(c p) n -> p c n", p=128), reads=[dIN], writes=[Wrb])
            gB, gbuf = c.alloc(D)
            bB, _ = c.alloc(D)
            brt, _ = c.alloc(36)
            c.dma(gB, ln1_g[L:L + 1, :].broadcast_to([128, D]), reads=[dIN], writes=[gbuf])
            c.dma(bB, ln1_b[L:L + 1, :].broadcast_to([128, D]), reads=[dIN], writes=[gbuf])
            c.dma(brt, b_rt[L:L + 1, :].broadcast_to([128, 36]), reads=[dIN], writes=[gbuf])
            c.op(dve, "memset", cnt_b, 0.0, writes=[b_cnt])
            otb_ring = [c.alloc(8 * 128, MMDT) for _ in range(2)]
            xr_ring = [c.alloc(D) for _ in range(2)]
            y_ring = [c.alloc(D) for _ in range(2)]
            x1_ring = [c.alloc(D) for _ in range(3)]
            x1T_ring = [c.alloc(8 * 128) for _ in range(2)]
            sc_ring = [{"st": c.alloc(12), "mv": c.alloc(4)} for _ in range(2)]
            rs_ring = [c.alloc(320) for _ in range(2)]
            for (rs, rsb) in rs_ring:
                c.op(dve, "memset", rs[:, 4:8], -BIG, writes=[rsb])
            for tt in range(T // 128):
                r0 = tt * 128
                otb, otbb = otb_ring[tt % 2]
                otb3 = otb.rearrange("p (c t) -> p c t", t=128)
                xr, xrb = xr_ring[tt % 2]
                y, yb = y_ring[tt % 2]
                x1, x1b = x1_ring[tt % 3]
                x1T, x1Tb = x1T_ring[tt % 2]
                x1T3 = x1T.rearrange("p (c t) -> p c t", t=128)
                sc = sc_ring[tt % 2]
                rs, rsb = rs_ring[tt % 2]
                c.dma(otb3, OT[:, r0:r0 + 128].rearrange("(c p) t -> p c t", p=128), reads=[dOT], writes=[otbb], q=wq)
                c.dma(xr, X[r0:r0 + 128, :], reads=[dX], writes=[xrb])
                for half in range(2):
                    pt, pb = PS[half]
                    c.mm(pt[:, :], pb, [(otb3[:, cc, :], Wo3[:, cc, half * 512:(half + 1) * 512], [otbb, Wob]) for cc in range(8)])
                    c.op(dve, "scalar_tensor_tensor", y[:, half * 512:(half + 1) * 512], xr[:, half * 512:(half + 1) * 512], ALPHA, pt[:, :],
                         op0=ALU.mult, op1=ALU.add, reads=[xrb, pb], writes=[yb])
                layer_norm(y, yb, gB, bB, gbuf, x1, x1b, sc)
                c.dma(X1[r0:r0 + 128, :], x1, reads=[x1b], writes=[dX1])
                for half in range(2):
                    pt, pb = PS[2 + half]
                    for cc in range(4):
                        c4 = half * 4 + cc
                        c.transpose(pt[:, cc * 128:(cc + 1) * 128], pb, x1[:, c4 * 128:(c4 + 1) * 128], [x1b], ident, signal=(cc == 3))
                    evac(x1T[:, half * 512:(half + 1) * 512], x1Tb, pt[:, :], pb)
                pl, plb = PS[4 + tt % 2]
                c.mm(pl[:, 0:36], plb, [(x1T3[:, cc, :], Wr3[:, cc, :], [x1Tb, Wrb]) for cc in range(8)])
                o = 40
                gm8 = rs[:, o:o + 8]; o += 8
                ngm = rs[:, o:o + 1]; o += 1
                ge = rs[:, o:o + 4]; o += 4
                gsum = rs[:, o:o + 1]; o += 1
                gp = rs[:, o:o + 1]; o += 1
                goh = rs[:, o:o + 4]; o += 4
                ml = rs[:, o:o + 32]; o += 32
                m8 = rs[:, o:o + 8]; o += 8
                sel = rs[:, o:o + 32]; o += 32
                sm = rs[:, o:o + 8]; o += 8
                ex = rs[:, o:o + 32]; o += 32
                wgt = rs[:, o:o + 32]; o += 32
                pos = rs[:, o:o + 32]; o += 32
                oh1 = rs[:, o:o + 32]; o += 32
                assert o <= 320
                R = dict(reads=[rsb], writes=[rsb])
                c.op(dve, "tensor_tensor", rs[:, 0:4], pl[:, 0:4], brt[:, 0:4], op=ALU.add, reads=[plb, gbuf], writes=[rsb])
                c.op(dve, "tensor_tensor", rs[:, 8:40], pl[:, 4:36], brt[:, 4:36], op=ALU.add, reads=[plb, gbuf], writes=[rsb])
                c.op(dve, "max", gm8, rs[:, 0:8], **R)
                c.op(dve, "tensor_scalar", ngm, gm8[:, 0:1], -1.0, None, op0=ALU.mult, **R)
                c.op(act, "activation", ge, rs[:, 0:4], AF.Exp, bias=ngm, scale=1.0, accum_out=gsum, **R)
                c.op(dve, "reciprocal", gp, gsum, **R)
                c.op(dve, "tensor_scalar", goh, rs[:, 0:4], gm8[:, 0:1], None, op0=ALU.is_ge, **R)
                c.op(dve, "tensor_scalar", goh, goh, 1.0, BIG, op0=ALU.subtract, op1=ALU.mult, **R)
                c.op(dve, "tensor_tensor", ml.rearrange("p (g e) -> p g e", e=8), rs[:, 8:40].rearrange("p (g e) -> p g e", e=8),
                     goh.unsqueeze(2).broadcast_to([128, 4, 8]), op=ALU.add, **R)
                c.op(dve, "max", m8, ml, **R)
                c.op(dve, "tensor_scalar", sel, ml, m8[:, 1:2], None, op0=ALU.is_ge, **R)
                c.op(dve, "tensor_scalar", oh1, ml, m8[:, 0:1], None, op0=ALU.is_ge, **R)
                c.op(dve, "tensor_tensor", sm[:, 0:1], m8[:, 1:2], m8[:, 0:1], op=ALU.subtract, **R)
                c.op(dve, "tensor_scalar", sm[:, 1:2], m8[:, 0:1], -1.0, None, op0=ALU.mult, **R)
                c.op(act, "activation", sm[:, 2:3], sm[:, 0:1], AF.Exp, **R)
                c.op(act, "activation", ex, ml, AF.Exp, bias=sm[:, 1:2], scale=1.0, **R)
                c.op(dve, "tensor_scalar", sm[:, 3:4], sm[:, 2:3], 1.0, None, op0=ALU.add, **R)
                c.op(dve, "reciprocal", sm[:, 3:4], sm[:, 3:4], **R)
                c.op(dve, "tensor_tensor", sm[:, 4:5], sm[:, 3:4], gp, op=ALU.mult, **R)
                c.op(dve, "scalar_tensor_tensor", wgt, ex, sm[:, 4:5], sel, op0=ALU.mult, op1=ALU.mult, **R)
                pp, ppb = PS[6 + tt % 2]
                c.mm(pp[:, 0:32], ppb, [(tri_f, sel, [b_const, rsb])])
                c.mm(pp[:, 32:64], ppb, [(ones_f, sel, [b_const, rsb])])
                c.op(dve, "tensor_tensor", pos, pp[:, 0:32], cnt_b, op=ALU.add, reads=[ppb, b_cnt, rsb], writes=[rsb])
                c.op(dve, "tensor_tensor", cnt_b, cnt_b, pp[:, 32:64], op=ALU.add, reads=[ppb, b_cnt], writes=[b_cnt])
                c.op(dve, "scalar_tensor_tensor", pos, pos, float(CAP - 1), eoff, op0=ALU.min, op1=ALU.add, reads=[rsb, b_const], writes=[rsb])
                c.op(dve, "tensor_tensor", sel, sel, oh1, op=ALU.subtract, **R)
                c.op(dve, "tensor_tensor", ex, oh1, pos, op=ALU.mult, **R)
                c.op(dve, "tensor_reduce", rt3[:, tt, 0:1], ex, axis=AX.X, op=ALU.add, reads=[rsb], writes=[b_rtab])
                c.op(dve, "tensor_tensor", ex, sel, pos, op=ALU.mult, **R)
                c.op(dve, "tensor_reduce", rt3[:, tt, 1:2], ex, axis=AX.X, op=ALU.add, reads=[rsb], writes=[b_rtab])
                c.op(dve, "tensor_tensor", ex, oh1, wgt, op=ALU.mult, **R)
                c.op(dve, "tensor_reduce", rt3[:, tt, 2:3], ex, axis=AX.X, op=ALU.add, reads=[rsb], writes=[b_rtab])
                c.op(dve, "tensor_tensor", ex, sel, wgt, op=ALU.mult, **R)
                c.op(dve, "tensor_reduce", rt3[:, tt, 3:4], ex, axis=AX.X, op=ALU.add, reads=[rsb], writes=[b_rtab])
                c.op(dve, "tensor_copy", ridx3[:, tt, :], rt3[:, tt, 0:2], reads=[b_rtab], writes=[b_ridx])
                for k in range(2):
                    c.dma(XS[:, :], x1, reads=[x1b, b_ridx], writes=[dXS], q=pool,
                          indirect=dict(out_offset=bass.IndirectOffsetOnAxis(ap=ridx3[:, tt, k:k + 1], axis=0), in_offset=None,
                                        bounds_check=bc_reg, oob_is_err=False))
            if debug and ("X1_%d" % L) in dbg:
                c.dma(dbg["X1_%d" % L], X1, reads=[dX1], writes=[dIN])
            if debug and ("rt%d" % L) in dbg:
                c.dma(dbg["rt%d" % L], rt, reads=[b_rtab], writes=[dIN])

            c.phase_reset(KEEP)
            w_ring = []
            for _ in range(2):
                w_ring.append((c.alloc(8 * DEXP, MMDT), c.alloc(8 * DEXP, MMDT), c.alloc(4 * D, MMDT)))
            xs_ring = [c.alloc(3 * D) for _ in range(2)]
            xsT_ring = [c.alloc(8 * CAP, MMDT) for _ in range(2)]
            hT_ring = [c.alloc(4 * CAP, MMDT) for _ in range(2)]
            sg_ring = [c.alloc(CAP) for _ in range(2)]
            ys_ring = [c.alloc(D) for _ in range(3)]
            NJ = CAP // 128
            ysi = 0
            sgi = 0

            def load_w(e):
                (wg, wgb), (wu, wub), (wd, wdb) = w_ring[e % 2]
                wg3 = wg.rearrange("p (c n) -> p c n", n=DEXP)
                wu3 = wu.rearrange("p (c n) -> p c n", n=DEXP)
                wd3 = wd.rearrange("p (c n) -> p c n", n=D)
                for cc in range(8):
                    c.dma(wg3[:, cc, :], w_e_gate[L, e, cc * 128:(cc + 1) * 128, :], reads=[dIN], writes=[wgb], q=wq)
                    c.dma(wu3[:, cc, :], w_e_up[L, e, cc * 128:(cc + 1) * 128, :], reads=[dIN], writes=[wub], q=wq)
                for cc in range(4):
                    for n0 in range(0, D, 512):
                        c.dma(wd3[:, cc, n0:n0 + 512], w_e_down[L, e, cc * 128:(cc + 1) * 128, n0:n0 + 512], reads=[dIN], writes=[wdb], q=wq)

            def load_xs(e):
                xs, xsb = xs_ring[e % 2]
                c.dma(xs.rearrange("p (j d) -> p j d", d=D), XS[e * CAP:(e + 1) * CAP, :].rearrange("(j p) d -> p j d", p=128),
                      reads=[dXS], writes=[xsb])

            load_w(0)
            load_xs(0)
            for e in range(NEXP):
                if e + 1 < NEXP:
                    load_w(e + 1)
                    load_xs(e + 1)
                (wg, wgb), (wu, wub), (wd, wdb) = w_ring[e % 2]
                wg3 = wg.rearrange("p (c n) -> p c n", n=DEXP)
                wu3 = wu.rearrange("p (c n) -> p c n", n=DEXP)
                wd3 = wd.rearrange("p (c n) -> p c n", n=D)
                xs, xsb = xs_ring[e % 2]
                xs3 = xs.rearrange("p (j d) -> p j d", d=D)
                xsT, xsTb = xsT_ring[e % 2]
                xsT3 = xsT.rearrange("p (c t) -> p c t", t=CAP)
                hT, hTb = hT_ring[e % 2]
                hT3 = hT.rearrange("p (c t) -> p c t", t=CAP)
                for cc in range(8):
                    pt, pb = PS[cc % 2]
                    for jj in range(NJ):
                        c.transpose(pt[:, jj * 128:(jj + 1) * 128], pb, xs3[:, jj, cc * 128:(cc + 1) * 128], [xsb], ident, signal=(jj == NJ - 1))
                    evac(xsT3[:, cc, :], xsTb, pt[:, 0:CAP], pb)
                for fc in range(4):
                    pg, pgb = PS[2 + fc % 2]
                    pu, pub = PS[4 + fc % 2]
                    c.mm(pg[:, 0:CAP], pgb, [(wg3[:, cc, fc * 128:(fc + 1) * 128], xsT3[:, cc, :], [wgb, xsTb]) for cc in range(8)])
                    c.mm(pu[:, 0:CAP], pub, [(wu3[:, cc, fc * 128:(fc + 1) * 128], xsT3[:, cc, :], [wub, xsTb]) for cc in range(8)])
                    sg, sgb = sg_ring[sgi % 2]; sgi += 1
                    c.op(act, "activation", sg, pg[:, 0:CAP], AF.Silu, reads=[pgb], writes=[sgb])
                    c.op(dve, "tensor_tensor", hT3[:, fc, :], sg, pu[:, 0:CAP], op=ALU.mult, reads=[sgb, pub], writes=[hTb])
                for jj in range(NJ):
                    ys, ysb = ys_ring[ysi % 3]; ysi += 1
                    for half in range(2):
                        pt, pb = PS[6 + half]
                        c.mm(pt[:, :], pb, [(hT3[:, fc, jj * 128:(jj + 1) * 128], wd3[:, fc, half * 512:(half + 1) * 512], [hTb, wdb]) for fc in range(4)])
                        evac(ys[:, half * 512:(half + 1) * 512], ysb, pt[:, :], pb)
                    r0 = e * CAP + jj * 128
                    c.dma(YS[r0:r0 + 128, :], ys, reads=[ysb], writes=[dYS])

            c.phase_reset(KEEP)
            Wg_, Wgb_ = c.alloc(8 * D, MMDT)
            Wg3 = Wg_.rearrange("p (c n) -> p c n", n=D)
            for cc in range(8):
                for n0 in range(0, D, 512):
                    c.dma(Wg3[:, cc, n0:n0 + 512], w_ple_gate[L, cc * 128:(cc + 1) * 128, n0:n0 + 512], reads=[dIN], writes=[Wgb_], q=wq)
            Wp_, Wpb_ = c.alloc(2 * D, MMDT)
            Wp3 = Wp_.rearrange("p (c n) -> p c n", n=D)
            for cc in range(2):
                for n0 in range(0, D, 512):
                    c.dma(Wp3[:, cc, n0:n0 + 512], w_ple_proj[L, cc * 128:(cc + 1) * 128, n0:n0 + 512], reads=[dIN], writes=[Wpb_], q=wq)
            gB, gbuf = c.alloc(D)
            bB, _ = c.alloc(D)
            c.dma(gB, ln2_g[L:L + 1, :].broadcast_to([128, D]), reads=[dIN], writes=[gbuf])
            c.dma(bB, ln2_b[L:L + 1, :].broadcast_to([128, D]), reads=[dIN], writes=[gbuf])
            y1_ring = [c.alloc(D) for _ in range(2)]
            y2_ring = [c.alloc(D) for _ in range(2)]
            x1_ring = [c.alloc(D) for _ in range(2)]
            pr_ring = [c.alloc(PLE) for _ in range(2)]
            x2_ring = [c.alloc(D) for _ in range(2)]
            x2T_ring = [c.alloc(8 * 128, MMDT) for _ in range(2)]
            pT_ring = [c.alloc(2 * 128, MMDT) for _ in range(2)]
            sig_ring = [c.alloc(D) for _ in range(2)]
            sc_ring = [{"st": c.alloc(12), "mv": c.alloc(4)} for _ in range(2)]
            dst = out if L == DEPTH - 1 else X
            dstb = dIN if L == DEPTH - 1 else dX
            for tt in range(T // 128):
                r0 = tt * 128
                i2 = tt % 2
                y1, y1b = y1_ring[i2]; y2, y2b = y2_ring[i2]; x1, x1b = x1_ring[i2]; pr, prb = pr_ring[i2]
                y, yb = y1, y1b; x2, x2b = x2_ring[i2]; x2T, x2Tb = x2T_ring[i2]; pT, pTb = pT_ring[i2]
                sig, sigb = sig_ring[i2]; x3, x3b = sig, sigb; sc = sc_ring[i2]
                x2T3 = x2T.rearrange("p (c t) -> p c t", t=128)
                pT3 = pT.rearrange("p (c t) -> p c t", t=128)
                for k, (yy, yyb) in enumerate(((y1, y1b), (y2, y2b))):
                    c.dma(yy, YS[:, :], reads=[dYS, b_ridx], writes=[yyb], q=pool,
                          indirect=dict(out_offset=None, in_offset=bass.IndirectOffsetOnAxis(ap=ridx3[:, tt, k:k + 1], axis=0),
                                        bounds_check=bc_reg, oob_is_err=False))
                c.dma(x1, X1[r0:r0 + 128, :], reads=[dX1], writes=[x1b])
                c.dma(pr, p_in[L, r0:r0 + 128, :], reads=[dIN], writes=[prb])
                c.op(dve, "tensor_scalar", y2, y2, rt3[:, tt, 3:4], None, op0=ALU.mult, reads=[y2b, b_rtab], writes=[y2b])
                c.op(dve, "scalar_tensor_tensor", y1, y1, rt3[:, tt, 2:3], y2, op0=ALU.mult, op1=ALU.add, reads=[y1b, y2b, b_rtab], writes=[y1b])
                c.op(dve, "scalar_tensor_tensor", y, x1, ALPHA, y1, op0=ALU.mult, op1=ALU.add, reads=[x1b, y1b], writes=[yb])
                layer_norm(y, yb, gB, bB, gbuf, x2, x2b, sc)
                for half in range(2):
                    pt, pb = PS[half]
                    for cc in range(4):
                        c4 = half * 4 + cc
                        c.transpose(pt[:, cc * 128:(cc + 1) * 128], pb, x2[:, c4 * 128:(c4 + 1) * 128], [x2b], ident, signal=(cc == 3))
                    evac(x2T[:, half * 512:(half + 1) * 512], x2Tb, pt[:, :], pb)
                pt, pb = PS[2]
                for cc in range(2):
                    c.transpose(pt[:, cc * 128:(cc + 1) * 128], pb, pr[:, cc * 128:(cc + 1) * 128], [prb], ident, signal=(cc == 1))
                evac(pT, pTb, pt[:, 0:256], pb)
                for half in range(2):
                    hsl = slice(half * 512, (half + 1) * 512)
                    pg, pgb = PS[4 + half]
                    c.mm(pg[:, :], pgb, [(x2T3[:, cc, :], Wg3[:, cc, hsl], [x2Tb, Wgb_]) for cc in range(8)])
                    c.op(act, "activation", sig[:, hsl], pg[:, :], AF.Sigmoid, reads=[pgb], writes=[sigb])
                    pq, pqb = PS[6 + half]
                    c.mm(pq[:, :], pqb, [(pT3[:, cc, :], Wp3[:, cc, hsl], [pTb, Wpb_]) for cc in range(2)])
                    c.op(dve, "tensor_tensor", sig[:, hsl], sig[:, hsl], pq[:, :], op=ALU.mult, reads=[sigb, pqb], writes=[sigb])
                c.op(pool, "tensor_tensor", x3, sig, x2, op=ALU.add, reads=[sigb, x2b], writes=[x3b])
                c.dma(dst[r0:r0 + 128, :], x3, reads=[x3b], writes=[dstb])
            if debug and ("X3_%d" % L) in dbg:
                c.barrier()
                c.dma(dbg["X3_%d" % L], dst, reads=[dstb], writes=[dIN])

        if nlayers < DEPTH:
            c.barrier()
            c.dma(out, X, reads=[dX], writes=[dIN])
        c.barrier()
    return nc


def _consts():
    cst = np.zeros((128, 6, 128), np.float32)
    k = np.arange(128)[:, None]
    m = np.arange(128)[None, :]
    cst[:, 0, :] = (k == m)
    cst[:, 1, :] = 1.0
    cst[:, 2, :] = (k < m)
    cst[:, 3, :] = (k > m)
    q = np.arange(512)[None, :]
    kk = np.arange(128)[:, None]
    cmask = np.zeros((9, 128, 512), np.float32)
    for i in range(4):
        cmask[i] = np.where((-128 * i + q - kk) >= 0, 0.0, -BIG)
    for i in range(5):
        dlt = 128 - 128 * i
        v = dlt + q - kk
        cmask[4 + i] = np.where((v >= 0) & (v < 128), 0.0, -BIG)
    cbase = (-(q - kk)).astype(np.float32)
    ceoff = np.tile((np.arange(NEXP) * CAP).astype(np.float32)[None, :], (128, 1))
    return cst, cmask, np.ascontiguousarray(cbase), np.ascontiguousarray(ceoff)


_NC_CACHE = {}


def kernel(**inputs):
    f = lambda a: np.ascontiguousarray(np.asarray(a, dtype=np.float32))
    x = f(inputs["x"]).reshape(NCORES, T, D)
    p = f(inputs["p"]).reshape(DEPTH, NCORES, T, PLE)
    cst, cmask, cbase, ceoff = _consts()
    w_rt = np.ascontiguousarray(np.concatenate([f(inputs["w_grp"]), f(inputs["w_exp"])], axis=2))
    b_rt = np.ascontiguousarray(np.concatenate([f(inputs["b_grp"]), f(inputs["b_exp"])], axis=1))
    shared = {k: f(inputs[k]) for k in ("w_qkv_a", "w_o_a", "w_qkv_b", "w_o_b", "sinks_b", "w_qkv_c", "w_o_c", "ln1_g", "ln1_b",
                                         "ln2_g", "ln2_b", "w_e_gate", "w_e_up", "w_e_down", "w_ple_gate", "w_ple_proj")}
    shared.update(w_rt=w_rt, b_rt=b_rt, cst=cst, cmask=cmask, cbase=cbase, ceoff=ceoff)
    if "nc" not in _NC_CACHE:
        _NC_CACHE["nc"] = build()
    nc = _NC_CACHE["nc"]
    in_maps = []
    for i in range(NCORES):
        m = dict(shared)
        m["x"] = x[i]
        m["p"] = np.ascontiguousarray(p[:, i])
        in_maps.append(m)
    res = run_bass_kernel_spmd(nc, in_maps, core_ids=list(range(NCORES)))
    o = np.stack([np.asarray(r["out"]) for r in res.results], axis=0)
    return o.reshape(16, SEQ, D).astype(np.float32)
```

```python
import numpy as np
import concourse.bass as bass
import concourse.mybir as mybir
from concourse.bass_utils import run_bass_kernel_spmd

F32 = mybir.dt.float32
F32R = mybir.dt.float32r
I32 = mybir.dt.int32
ALU = mybir.AluOpType
AF = mybir.ActivationFunctionType
AX = mybir.AxisListType

USE_F32R = True
MMDT = F32R if USE_F32R else F32

NCORES = 8
D = 1024
SEQ = 2048
NSEQ = 2
T = NSEQ * SEQ
DEPTH = 4
NH = 16
HD = 64
NEXP = 32
DEXP = 512
CAP = 384
PLE = 256
ALPHA = (2.0 * DEPTH) ** 0.25
EPS = 1e-5
BIG = 30000.0
SLOPES = [2.0 ** (-8.0 * (h + 1) / NH) for h in range(NH)]
XS_ROWS = NEXP * CAP + 128


class Eng:
    def __init__(self, name, h, sem):
        self.name, self.h, self.sem = name, h, sem
        self.count = 0
        self.known = {}


class Buf:
    __slots__ = ("name", "writers", "readers")

    def __init__(self, name):
        self.name = name
        self.writers = {}
        self.readers = {}


class Ctx:
    def __init__(self, nc, es):
        self.nc = nc
        self.es = es
        mk = lambda n, h: Eng(n, h, es.enter_context(nc.semaphore("s_" + n)))
        self.pe = mk("pe", nc.tensor)
        self.act = mk("act", nc.scalar)
        self.dve = mk("dve", nc.vector)
        self.pool = mk("pool", nc.gpsimd)
        self.sp = mk("sp", nc.sync)
        self.engs = [self.pe, self.act, self.dve, self.pool, self.sp]
        self.sems = {e.name: e.sem for e in self.engs}
        self.ndma = 40
        self.dsem = []
        for i in range(self.ndma):
            s = es.enter_context(nc.semaphore("s_dma%d" % i))
            self.dsem.append(s)
            self.sems["d%d" % i] = s
        self.dval = [0] * self.ndma
        self.dnext = 0
        self.FW, self.RW = 19100, 34048
        self.arena = es.enter_context(nc.sbuf_tensor("arena_f", [128, self.FW], F32))
        self.arena_r = es.enter_context(nc.sbuf_tensor("arena_r", [128, self.RW], MMDT))
        self.atop = 0
        self.rtop = 0
        self.psum = []
        for i in range(8):
            t = es.enter_context(nc.psum_tensor("psb%d" % i, [128, 512], F32))
            self.psum.append((t, Buf("ps%d" % i)))
        self.nbuf = 0

    def alloc(self, ncols, dt=F32, name=None):
        if dt == MMDT and USE_F32R:
            a = self.arena_r[:, self.rtop:self.rtop + ncols]
            self.rtop += ncols
            assert self.rtop <= self.RW, "SBUF R arena overflow %d" % self.rtop
        else:
            a = self.arena[:, self.atop:self.atop + ncols]
            self.atop += ncols
            assert self.atop <= self.FW, "SBUF F arena overflow %d" % self.atop
            if dt != F32:
                a = a.bitcast(dt)
        self.nbuf += 1
        return a, Buf(name or ("b%d" % self.nbuf))

    def wait(self, E, key, val):
        if E.known.get(key, 0) >= val:
            return
        E.h.wait_ge(self.sems[key], val)
        E.known[key] = val

    def _deps(self, E, reads, writes):
        evs = {}
        for b in reads:
            for k, v in b.writers.items():
                evs[k] = max(evs.get(k, 0), v)
        for b in writes:
            for k, v in b.readers.items():
                if k != E.name:
                    evs[k] = max(evs.get(k, 0), v)
            for k, v in b.writers.items():
                if k != E.name:
                    evs[k] = max(evs.get(k, 0), v)
        for k, v in evs.items():
            self.wait(E, k, v)

    def _record(self, key, val, reads, writes):
        for b in reads:
            b.readers[key] = max(b.readers.get(key, 0), val)
        for b in writes:
            if b.readers:
                b.readers = {}
                b.writers = {key: val}
            else:
                b.writers[key] = max(b.writers.get(key, 0), val)

    def op(self, E, fname, *args, reads=(), writes=(), signal=True, **kw):
        self._deps(E, reads, writes)
        ins = getattr(E.h, fname)(*args, **kw)
        if signal:
            E.count += 1
            ins.then_inc(E.sem, 1)
            self._record(E.name, E.count, reads, writes)
        else:
            self._record(E.name, E.count + 1, reads, writes)
        return ins

    def dma(self, out, in_, reads=(), writes=(), q=None, indirect=None, **kw):
        Q = q or self.sp
        self._deps(Q, reads, writes)
        i = self.dnext
        self.dnext = (self.dnext + 1) % self.ndma
        key = "d%d" % i
        self.wait(Q, key, self.dval[i])
        if indirect is None:
            ins = Q.h.dma_start(out=out, in_=in_, **kw)
        else:
            ins = Q.h.indirect_dma_start(out=out, in_=in_, **indirect)
        self.dval[i] += 16
        ins.then_inc(self.dsem[i], 16)
        self._record(key, self.dval[i], reads, writes)

    def barrier(self):
        for E in self.engs:
            for F in self.engs:
                if F is not E and F.count > 0:
                    self.wait(E, F.name, F.count)
            for i in range(self.ndma):
                if self.dval[i] > 0:
                    self.wait(E, "d%d" % i, self.dval[i])

    def phase_reset(self, keep):
        self.barrier()
        self.atop, self.rtop = keep

    def mm(self, out, outbuf, parts, start=True, stop=True):
        n = len(parts)
        for i, (l, r, rb) in enumerate(parts):
            last = (i == n - 1)
            self.op(self.pe, "matmul", out, l, r, start=(start and i == 0), stop=(stop and last),
                    reads=rb, writes=[outbuf], signal=last)

    def transpose(self, out, outbuf, in_, inbufs, ident, signal=True):
        self.op(self.pe, "transpose", out, in_, ident, reads=inbufs, writes=[outbuf], signal=signal)


def build(nlayers=DEPTH, debug=None):
    from contextlib import ExitStack
    nc = bass.Bass("TRN2", target_bir_lowering=False)
    dr = lambda n, s, kind="ExternalInput", dt=F32: nc.dram_tensor(n, list(s), dt, kind=kind).ap()
    x_in = dr("x", [T, D])
    p_in = dr("p", [DEPTH, T, PLE])
    w_qkv_a = dr("w_qkv_a", [2, D, 3 * D]); w_o_a = dr("w_o_a", [2, D, D])
    w_qkv_b = dr("w_qkv_b", [1, D, D + 512]); w_o_b = dr("w_o_b", [1, D, D])
    sinks_b = dr("sinks_b", [1, NH])
    w_qkv_c = dr("w_qkv_c", [1, D, 3 * D]); w_o_c = dr("w_o_c", [1, D, D])
    ln1_g = dr("ln1_g", [DEPTH, D]); ln1_b = dr("ln1_b", [DEPTH, D])
    ln2_g = dr("ln2_g", [DEPTH, D]); ln2_b = dr("ln2_b", [DEPTH, D])
    w_rt = dr("w_rt", [DEPTH, D, 36]); b_rt = dr("b_rt", [DEPTH, 36])
    w_e_gate = dr("w_e_gate", [DEPTH, NEXP, D, DEXP]); w_e_up = dr("w_e_up", [DEPTH, NEXP, D, DEXP])
    w_e_down = dr("w_e_down", [DEPTH, NEXP, DEXP, D])
    w_ple_gate = dr("w_ple_gate", [DEPTH, D, D]); w_ple_proj = dr("w_ple_proj", [DEPTH, PLE, D])
    cst = dr("cst", [128, 6, 128])
    cmask = dr("cmask", [9, 128, 512])
    cbase = dr("cbase", [128, 512])
    ceoff = dr("ceoff", [128, NEXP])
    out = dr("out", [T, D], kind="ExternalOutput")
    X = dr("Xs", [T, D], kind="Internal"); X1 = dr("X1s", [T, D], kind="Internal")
    QT = dr("QTs", [D, T], kind="Internal"); KT = dr("KTs", [D, T], kind="Internal")
    V = dr("Vs", [T, D], kind="Internal"); OT = dr("OTs", [D, T], kind="Internal")
    XS = dr("XSs", [XS_ROWS, D], kind="Internal"); YS = dr("YSs", [XS_ROWS, D], kind="Internal")
    dbg = {}
    if debug:
        for nme, shp in debug.items():
            dbg[nme] = dr("dbg_" + nme, shp, kind="ExternalOutput")
    dX, dX1, dQT, dKT, dV, dOT, dXS, dYS = [Buf(n) for n in ("X", "X1", "QT", "KT", "V", "OT", "XS", "YS")]
    dIN = Buf("in")

    with ExitStack() as es:
        c = Ctx(nc, es)
        pe, act, dve, pool, sp = c.pe, c.act, c.dve, c.pool, c.sp
        wq = pool if USE_F32R else sp
        PS = c.psum
        bc_reg = nc.gpsimd.to_reg(XS_ROWS - 1)

        ident, b_const = c.alloc(128)
        ones_r, _ = c.alloc(128, MMDT)
        tri_f, _ = c.alloc(128)
        ustr_r, _ = c.alloc(128, MMDT)
        ones_f, _ = c.alloc(128)
        cb_f, _ = c.alloc(512)
        eoff, _ = c.alloc(NEXP)
        rt, b_rtab = c.alloc(32 * 4)
        ridx, b_ridx = c.alloc(32 * 2, I32)
        cnt_b, b_cnt = c.alloc(NEXP)
        c.dma(ident, cst[:, 0, :], writes=[b_const])
        c.dma(ones_f, cst[:, 1, :], writes=[b_const])
        c.dma(tri_f, cst[:, 2, :], writes=[b_const])
        c.dma(ones_r, cst[:, 1, :], writes=[b_const], q=wq)
        c.dma(ustr_r, cst[:, 3, :], writes=[b_const], q=wq)
        c.dma(cb_f, cbase, writes=[b_const])
        c.dma(eoff, ceoff, writes=[b_const])
        KEEP = (c.atop, c.rtop)
        rt3 = rt.rearrange("p (t k) -> p t k", k=4)
        ridx3 = ridx.rearrange("p (t k) -> p t k", k=2)

        for i in range(8):
            c.dma(X[i * 512:(i + 1) * 512, :], x_in[i * 512:(i + 1) * 512, :], reads=[dIN], writes=[dX])

        state = {"ev": 0}

        def evac(out_ap, outb, in_ap, inb, extra_reads=()):
            state["ev"] ^= 1
            if state["ev"]:
                c.op(act, "copy", out_ap, in_ap, reads=[inb, *extra_reads], writes=[outb])
            else:
                c.op(dve, "tensor_copy", out_ap, in_ap, reads=[inb, *extra_reads], writes=[outb])

        def layer_norm(y, yb, gB, bB, gbuf, outt, outb, sc):
            st, stb = sc["st"]
            mv, mvb = sc["mv"]
            c.op(dve, "bn_stats", st[:, 0:6], y[:, 0:512], reads=[yb], writes=[stb])
            c.op(dve, "bn_stats", st[:, 6:12], y[:, 512:1024], reads=[yb], writes=[stb])
            c.op(dve, "bn_aggr", mv[:, 0:2], st[:, 0:12], reads=[stb], writes=[mvb])
            c.op(dve, "tensor_scalar", mv[:, 2:3], mv[:, 1:2], EPS, None, op0=ALU.add, reads=[mvb], writes=[mvb])
            c.op(act, "activation", mv[:, 2:3], mv[:, 2:3], AF.Sqrt, reads=[mvb], writes=[mvb])
            c.op(dve, "reciprocal", mv[:, 2:3], mv[:, 2:3], reads=[mvb], writes=[mvb])
            c.op(dve, "scalar_tensor_tensor", mv[:, 3:4], mv[:, 0:1], -1.0, mv[:, 2:3], op0=ALU.mult, op1=ALU.mult,
                 reads=[mvb], writes=[mvb])
            c.op(act, "activation", y, y, AF.Identity, bias=mv[:, 3:4], scale=mv[:, 2:3], reads=[yb, mvb], writes=[yb])
            c.op(dve, "tensor_tensor", y, y, gB, op=ALU.mult, reads=[yb, gbuf], writes=[yb])
            c.op(pool, "tensor_tensor", outt, y, bB, op=ALU.add, reads=[yb, gbuf], writes=[outb])

        for L in range(nlayers):
            mixer, j = L % 3, L // 3
            if mixer == 0:
                Wqkv, Wo, NQ, NKV = w_qkv_a[j], w_o_a[j], 1024, 1024
            elif mixer == 1:
                Wqkv, Wo, NQ, NKV = w_qkv_b[j], w_o_b[j], 1024, 256
            else:
                Wqkv, Wo, NQ, NKV = w_qkv_c[j], w_o_c[j], 1024, 1024
            NQKV = NQ + 2 * NKV

            c.phase_reset(KEEP)
            Wt, Wb = c.alloc(8 * NQKV, MMDT)
            W3 = Wt.rearrange("p (c n) -> p c n", n=NQKV)
            for cc in range(8):
                for n0 in range(0, NQKV, 512):
                    c.dma(W3[:, cc, n0:n0 + 512], Wqkv[cc * 128:(cc + 1) * 128, n0:n0 + 512], reads=[dIN], writes=[Wb], q=wq)
            xr_ring = [c.alloc(4 * 1024) for _ in range(2)]
            xT_ring = [c.alloc(8 * 512, MMDT) for _ in range(2)]
            stg_ring = [c.alloc(512) for _ in range(4)]
            sti = 0
            pso = 0
            for tb in range(T // 512):
                xr, xrb = xr_ring[tb % 2]
                xT, xTb = xT_ring[tb % 2]
                xr3 = xr.rearrange("p (j d) -> p j d", d=1024)
                xT3 = xT.rearrange("p (c t) -> p c t", t=512)
                c.dma(xr3, X[tb * 512:(tb + 1) * 512, :].rearrange("(j p) d -> p j d", p=128), reads=[dX], writes=[xrb])
                for cc in range(8):
                    pt, pb = PS[cc % 2]
                    for jj in range(4):
                        c.transpose(pt[:, jj * 128:(jj + 1) * 128], pb, xr3[:, jj, cc * 128:(cc + 1) * 128], [xrb], ident,
                                    signal=(jj == 3))
                    evac(xT3[:, cc, :], xTb, pt[:, :], pb)
                for fc in range((NQ + NKV) // 128):
                    pt, pb = PS[2 + pso % 4]; pso += 1
                    c.mm(pt[:, :], pb, [(W3[:, cc, fc * 128:(fc + 1) * 128], xT3[:, cc, :], [Wb, xTb]) for cc in range(8)])
                    sg, sgb = stg_ring[sti % 4]; sti += 1
                    evac(sg, sgb, pt[:, :], pb)
                    if fc < NQ // 128:
                        c.dma(QT[fc * 128:(fc + 1) * 128, tb * 512:(tb + 1) * 512], sg, reads=[sgb], writes=[dQT])
                    else:
                        f2 = fc - NQ // 128
                        c.dma(KT[f2 * 128:(f2 + 1) * 128, tb * 512:(tb + 1) * 512], sg, reads=[sgb], writes=[dKT])
                for jj in range(4):
                    for v0 in range(0, NKV, 512):
                        nv = min(512, NKV - v0)
                        pt, pb = PS[2 + pso % 4]; pso += 1
                        c.mm(pt[:, 0:nv], pb, [(xT3[:, cc, jj * 128:(jj + 1) * 128], W3[:, cc, NQ + NKV + v0:NQ + NKV + v0 + nv], [Wb, xTb])
                                               for cc in range(8)])
                        sg, sgb = stg_ring[sti % 4]; sti += 1
                        evac(sg[:, 0:nv], sgb, pt[:, 0:nv], pb)
                        r0 = tb * 512 + jj * 128
                        c.dma(V[r0:r0 + 128, v0:v0 + nv], sg[:, 0:nv], reads=[sgb], writes=[dV])
            if debug and ("QT%d" % L) in dbg:
                c.dma(dbg["QT%d" % L], QT, reads=[dQT], writes=[dIN])
                c.dma(dbg["KT%d" % L], KT, reads=[dKT], writes=[dIN])
                c.dma(dbg["V%d" % L], V, reads=[dV], writes=[dIN])

            c.phase_reset(KEEP)
            LA = 3
            ld_ring = []
            for _ in range(2):
                qt_, qb_ = c.alloc(SEQ, MMDT)
                kt_, kb_ = c.alloc(SEQ, MMDT)
                va_, vb_ = c.alloc(16 * 2 * 128, MMDT)
                ld_ring.append((qt_, qb_, kt_, kb_, va_, vb_))
                c.op(dve, "tensor_copy", va_.rearrange("p (a b) -> p a b", b=128), ones_f.unsqueeze(1).broadcast_to([128, 32, 128]),
                     reads=[b_const], writes=[vb_])
            ot_ring = [c.alloc(SEQ, MMDT) for _ in range(2)]
            tmp_ring = [c.alloc(512) for _ in range(3)]
            P_ring = [c.alloc(512, MMDT) for _ in range(LA + 3)]
            rec_ring = [c.alloc(512) for _ in range(2)]
            cnt = {}

            def nxt(ring, key):
                i = cnt.get(key, 0)
                cnt[key] = i + 1
                return ring[i % len(ring)]

            if mixer in (0, 1):
                bq_ring = [c.alloc(512) for _ in range(2)]
                nm = 4 if mixer == 0 else 5
                masks = [c.alloc(512) for _ in range(nm)]
                for i, (m, mb) in enumerate(masks):
                    c.dma(m, cmask[i if mixer == 0 else 4 + i], writes=[mb])
                bqm_ring = [[c.alloc(512) for _ in range(nm)] for _ in range(2)]
            if mixer == 0:
                comb_ring = [c.alloc(512) for _ in range(3)]
                km, kmb = c.alloc(8)
                kmr, kmrb = c.alloc(8, MMDT)
                kmrep, kmrepb = c.alloc(8 * 128, MMDT)
                kmrep3 = kmrep.rearrange("p (n m) -> p n m", m=128)
                gsb_ring = [c.alloc(8) for _ in range(4)]
                top_ring = [c.alloc(12) for _ in range(4)]
                diag_ring = [c.alloc(128, MMDT) for _ in range(8)]
                thr_ring = [c.alloc(512) for _ in range(2)]
            elif mixer == 1:
                esk, eskb = c.alloc(NH)
                c.dma(esk, sinks_b[0:1, :].broadcast_to([128, NH]), reads=[dIN], writes=[eskb])
                c.op(act, "activation", esk, esk, AF.Exp, reads=[eskb], writes=[eskb])
            else:
                sp_ring = [c.alloc(512, MMDT) for _ in range(4)]
                e_ring = [c.alloc(512) for _ in range(2)]
                racc_ring = [c.alloc(512, MMDT) for _ in range(5)]
                t3_ring = [c.alloc(512) for _ in range(2)]

            units = [(s, hp) for s in range(NSEQ) for hp in range(8)]

            def load_unit(u):
                s, hp = units[u]
                qt_, qb_, kt_, kb_, va_, vb_ = ld_ring[u % 2]
                va4 = va_.rearrange("p (k h m) -> p k h m", h=2, m=128)
                ts0 = s * SEQ
                c.dma(qt_, QT[hp * 128:(hp + 1) * 128, ts0:ts0 + SEQ], reads=[dQT], writes=[qb_], q=wq)
                if mixer == 1:
                    g = hp // 2
                    for hh in range(2):
                        c.dma(kt_[hh * 64:(hh + 1) * 64, :], KT[g * 64:(g + 1) * 64, ts0:ts0 + SEQ], reads=[dKT], writes=[kb_], q=wq)
                        c.dma(va4[:, :, hh, 0:64], V[ts0:ts0 + SEQ, g * 64:(g + 1) * 64].rearrange("(k p) d -> p k d", p=128),
                              reads=[dV], writes=[vb_], q=wq)
                else:
                    c.dma(kt_, KT[hp * 128:(hp + 1) * 128, ts0:ts0 + SEQ], reads=[dKT], writes=[kb_], q=wq)
                    for hh in range(2):
                        f0 = hp * 128 + hh * 64
                        c.dma(va4[:, :, hh, 0:64], V[ts0:ts0 + SEQ, f0:f0 + 64].rearrange("(k p) d -> p k d", p=128),
                              reads=[dV], writes=[vb_], q=wq)

            def unit_prep(u):
                qt_, qb_, kt_, kb_, va_, vb_ = ld_ring[u % 2]
                c.op(dve, "tensor_reduce", km, kt_.bitcast(F32).rearrange("p (n k) -> p n k", k=256), axis=AX.X, op=ALU.add,
                     reads=[kb_], writes=[kmb])
                c.op(dve, "tensor_scalar", km, km, 1.0 / 256.0, None, op0=ALU.mult, reads=[kmb], writes=[kmb])
                c.op(dve, "tensor_copy", kmr, km, reads=[kmb], writes=[kmrb])
                for n in range(8):
                    c.op(dve, "tensor_scalar", kmrep3[:, n, :], ones_f, km[:, n:n + 1], None, op0=ALU.mult,
                         reads=[kmb, b_const], writes=[kmrepb])

            def head_prep(u, hh, st):
                s, hp = units[u]
                slope = SLOPES[hp * 2 + hh]
                bq, bqb = nxt(bq_ring, "bq")
                c.op(dve, "tensor_scalar", bq, cb_f, slope, None, op0=ALU.mult, reads=[b_const], writes=[bqb])
                bqm = bqm_ring[cnt.get("bqm", 0) % 2]
                cnt["bqm"] = cnt.get("bqm", 0) + 1
                for mi in range(nm):
                    c.op(dve, "tensor_tensor", bqm[mi][0], bq, masks[mi][0], op=ALU.add, reads=[bqb, masks[mi][1]], writes=[bqm[mi][1]])
                st["bq"], st["bqb"], st["bqm"] = bq, bqb, bqm

            def sel_prep1(u, hh, qc, st):
                qt_, qb_ = ld_ring[u % 2][0:2]
                hs = slice(hh * 64, (hh + 1) * 64)
                dgs = []
                for jq in range(4):
                    qtile = qc * 4 + jq
                    blk = qtile // 2
                    q128 = slice(qtile * 128, (qtile + 1) * 128)
                    pg, pgb = PS[7]
                    c.mm(pg[:, 0:8], pgb, [(qt_[hs, q128], kmr[hs, 0:8], [qb_, kmrb])])
                    gs, gsb = nxt(gsb_ring, "gsb")
                    c.op(dve, "memset", gs, -BIG, writes=[gsb])
                    c.op(dve, "tensor_copy", gs[:, 0:blk], pg[:, 0:blk], reads=[pgb], writes=[gsb])
                    tp, tpb = nxt(top_ring, "top")
                    c.op(dve, "max", tp[:, 0:8], gs, reads=[gsb], writes=[tpb])
                    c.op(dve, "tensor_tensor", tp[:, 8:9], tp[:, 2:3], tp[:, 3:4], op=ALU.add, reads=[tpb], writes=[tpb])
                    c.op(dve, "tensor_scalar", tp[:, 9:10], tp[:, 8:9], 0.5, None, op0=ALU.mult, reads=[tpb], writes=[tpb])
                    dg, dgb = nxt(diag_ring, "diag")
                    c.op(dve, "tensor_scalar", dg, ident, tp[:, 9:10], None, op0=ALU.mult, reads=[tpb, b_const], writes=[dgb])
                    dgs.append((dg, dgb))
                st[("dg", qc)] = dgs

            def sel_prep2(u, hh, qc, st):
                thr, thrb = nxt(thr_ring, "thr")
                pth, pthb = PS[7]
                for jq, (dg, dgb) in enumerate(st[("dg", qc)]):
                    c.mm(pth[:, jq * 128:(jq + 1) * 128], pthb, [(ones_r, dg, [dgb, b_const])])
                c.op(act, "copy", thr, pth[:, :], reads=[pthb], writes=[thrb])
                st[("thr", qc)] = (thr, thrb)

            tasks = []
            for u, (s, hp) in enumerate(units):
                qt_, qb_, kt_, kb_, va_, vb_ = ld_ring[u % 2]
                ot_, ob_ = ot_ring[u % 2]
                va4 = va_.rearrange("p (k h m) -> p k h m", h=2, m=128)
                ts0 = s * SEQ
                utasks = []
                for hh in range(2):
                    h = hp * 2 + hh
                    hs = slice(hh * 64, (hh + 1) * 64)
                    slope = SLOPES[h]
                    st = {}
                    for qc in range(4):
                        qs = slice(qc * 512, (qc + 1) * 512)
                        if mixer == 0:
                            kts = list(range(0, 4 * qc + 4))
                        elif mixer == 1:
                            kts = list(range(max(0, 4 * qc - 1), 4 * qc + 4))
                        else:
                            kts = list(range(4 * qc + 3, -1, -1))
                        grp = {"acc": None, "racc": None}
                        nk = len(kts)
                        for ki, kt in enumerate(kts):
                            ks = slice(kt * 128, (kt + 1) * 128)
                            first, last = (ki == 0), (ki == nk - 1)
                            pre = []
                            if hh == 0 and qc == 0 and ki == 0:
                                if u == 0:
                                    pre.append(lambda u=u: load_unit(u))
                                if mixer == 0:
                                    pre.append(lambda u=u: unit_prep(u))
                            if qc == 0 and ki == 0 and mixer in (0, 1):
                                pre.append(lambda u=u, hh=hh, st=st: head_prep(u, hh, st))
                            if mixer == 0 and qc in (1, 2):
                                if ki == 0:
                                    pre.append(lambda u=u, hh=hh, qc=qc, st=st: sel_prep1(u, hh, qc + 1, st))
                                if ki == nk // 2:
                                    pre.append(lambda u=u, hh=hh, qc=qc, st=st: sel_prep2(u, hh, qc + 1, st))
                            if hh == 0 and qc == 1 and ki == 0 and u + 1 < len(units):
                                pre.append(lambda u=u: load_unit(u + 1))

                            if mixer in (0, 1):
                                def A(pre=pre, grp=grp, st=st, kt=kt, ks=ks, qs=qs, qc=qc, hs=hs, first=first, qt_=qt_, qb_=qb_, kt_=kt_, kb_=kb_,
                                      slope=slope, tk=None):
                                    for f in pre:
                                        f()
                                    if first:
                                        grp["acc"] = PS[5 + cnt.get("acc", 0) % 2]
                                        cnt["acc"] = cnt.get("acc", 0) + 1
                                    bq, bqb, bqm = st["bq"], st["bqb"], st["bqm"]
                                    n = kt // 2
                                    bias2 = None
                                    if mixer == 0:
                                        mi = kt - 4 * qc
                                        if qc >= 2 and n <= 2 * qc:
                                            if kt % 2 == 0:
                                                thr, thrb = st[("thr", qc)]
                                                pg, pgb = PS[3 + cnt.get("G", 0) % 2]
                                                cnt["G"] = cnt.get("G", 0) + 1
                                                c.mm(pg[:, :], pgb, [(kmrep3[hs, n, :], qt_[hs, qs], [kmrepb, qb_])])
                                                cm, cmb = nxt(comb_ring, "comb")
                                                c.op(dve, "tensor_tensor", cm, pg[:, :], thr, op=ALU.is_ge, reads=[pgb, thrb], writes=[cmb])
                                                c.op(dve, "tensor_scalar", cm, cm, 1.0, BIG, op0=ALU.subtract, op1=ALU.mult, reads=[cmb], writes=[cmb])
                                                c.op(dve, "tensor_tensor", cm, cm, bq, op=ALU.add, reads=[cmb, bqb], writes=[cmb])
                                                st["comb"] = (cm, cmb)
                                            bias = st["comb"]
                                            if n == 2 * qc:
                                                bias2 = bqm[mi]
                                        else:
                                            bias = bqm[mi] if mi >= 0 else (bq, bqb)
                                    else:
                                        bias = bqm[kt - 4 * qc + 1]
                                    pS, pSb = PS[cnt.get("S", 0) % 3]
                                    cnt["S"] = cnt.get("S", 0) + 1
                                    c.mm(pS[:, :], pSb, [(kt_[hs, ks], qt_[hs, qs], [kb_, qb_])])
                                    tm, tmb = nxt(tmp_ring, "tmp")
                                    c.op(dve, "scalar_tensor_tensor", tm, pS[:, :], 0.125, bias[0], op0=ALU.mult, op1=ALU.add,
                                         reads=[pSb, bias[1]], writes=[tmb])
                                    if bias2 is not None:
                                        c.op(dve, "scalar_tensor_tensor", tm[:, 0:256], pS[:, 0:256], 0.125, bias2[0][:, 0:256], op0=ALU.mult, op1=ALU.add,
                                             reads=[pSb, bias2[1], tmb], writes=[tmb])
                                    Pt, Pb = nxt(P_ring, "P")
                                    c.op(act, "activation", Pt, tm, AF.Exp, bias=float(-slope * (qc * 512 - kt * 128)), scale=1.0,
                                         reads=[tmb], writes=[Pb])
                                    tk["P"] = (Pt, Pb)

                                def B(grp=grp, kt=kt, hh=hh, h=h, hs=hs, qs=qs, first=first, last=last, va4=va4, vb_=vb_, ot_=ot_, ob_=ob_,
                                      hp=hp, ts0=ts0, qc=qc, tk=None):
                                    acc, accb = grp["acc"]
                                    Pt, Pb = tk["P"]
                                    c.mm(acc[:, :], accb, [(va4[:, kt, hh, :], Pt, [vb_, Pb])], start=first, stop=last)
                                    if last:
                                        rc, rcb = nxt(rec_ring, "rec")
                                        if mixer == 1:
                                            c.op(dve, "tensor_scalar", rc[64:128, :], acc[64:128, :], esk[64:128, h:h + 1], None, op0=ALU.add,
                                                 reads=[accb, eskb], writes=[rcb])
                                            c.op(dve, "reciprocal", rc[64:128, :], rc[64:128, :], reads=[rcb], writes=[rcb])
                                        else:
                                            c.op(dve, "reciprocal", rc[64:128, :], acc[64:128, :], reads=[accb], writes=[rcb])
                                        c.op(dve, "tensor_tensor", ot_[hs, qs], acc[0:64, :], rc[64:128, :], op=ALU.mult, reads=[accb, rcb], writes=[ob_])
                                        if hh == 1 and qc == 3:
                                            c.dma(OT[hp * 128:(hp + 1) * 128, ts0:ts0 + SEQ], ot_.bitcast(F32), reads=[ob_], writes=[dOT])
                                tk = {}
                                utasks.append((lambda A=A, tk=tk: A(tk=tk), None, lambda B=B, tk=tk: B(tk=tk)))
                            else:
                                diag = kt >= 4 * qc
                                base = qc * 512 - kt * 128

                                def A1(pre=pre, grp=grp, kt=kt, ks=ks, qs=qs, hs=hs, first=first, last=last, diag=diag, base=base,
                                       qt_=qt_, qb_=qb_, kt_=kt_, kb_=kb_, tk=None):
                                    for f in pre:
                                        f()
                                    if first:
                                        grp["acc"] = PS[5 + cnt.get("acc", 0) % 2]
                                        cnt["acc"] = cnt.get("acc", 0) + 1
                                        grp["racc"] = None
                                    pS, pSb = PS[cnt.get("S", 0) % 3]
                                    cnt["S"] = cnt.get("S", 0) + 1
                                    c.mm(pS[:, :], pSb, [(kt_[hs, ks], qt_[hs, qs], [kb_, qb_])])
                                    et, etb = nxt(e_ring, "e")
                                    c.op(act, "activation", et, pS[:, :], AF.Exp, scale=0.125, reads=[pSb], writes=[etb])
                                    spt, spb = nxt(sp_ring, "sp")
                                    c.op(act, "activation", spt, et, AF.Ln, bias=1.0, scale=1.0, reads=[etb], writes=[spb])
                                    if diag:
                                        c.op(pool, "affine_select", spt, spt, pattern=[[1, 512]], compare_op=ALU.is_gt, fill=0.0,
                                             base=base, channel_multiplier=-1, reads=[spb], writes=[spb])
                                    tk["S"] = (pS, pSb)
                                    tk["sp"] = (spt, spb)
                                    tk["rprev"] = grp["racc"]
                                    if not last:
                                        rn, rnb = nxt(racc_ring, "racc")
                                        if grp["racc"] is None:
                                            c.op(pool, "tensor_copy", rn, spt, reads=[spb], writes=[rnb])
                                        else:
                                            c.op(pool, "tensor_tensor", rn, grp["racc"][0].bitcast(F32), spt.bitcast(F32), op=ALU.add,
                                                 reads=[grp["racc"][1], spb], writes=[rnb])
                                        grp["racc"] = (rn, rnb)

                                def A2(diag=diag, base=base, tk=None):
                                    pS, pSb = tk["S"]
                                    spt, spb = tk["sp"]
                                    pa, pab = PS[3 + cnt.get("pa", 0) % 2]
                                    cnt["pa"] = cnt.get("pa", 0) + 1
                                    parts = [(ustr_r, spt, [spb, b_const])]
                                    if tk["rprev"] is not None:
                                        parts.append((ones_r, tk["rprev"][0], [tk["rprev"][1], b_const]))
                                    c.mm(pa[:, :], pab, parts)
                                    tm, tmb = nxt(tmp_ring, "tmp")
                                    c.op(dve, "scalar_tensor_tensor", tm, pS[:, :], 0.125, spt.bitcast(F32), op0=ALU.mult, op1=ALU.subtract,
                                         reads=[pSb, spb], writes=[tmb])
                                    t3, t3b = nxt(t3_ring, "t3")
                                    c.op(dve, "tensor_tensor", t3, tm, pa[:, :], op=ALU.subtract, reads=[tmb, pab], writes=[t3b])
                                    Pt, Pb = nxt(P_ring, "P")
                                    c.op(act, "activation", Pt, t3, AF.Exp, reads=[t3b], writes=[Pb])
                                    if diag:
                                        c.op(pool, "affine_select", Pt, Pt, pattern=[[1, 512]], compare_op=ALU.is_gt, fill=0.0,
                                             base=base, channel_multiplier=-1, reads=[Pb], writes=[Pb])
                                    tk["P"] = (Pt, Pb)

                                def B(grp=grp, kt=kt, hh=hh, hs=hs, qs=qs, first=first, last=last, va4=va4, vb_=vb_, ot_=ot_, ob_=ob_,
                                      hp=hp, ts0=ts0, qc=qc, tk=None):
                                    acc, accb = grp["acc"]
                                    Pt, Pb = tk["P"]
                                    c.mm(acc[:, :], accb, [(va4[:, kt, hh, :], Pt, [vb_, Pb])], start=first, stop=last)
                                    if last:
                                        c.op(act, "copy", ot_[hs, qs], acc[0:64, :], reads=[accb], writes=[ob_])
                                        if hh == 1 and qc == 3:
                                            c.dma(OT[hp * 128:(hp + 1) * 128, ts0:ts0 + SEQ], ot_.bitcast(F32), reads=[ob_], writes=[dOT])
                                tk = {}
                                utasks.append((lambda A1=A1, tk=tk: A1(tk=tk), lambda A2=A2, tk=tk: A2(tk=tk), lambda B=B, tk=tk: B(tk=tk)))
                tasks.extend(utasks)
            lags = (0, 0, LA) if mixer in (0, 1) else (0, 1, 3)
            for t in range(len(tasks) + lags[-1]):
                for k in range(3):
                    i = t - lags[k]
                    if 0 <= i < len(tasks) and tasks[i][k] is not None:
                        tasks[i][k]()
            if debug and ("OT%d" % L) in dbg:
                c.dma(dbg["OT%d" % L], OT, reads=[dOT], writes=[dIN])

            c.phase_reset(KEEP)
            Wot, Wob = c.alloc(8 * D, MMDT)
            Wo3 = Wot.rearrange("p (c n) -> p c n", n=D)
            for cc in range(8):
                for n0 in range(0, D, 512):
                    c.dma(Wo3[:, cc, n0:n0 + 512], Wo[cc * 128:(cc + 1) * 128, n0:n0 + 512], reads=[dIN], writes=[Wob], q=wq)
            Wr, Wrb = c.alloc(8 * 36)
            Wr3 = Wr.rearrange("p (c n) -> p c n", n=36)
            c.dma(Wr3, w_rt[L].rearrange("(c p) n -> p c n", p=128), reads=[dIN], writes=[Wrb])
            gB, gbuf = c.alloc(D)
            bB, _ = c.alloc(D)
            brt, _ = c.alloc(36)
            c.dma(gB, ln1_g[L:L + 1, :].broadcast_to([128, D]), reads=[dIN], writes=[gbuf])
            c.dma(bB, ln1_b[L:L + 1, :].broadcast_to([128, D]), reads=[dIN], writes=[gbuf])
            c.dma(brt, b_rt[L:L + 1, :].broadcast_to([128, 36]), reads=[dIN], writes=[gbuf])
            c.op(dve, "memset", cnt_b, 0.0, writes=[b_cnt])
            otb_ring = [c.alloc(8 * 128, MMDT) for _ in range(3)]
            xr_ring = [c.alloc(D) for _ in range(4)]
            x1_ring = [c.alloc(D) for _ in range(6)]
            x1T_ring = [c.alloc(8 * 128) for _ in range(2)]
            sc_ring = [{"st": c.alloc(12), "mv": c.alloc(4)} for _ in range(4)]
            rs_ring = [c.alloc(320) for _ in range(4)]
            for (rs, rsb) in rs_ring:
                c.op(dve, "memset", rs[:, 4:8], -BIG, writes=[rsb])

            def ln_a(y, yb, sc):
                st, stb = sc["st"]
                mv, mvb = sc["mv"]
                c.op(dve, "bn_stats", st[:, 0:6], y[:, 0:512], reads=[yb], writes=[stb])
                c.op(dve, "bn_stats", st[:, 6:12], y[:, 512:1024], reads=[yb], writes=[stb])
                c.op(dve, "bn_aggr", mv[:, 0:2], st[:, 0:12], reads=[stb], writes=[mvb])
                c.op(dve, "tensor_scalar", mv[:, 2:3], mv[:, 1:2], EPS, None, op0=ALU.add, reads=[mvb], writes=[mvb])
                c.op(act, "activation", mv[:, 2:3], mv[:, 2:3], AF.Sqrt, reads=[mvb], writes=[mvb])

            def ln_b(y, yb, sc):
                mv, mvb = sc["mv"]
                c.op(dve, "reciprocal", mv[:, 2:3], mv[:, 2:3], reads=[mvb], writes=[mvb])
                c.op(dve, "scalar_tensor_tensor", mv[:, 3:4], mv[:, 0:1], -1.0, mv[:, 2:3], op0=ALU.mult, op1=ALU.mult,
                     reads=[mvb], writes=[mvb])
                c.op(act, "activation", y, y, AF.Identity, bias=mv[:, 3:4], scale=mv[:, 2:3], reads=[yb, mvb], writes=[yb])

            def ln_c(y, yb, gB_, bB_, gbuf_, outt, outb):
                c.op(dve, "tensor_tensor", y, y, gB_, op=ALU.mult, reads=[yb, gbuf_], writes=[yb])
                c.op(pool, "tensor_tensor", outt, y, bB_, op=ALU.add, reads=[yb, gbuf_], writes=[outb])

            def p3_stages(tt):
                r0 = tt * 128
                otb, otbb = otb_ring[tt % 3]
                otb3 = otb.rearrange("p (c t) -> p c t", t=128)
                xr, xrb = xr_ring[tt % 4]
                y, yb = xr, xrb
                x1, x1b = x1_ring[tt % 6]
                x1T, x1Tb = x1T_ring[tt % 2]
                x1T3 = x1T.rearrange("p (c t) -> p c t", t=128)
                sc = sc_ring[tt % 4]
                rs, rsb = rs_ring[tt % 4]
                o = 40
                gm8 = rs[:, o:o + 8]; o += 8
                ngm = rs[:, o:o + 1]; o += 1
                ge = rs[:, o:o + 4]; o += 4
                gsum = rs[:, o:o + 1]; o += 1
                gp = rs[:, o:o + 1]; o += 1
                goh = rs[:, o:o + 4]; o += 4
                ml = rs[:, o:o + 32]; o += 32
                m8 = rs[:, o:o + 8]; o += 8
                sel = rs[:, o:o + 32]; o += 32
                sm = rs[:, o:o + 8]; o += 8
                ex = rs[:, o:o + 32]; o += 32
                wgt = rs[:, o:o + 32]; o += 32
                pos = rs[:, o:o + 32]; o += 32
                oh1 = rs[:, o:o + 32]; o += 32
                assert o <= 320
                R = dict(reads=[rsb], writes=[rsb])
                pl, plb = PS[4 + tt % 2]
                pp, ppb = PS[6 + tt % 2]

                def s0():
                    c.dma(otb3, OT[:, r0:r0 + 128].rearrange("(c p) t -> p c t", p=128), reads=[dOT], writes=[otbb], q=wq)
                    c.dma(xr, X[r0:r0 + 128, :], reads=[dX], writes=[xrb])

                def s1a():
                    for half in range(2):
                        pt, pb = PS[half]
                        c.mm(pt[:, :], pb, [(otb3[:, cc, :], Wo3[:, cc, half * 512:(half + 1) * 512], [otbb, Wob]) for cc in range(8)])
                        c.op(dve, "scalar_tensor_tensor", y[:, half * 512:(half + 1) * 512], xr[:, half * 512:(half + 1) * 512], ALPHA, pt[:, :],
                             op0=ALU.mult, op1=ALU.add, reads=[xrb, pb], writes=[yb])
                    ln_a(y, yb, sc)

                def s1b():
                    ln_b(y, yb, sc)

                def s1c():
                    ln_c(y, yb, gB, bB, gbuf, x1, x1b)
                    c.dma(X1[r0:r0 + 128, :], x1, reads=[x1b], writes=[dX1])

                def s2():
                    for half in range(2):
                        pt, pb = PS[2 + half]
                        for cc in range(4):
                            c4 = half * 4 + cc
                            c.transpose(pt[:, cc * 128:(cc + 1) * 128], pb, x1[:, c4 * 128:(c4 + 1) * 128], [x1b], ident, signal=(cc == 3))
                        evac(x1T[:, half * 512:(half + 1) * 512], x1Tb, pt[:, :], pb)
                    c.mm(pl[:, 0:36], plb, [(x1T3[:, cc, :], Wr3[:, cc, :], [x1Tb, Wrb]) for cc in range(8)])

                def s3():
                    c.op(dve, "tensor_tensor", rs[:, 0:4], pl[:, 0:4], brt[:, 0:4], op=ALU.add, reads=[plb, gbuf], writes=[rsb])
                    c.op(dve, "tensor_tensor", rs[:, 8:40], pl[:, 4:36], brt[:, 4:36], op=ALU.add, reads=[plb, gbuf], writes=[rsb])
                    c.op(dve, "max", gm8, rs[:, 0:8], **R)
                    c.op(dve, "tensor_scalar", ngm, gm8[:, 0:1], -1.0, None, op0=ALU.mult, **R)
                    c.op(dve, "tensor_scalar", goh, rs[:, 0:4], gm8[:, 0:1], None, op0=ALU.is_ge, **R)
                    c.op(dve, "tensor_scalar", goh, goh, 1.0, BIG, op0=ALU.subtract, op1=ALU.mult, **R)
                    c.op(dve, "tensor_tensor", ml.rearrange("p (g e) -> p g e", e=8), rs[:, 8:40].rearrange("p (g e) -> p g e", e=8),
                         goh.unsqueeze(2).broadcast_to([128, 4, 8]), op=ALU.add, **R)
                    c.op(dve, "max", m8, ml, **R)
                    c.op(dve, "tensor_scalar", sel, ml, m8[:, 1:2], None, op0=ALU.is_ge, **R)
                    c.op(dve, "tensor_scalar", oh1, ml, m8[:, 0:1], None, op0=ALU.is_ge, **R)
                    c.op(dve, "tensor_tensor", sm[:, 0:1], m8[:, 1:2], m8[:, 0:1], op=ALU.subtract, **R)
                    c.op(dve, "tensor_scalar", sm[:, 1:2], m8[:, 0:1], -1.0, None, op0=ALU.mult, **R)
                    c.op(act, "activation", ge, rs[:, 0:4], AF.Exp, bias=ngm, scale=1.0, accum_out=gsum, **R)
                    c.op(act, "activation", sm[:, 2:3], sm[:, 0:1], AF.Exp, **R)
                    c.op(act, "activation", ex, ml, AF.Exp, bias=sm[:, 1:2], scale=1.0, **R)
                    c.mm(pp[:, 0:32], ppb, [(tri_f, sel, [b_const, rsb])])
                    c.mm(pp[:, 32:64], ppb, [(ones_f, sel, [b_const, rsb])])

                def s4():
                    c.op(dve, "reciprocal", gp, gsum, **R)
                    c.op(dve, "tensor_scalar", sm[:, 3:4], sm[:, 2:3], 1.0, None, op0=ALU.add, **R)
                    c.op(dve, "reciprocal", sm[:, 3:4], sm[:, 3:4], **R)
                    c.op(dve, "tensor_tensor", sm[:, 4:5], sm[:, 3:4], gp, op=ALU.mult, **R)
                    c.op(dve, "scalar_tensor_tensor", wgt, ex, sm[:, 4:5], sel, op0=ALU.mult, op1=ALU.mult, **R)
                    c.op(dve, "tensor_tensor", pos, pp[:, 0:32], cnt_b, op=ALU.add, reads=[ppb, b_cnt, rsb], writes=[rsb])
                    c.op(dve, "tensor_tensor", cnt_b, cnt_b, pp[:, 32:64], op=ALU.add, reads=[ppb, b_cnt], writes=[b_cnt])
                    c.op(dve, "scalar_tensor_tensor", pos, pos, float(CAP - 1), eoff, op0=ALU.min, op1=ALU.add, reads=[rsb, b_const], writes=[rsb])
                    c.op(dve, "tensor_tensor", sel, sel, oh1, op=ALU.subtract, **R)
                    c.op(dve, "tensor_tensor", ex, oh1, pos, op=ALU.mult, **R)
                    c.op(dve, "tensor_reduce", rt3[:, tt, 0:1], ex, axis=AX.X, op=ALU.add, reads=[rsb], writes=[b_rtab])
                    c.op(dve, "tensor_tensor", ex, sel, pos, op=ALU.mult, **R)
                    c.op(dve, "tensor_reduce", rt3[:, tt, 1:2], ex, axis=AX.X, op=ALU.add, reads=[rsb], writes=[b_rtab])
                    c.op(dve, "tensor_tensor", ex, oh1, wgt, op=ALU.mult, **R)
                    c.op(dve, "tensor_reduce", rt3[:, tt, 2:3], ex, axis=AX.X, op=ALU.add, reads=[rsb], writes=[b_rtab])
                    c.op(dve, "tensor_tensor", ex, sel, wgt, op=ALU.mult, **R)
                    c.op(dve, "tensor_reduce", rt3[:, tt, 3:4], ex, axis=AX.X, op=ALU.add, reads=[rsb], writes=[b_rtab])
                    c.op(dve, "tensor_copy", ridx3[:, tt, :], rt3[:, tt, 0:2], reads=[b_rtab], writes=[b_ridx])

                def s5():
                    for k in range(2):
                        c.dma(XS[:, :], x1, reads=[x1b, b_ridx], writes=[dXS], q=pool,
                              indirect=dict(out_offset=bass.IndirectOffsetOnAxis(ap=ridx3[:, tt, k:k + 1], axis=0), in_offset=None,
                                            bounds_check=bc_reg, oob_is_err=False))
                return (s0, s1a, s1b, s1c, s2, s3, s4, s5)

            p3t = [p3_stages(tt) for tt in range(T // 128)]
            nst = 8
            for t in range(len(p3t) + nst - 1):
                for k in range(nst - 1, -1, -1):
                    i = t - k
                    if 0 <= i < len(p3t):
                        p3t[i][k]()
            if debug and ("X1_%d" % L) in dbg:
                c.dma(dbg["X1_%d" % L], X1, reads=[dX1], writes=[dIN])
            if debug and ("rt%d" % L) in dbg:
                c.dma(dbg["rt%d" % L], rt, reads=[b_rtab], writes=[dIN])

            c.phase_reset(KEEP)
            w_ring = []
            for _ in range(2):
                w_ring.append((c.alloc(8 * DEXP, MMDT), c.alloc(8 * DEXP, MMDT), c.alloc(4 * D, MMDT)))
            xs_ring = [c.alloc(3 * D) for _ in range(2)]
            xsT_ring = [c.alloc(8 * CAP, MMDT) for _ in range(2)]
            hT_ring = [c.alloc(4 * CAP, MMDT) for _ in range(2)]
            sg_ring = [c.alloc(CAP) for _ in range(2)]
            ys_ring = [c.alloc(D) for _ in range(3)]
            NJ = CAP // 128
            ysi = 0
            sgi = 0

            def load_w(e):
                (wg, wgb), (wu, wub), (wd, wdb) = w_ring[e % 2]
                wg3 = wg.rearrange("p (c n) -> p c n", n=DEXP)
                wu3 = wu.rearrange("p (c n) -> p c n", n=DEXP)
                wd3 = wd.rearrange("p (c n) -> p c n", n=D)
                for cc in range(8):
                    c.dma(wg3[:, cc, :], w_e_gate[L, e, cc * 128:(cc + 1) * 128, :], reads=[dIN], writes=[wgb], q=wq)
                    c.dma(wu3[:, cc, :], w_e_up[L, e, cc * 128:(cc + 1) * 128, :], reads=[dIN], writes=[wub], q=wq)
                for cc in range(4):
                    for n0 in range(0, D, 512):
                        c.dma(wd3[:, cc, n0:n0 + 512], w_e_down[L, e, cc * 128:(cc + 1) * 128, n0:n0 + 512], reads=[dIN], writes=[wdb], q=wq)

            def load_xs(e):
                xs, xsb = xs_ring[e % 2]
                c.dma(xs.rearrange("p (j d) -> p j d", d=D), XS[e * CAP:(e + 1) * CAP, :].rearrange("(j p) d -> p j d", p=128),
                      reads=[dXS], writes=[xsb])

            load_w(0)
            load_xs(0)
            for e in range(NEXP):
                if e + 1 < NEXP:
                    load_w(e + 1)
                    load_xs(e + 1)
                (wg, wgb), (wu, wub), (wd, wdb) = w_ring[e % 2]
                wg3 = wg.rearrange("p (c n) -> p c n", n=DEXP)
                wu3 = wu.rearrange("p (c n) -> p c n", n=DEXP)
                wd3 = wd.rearrange("p (c n) -> p c n", n=D)
                xs, xsb = xs_ring[e % 2]
                xs3 = xs.rearrange("p (j d) -> p j d", d=D)
                xsT, xsTb = xsT_ring[e % 2]
                xsT3 = xsT.rearrange("p (c t) -> p c t", t=CAP)
                hT, hTb = hT_ring[e % 2]
                hT3 = hT.rearrange("p (c t) -> p c t", t=CAP)
                for cc in range(8):
                    pt, pb = PS[cc % 2]
                    for jj in range(NJ):
                        c.transpose(pt[:, jj * 128:(jj + 1) * 128], pb, xs3[:, jj, cc * 128:(cc + 1) * 128], [xsb], ident, signal=(jj == NJ - 1))
                    evac(xsT3[:, cc, :], xsTb, pt[:, 0:CAP], pb)
                for fc in range(4):
                    pg, pgb = PS[2 + fc % 2]
                    pu, pub = PS[4 + fc % 2]
                    c.mm(pg[:, 0:CAP], pgb, [(wg3[:, cc, fc * 128:(fc + 1) * 128], xsT3[:, cc, :], [wgb, xsTb]) for cc in range(8)])
                    c.mm(pu[:, 0:CAP], pub, [(wu3[:, cc, fc * 128:(fc + 1) * 128], xsT3[:, cc, :], [wub, xsTb]) for cc in range(8)])
                    sg, sgb = sg_ring[sgi % 2]; sgi += 1
                    c.op(act, "activation", sg, pg[:, 0:CAP], AF.Silu, reads=[pgb], writes=[sgb])
                    c.op(dve, "tensor_tensor", hT3[:, fc, :], sg, pu[:, 0:CAP], op=ALU.mult, reads=[sgb, pub], writes=[hTb])
                for jj in range(NJ):
                    ys, ysb = ys_ring[ysi % 3]; ysi += 1
                    for half in range(2):
                        pt, pb = PS[6 + half]
                        c.mm(pt[:, :], pb, [(hT3[:, fc, jj * 128:(jj + 1) * 128], wd3[:, fc, half * 512:(half + 1) * 512], [hTb, wdb]) for fc in range(4)])
                        evac(ys[:, half * 512:(half + 1) * 512], ysb, pt[:, :], pb)
                    r0 = e * CAP + jj * 128
                    c.dma(YS[r0:r0 + 128, :], ys, reads=[ysb], writes=[dYS])

            c.phase_reset(KEEP)
            Wg_, Wgb_ = c.alloc(8 * D, MMDT)
            Wg3 = Wg_.rearrange("p (c n) -> p c n", n=D)
            for cc in range(8):
                for n0 in range(0, D, 512):
                    c.dma(Wg3[:, cc, n0:n0 + 512], w_ple_gate[L, cc * 128:(cc + 1) * 128, n0:n0 + 512], reads=[dIN], writes=[Wgb_], q=wq)
            Wp_, Wpb_ = c.alloc(2 * D, MMDT)
            Wp3 = Wp_.rearrange("p (c n) -> p c n", n=D)
            for cc in range(2):
                for n0 in range(0, D, 512):
                    c.dma(Wp3[:, cc, n0:n0 + 512], w_ple_proj[L, cc * 128:(cc + 1) * 128, n0:n0 + 512], reads=[dIN], writes=[Wpb_], q=wq)
            gB, gbuf = c.alloc(D)
            bB, _ = c.alloc(D)
            c.dma(gB, ln2_g[L:L + 1, :].broadcast_to([128, D]), reads=[dIN], writes=[gbuf])
            c.dma(bB, ln2_b[L:L + 1, :].broadcast_to([128, D]), reads=[dIN], writes=[gbuf])
            y1_ring = [c.alloc(D) for _ in range(4)]
            y2_ring = [c.alloc(D) for _ in range(2)]
            x1_ring = [c.alloc(D) for _ in range(2)]
            pr_ring = [c.alloc(PLE) for _ in range(6)]
            x2_ring = [c.alloc(D) for _ in range(3)]
            x2T_ring = [c.alloc(8 * 128, MMDT) for _ in range(2)]
            pT_ring = [c.alloc(2 * 128, MMDT) for _ in range(2)]
            sig_ring = [c.alloc(D) for _ in range(2)]
            sc_ring = [{"st": c.alloc(12), "mv": c.alloc(4)} for _ in range(4)]
            dst = out if L == DEPTH - 1 else X
            dstb = dIN if L == DEPTH - 1 else dX

            def p5_stages(tt):
                r0 = tt * 128
                y1, y1b = y1_ring[tt % 4]; y2, y2b = y2_ring[tt % 2]; x1, x1b = x1_ring[tt % 2]; pr, prb = pr_ring[tt % 6]
                y, yb = y1, y1b
                x2, x2b = x2_ring[tt % 3]; x2T, x2Tb = x2T_ring[tt % 2]; pT, pTb = pT_ring[tt % 2]
                sig, sigb = sig_ring[tt % 2]; sc = sc_ring[tt % 4]
                x2T3 = x2T.rearrange("p (c t) -> p c t", t=128)
                pT3 = pT.rearrange("p (c t) -> p c t", t=128)

                def s0():
                    for k, (yy, yyb) in enumerate(((y1, y1b), (y2, y2b))):
                        c.dma(yy, YS[:, :], reads=[dYS, b_ridx], writes=[yyb], q=pool,
                              indirect=dict(out_offset=None, in_offset=bass.IndirectOffsetOnAxis(ap=ridx3[:, tt, k:k + 1], axis=0),
                                            bounds_check=bc_reg, oob_is_err=False))
                    c.dma(x1, X1[r0:r0 + 128, :], reads=[dX1], writes=[x1b])
                    c.dma(pr, p_in[L, r0:r0 + 128, :], reads=[dIN], writes=[prb])

                def s1a():
                    c.op(dve, "tensor_scalar", y2, y2, rt3[:, tt, 3:4], None, op0=ALU.mult, reads=[y2b, b_rtab], writes=[y2b])
                    c.op(dve, "scalar_tensor_tensor", y1, y1, rt3[:, tt, 2:3], y2, op0=ALU.mult, op1=ALU.add, reads=[y1b, y2b, b_rtab], writes=[y1b])
                    c.op(dve, "scalar_tensor_tensor", y, x1, ALPHA, y1, op0=ALU.mult, op1=ALU.add, reads=[x1b, y1b], writes=[yb])
                    ln_a(y, yb, sc)

                def s1b():
                    ln_b(y, yb, sc)

                def s1c():
                    ln_c(y, yb, gB, bB, gbuf, x2, x2b)

                def s2():
                    for half in range(2):
                        pt, pb = PS[half]
                        for cc in range(4):
                            c4 = half * 4 + cc
                            c.transpose(pt[:, cc * 128:(cc + 1) * 128], pb, x2[:, c4 * 128:(c4 + 1) * 128], [x2b], ident, signal=(cc == 3))
                        evac(x2T[:, half * 512:(half + 1) * 512], x2Tb, pt[:, :], pb)
                    pt, pb = PS[2]
                    for cc in range(2):
                        c.transpose(pt[:, cc * 128:(cc + 1) * 128], pb, pr[:, cc * 128:(cc + 1) * 128], [prb], ident, signal=(cc == 1))
                    evac(pT, pTb, pt[:, 0:256], pb)

                def s3a():
                    for half in range(2):
                        hsl = slice(half * 512, (half + 1) * 512)
                        pg, pgb = PS[4 + half]
                        c.mm(pg[:, :], pgb, [(x2T3[:, cc, :], Wg3[:, cc, hsl], [x2Tb, Wgb_]) for cc in range(8)])
                        c.op(act, "activation", sig[:, hsl], pg[:, :], AF.Sigmoid, reads=[pgb], writes=[sigb])
                        pq, pqb = PS[6 + half]
                        c.mm(pq[:, :], pqb, [(pT3[:, cc, :], Wp3[:, cc, hsl], [pTb, Wpb_]) for cc in range(2)])

                def s3b():
                    for half in range(2):
                        hsl = slice(half * 512, (half + 1) * 512)
                        pq, pqb = PS[6 + half]
                        c.op(dve, "tensor_tensor", sig[:, hsl], sig[:, hsl], pq[:, :], op=ALU.mult, reads=[sigb, pqb], writes=[sigb])
                    c.op(pool, "tensor_tensor", sig, sig, x2, op=ALU.add, reads=[sigb, x2b], writes=[sigb])
                    c.dma(dst[r0:r0 + 128, :], sig, reads=[sigb], writes=[dstb])
                return (s0, s1a, s1b, s1c, s2, s3a, s3b)

            p5t = [p5_stages(tt) for tt in range(T // 128)]
            nst = 7
            for t in range(len(p5t) + nst - 1):
                for k in range(nst - 1, -1, -1):
                    i = t - k
                    if 0 <= i < len(p5t):
                        p5t[i][k]()
            if debug and ("X3_%d" % L) in dbg:
                c.barrier()
                c.dma(dbg["X3_%d" % L], dst, reads=[dstb], writes=[dIN])

        if nlayers < DEPTH:
            c.barrier()
            c.dma(out, X, reads=[dX], writes=[dIN])
        c.barrier()
    return nc


def _consts():
    cst = np.zeros((128, 6, 128), np.float32)
    k = np.arange(128)[:, None]
    m = np.arange(128)[None, :]
    cst[:, 0, :] = (k == m)
    cst[:, 1, :] = 1.0
    cst[:, 2, :] = (k < m)
    cst[:, 3, :] = (k > m)
    q = np.arange(512)[None, :]
    kk = np.arange(128)[:, None]
    cmask = np.zeros((9, 128, 512), np.float32)
    for i in range(4):
        cmask[i] = np.where((-128 * i + q - kk) >= 0, 0.0, -BIG)
    for i in range(5):
        dlt = 128 - 128 * i
        v = dlt + q - kk
        cmask[4 + i] = np.where((v >= 0) & (v < 128), 0.0, -BIG)
    cbase = (-(q - kk)).astype(np.float32)
    ceoff = np.tile((np.arange(NEXP) * CAP).astype(np.float32)[None, :], (128, 1))
    return cst, cmask, np.ascontiguousarray(cbase), np.ascontiguousarray(ceoff)


_NC_CACHE = {}


def kernel(**inputs):
    f = lambda a: np.ascontiguousarray(np.asarray(a, dtype=np.float32))
    x = f(inputs["x"]).reshape(NCORES, T, D)
    p = f(inputs["p"]).reshape(DEPTH, NCORES, T, PLE)
    cst, cmask, cbase, ceoff = _consts()
    w_rt = np.ascontiguousarray(np.concatenate([f(inputs["w_grp"]), f(inputs["w_exp"])], axis=2))
    b_rt = np.ascontiguousarray(np.concatenate([f(inputs["b_grp"]), f(inputs["b_exp"])], axis=1))
    shared = {k: f(inputs[k]) for k in ("w_qkv_a", "w_o_a", "w_qkv_b", "w_o_b", "sinks_b", "w_qkv_c", "w_o_c", "ln1_g", "ln1_b",
                                         "ln2_g", "ln2_b", "w_e_gate", "w_e_up", "w_e_down", "w_ple_gate", "w_ple_proj")}
    shared.update(w_rt=w_rt, b_rt=b_rt, cst=cst, cmask=cmask, cbase=cbase, ceoff=ceoff)
    if "nc" not in _NC_CACHE:
        _NC_CACHE["nc"] = build()
    nc = _NC_CACHE["nc"]
    in_maps = []
    for i in range(NCORES):
        m = dict(shared)
        m["x"] = x[i]
        m["p"] = np.ascontiguousarray(p[:, i])
        in_maps.append(m)
    res = run_bass_kernel_spmd(nc, in_maps, core_ids=list(range(NCORES)))
    o = np.stack([np.asarray(r["out"]) for r in res.results], axis=0)
    return o.reshape(16, SEQ, D).astype(np.float32)
```

```python
import numpy as np
import concourse.bass as bass
import concourse.mybir as mybir
from concourse.bass_utils import run_bass_kernel_spmd

F32 = mybir.dt.float32
F32R = mybir.dt.float32r
I32 = mybir.dt.int32
ALU = mybir.AluOpType
AF = mybir.ActivationFunctionType
AX = mybir.AxisListType

USE_F32R = True
MMDT = F32R if USE_F32R else F32

NCORES = 8
D = 1024
SEQ = 2048
NSEQ = 2
T = NSEQ * SEQ
DEPTH = 4
NH = 16
HD = 64
NEXP = 32
DEXP = 512
CAP = 384
PLE = 256
ALPHA = (2.0 * DEPTH) ** 0.25
EPS = 1e-5
BIG = 30000.0
SLOPES = [2.0 ** (-8.0 * (h + 1) / NH) for h in range(NH)]
XS_ROWS = NEXP * CAP + 128


class Eng:
    def __init__(self, name, h, sem):
        self.name, self.h, self.sem = name, h, sem
        self.count = 0
        self.known = {}


class Buf:
    __slots__ = ("name", "writers", "readers")

    def __init__(self, name):
        self.name = name
        self.writers = {}
        self.readers = {}


class Ctx:
    def __init__(self, nc, es):
        self.nc = nc
        self.es = es
        mk = lambda n, h: Eng(n, h, es.enter_context(nc.semaphore("s_" + n)))
        self.pe = mk("pe", nc.tensor)
        self.act = mk("act", nc.scalar)
        self.dve = mk("dve", nc.vector)
        self.pool = mk("pool", nc.gpsimd)
        self.sp = mk("sp", nc.sync)
        self.engs = [self.pe, self.act, self.dve, self.pool, self.sp]
        self.sems = {e.name: e.sem for e in self.engs}
        self.ndma = 40
        self.dsem = []
        for i in range(self.ndma):
            s = es.enter_context(nc.semaphore("s_dma%d" % i))
            self.dsem.append(s)
            self.sems["d%d" % i] = s
        self.dval = [0] * self.ndma
        self.dnext = 0
        self.FW, self.RW = 19100, 34048
        self.arena = es.enter_context(nc.sbuf_tensor("arena_f", [128, self.FW], F32))
        self.arena_r = es.enter_context(nc.sbuf_tensor("arena_r", [128, self.RW], MMDT))
        self.atop = 0
        self.rtop = 0
        self.psum = []
        for i in range(8):
            t = es.enter_context(nc.psum_tensor("psb%d" % i, [128, 512], F32))
            self.psum.append((t, Buf("ps%d" % i)))
        self.nbuf = 0

    def alloc(self, ncols, dt=F32, name=None):
        if dt == MMDT and USE_F32R:
            a = self.arena_r[:, self.rtop:self.rtop + ncols]
            self.rtop += ncols
            assert self.rtop <= self.RW, "SBUF R arena overflow %d" % self.rtop
        else:
            a = self.arena[:, self.atop:self.atop + ncols]
            self.atop += ncols
            assert self.atop <= self.FW, "SBUF F arena overflow %d" % self.atop
            if dt != F32:
                a = a.bitcast(dt)
        self.nbuf += 1
        return a, Buf(name or ("b%d" % self.nbuf))

    def wait(self, E, key, val):
        if E.known.get(key, 0) >= val:
            return
        E.h.wait_ge(self.sems[key], val)
        E.known[key] = val

    def _deps(self, E, reads, writes):
        evs = {}
        for b in reads:
            for k, v in b.writers.items():
                evs[k] = max(evs.get(k, 0), v)
        for b in writes:
            for k, v in b.readers.items():
                if k != E.name:
                    evs[k] = max(evs.get(k, 0), v)
            for k, v in b.writers.items():
                if k != E.name:
                    evs[k] = max(evs.get(k, 0), v)
        for k, v in evs.items():
            self.wait(E, k, v)

    def _record(self, key, val, reads, writes):
        for b in reads:
            b.readers[key] = max(b.readers.get(key, 0), val)
        for b in writes:
            if b.readers:
                b.readers = {}
                b.writers = {key: val}
            else:
                b.writers[key] = max(b.writers.get(key, 0), val)

    def op(self, E, fname, *args, reads=(), writes=(), signal=True, **kw):
        self._deps(E, reads, writes)
        ins = getattr(E.h, fname)(*args, **kw)
        if signal:
            E.count += 1
            ins.then_inc(E.sem, 1)
            self._record(E.name, E.count, reads, writes)
        else:
            self._record(E.name, E.count + 1, reads, writes)
        return ins

    def dma(self, out, in_, reads=(), writes=(), q=None, indirect=None, **kw):
        Q = q or self.sp
        self._deps(Q, reads, writes)
        i = self.dnext
        self.dnext = (self.dnext + 1) % self.ndma
        key = "d%d" % i
        self.wait(Q, key, self.dval[i])
        if indirect is None:
            ins = Q.h.dma_start(out=out, in_=in_, **kw)
        else:
            ins = Q.h.indirect_dma_start(out=out, in_=in_, **indirect)
        self.dval[i] += 16
        ins.then_inc(self.dsem[i], 16)
        self._record(key, self.dval[i], reads, writes)

    def barrier(self):
        for E in self.engs:
            for F in self.engs:
                if F is not E and F.count > 0:
                    self.wait(E, F.name, F.count)
            for i in range(self.ndma):
                if self.dval[i] > 0:
                    self.wait(E, "d%d" % i, self.dval[i])

    def phase_reset(self, keep):
        self.barrier()
        self.atop, self.rtop = keep

    def mm(self, out, outbuf, parts, start=True, stop=True):
        n = len(parts)
        for i, (l, r, rb) in enumerate(parts):
            last = (i == n - 1)
            self.op(self.pe, "matmul", out, l, r, start=(start and i == 0), stop=(stop and last),
                    reads=rb, writes=[outbuf], signal=last)

    def transpose(self, out, outbuf, in_, inbufs, ident, signal=True):
        self.op(self.pe, "transpose", out, in_, ident, reads=inbufs, writes=[outbuf], signal=signal)


def build(nlayers=DEPTH, debug=None):
    from contextlib import ExitStack
    nc = bass.Bass("TRN2", target_bir_lowering=False)
    dr = lambda n, s, kind="ExternalInput", dt=F32: nc.dram_tensor(n, list(s), dt, kind=kind).ap()
    x_in = dr("x", [T, D])
    p_in = dr("p", [DEPTH, T, PLE])
    w_qkv_a = dr("w_qkv_a", [2, D, 3 * D]); w_o_a = dr("w_o_a", [2, D, D])
    w_qkv_b = dr("w_qkv_b", [1, D, D + 512]); w_o_b = dr("w_o_b", [1, D, D])
    sinks_b = dr("sinks_b", [1, NH])
    w_qkv_c = dr("w_qkv_c", [1, D, 3 * D]); w_o_c = dr("w_o_c", [1, D, D])
    ln1_g = dr("ln1_g", [DEPTH, D]); ln1_b = dr("ln1_b", [DEPTH, D])
    ln2_g = dr("ln2_g", [DEPTH, D]); ln2_b = dr("ln2_b", [DEPTH, D])
    w_rt = dr("w_rt", [DEPTH, D, 36]); b_rt = dr("b_rt", [DEPTH, 36])
    w_e_gate = dr("w_e_gate", [DEPTH, NEXP, D, DEXP]); w_e_up = dr("w_e_up", [DEPTH, NEXP, D, DEXP])
    w_e_down = dr("w_e_down", [DEPTH, NEXP, DEXP, D])
    w_ple_gate = dr("w_ple_gate", [DEPTH, D, D]); w_ple_proj = dr("w_ple_proj", [DEPTH, PLE, D])
    cst = dr("cst", [128, 6, 128])
    cmask = dr("cmask", [9, 128, 512])
    cbase = dr("cbase", [128, 512])
    ceoff = dr("ceoff", [128, NEXP])
    out = dr("out", [T, D], kind="ExternalOutput")
    X = dr("Xs", [T, D], kind="Internal"); X1 = dr("X1s", [T, D], kind="Internal")
    QT = dr("QTs", [D, T], kind="Internal"); KT = dr("KTs", [D, T], kind="Internal")
    V = dr("Vs", [T, D], kind="Internal"); OT = dr("OTs", [D, T], kind="Internal")
    XS = dr("XSs", [XS_ROWS, D], kind="Internal"); YS = dr("YSs", [XS_ROWS, D], kind="Internal")
    dbg = {}
    if debug:
        for nme, shp in debug.items():
            dbg[nme] = dr("dbg_" + nme, shp, kind="ExternalOutput")
    dX, dX1, dQT, dKT, dV, dOT, dXS, dYS = [Buf(n) for n in ("X", "X1", "QT", "KT", "V", "OT", "XS", "YS")]
    dIN = Buf("in")

    with ExitStack() as es:
        c = Ctx(nc, es)
        pe, act, dve, pool, sp = c.pe, c.act, c.dve, c.pool, c.sp
        wq = pool if USE_F32R else sp
        PS = c.psum
        bc_reg = nc.gpsimd.to_reg(XS_ROWS - 1)

        ident, b_const = c.alloc(128)
        ones_r, _ = c.alloc(128, MMDT)
        tri_f, _ = c.alloc(128)
        ustr_r, _ = c.alloc(128, MMDT)
        ones_f, _ = c.alloc(128)
        cb_f, _ = c.alloc(512)
        eoff, _ = c.alloc(NEXP)
        rt, b_rtab = c.alloc(32 * 4)
        ridx, b_ridx = c.alloc(32 * 2, I32)
        cnt_b, b_cnt = c.alloc(NEXP)
        c.dma(ident, cst[:, 0, :], writes=[b_const])
        c.dma(ones_f, cst[:, 1, :], writes=[b_const])
        c.dma(tri_f, cst[:, 2, :], writes=[b_const])
        c.dma(ones_r, cst[:, 1, :], writes=[b_const], q=wq)
        c.dma(ustr_r, cst[:, 3, :], writes=[b_const], q=wq)
        c.dma(cb_f, cbase, writes=[b_const])
        c.dma(eoff, ceoff, writes=[b_const])
        KEEP = (c.atop, c.rtop)
        rt3 = rt.rearrange("p (t k) -> p t k", k=4)
        ridx3 = ridx.rearrange("p (t k) -> p t k", k=2)

        for i in range(8):
            c.dma(X[i * 512:(i + 1) * 512, :], x_in[i * 512:(i + 1) * 512, :], reads=[dIN], writes=[dX])

        state = {"ev": 0}

        def evac(out_ap, outb, in_ap, inb, extra_reads=()):
            state["ev"] ^= 1
            if state["ev"]:
                c.op(act, "copy", out_ap, in_ap, reads=[inb, *extra_reads], writes=[outb])
            else:
                c.op(dve, "tensor_copy", out_ap, in_ap, reads=[inb, *extra_reads], writes=[outb])

        def layer_norm(y, yb, gB, bB, gbuf, outt, outb, sc):
            st, stb = sc["st"]
            mv, mvb = sc["mv"]
            c.op(dve, "bn_stats", st[:, 0:6], y[:, 0:512], reads=[yb], writes=[stb])
            c.op(dve, "bn_stats", st[:, 6:12], y[:, 512:1024], reads=[yb], writes=[stb])
            c.op(dve, "bn_aggr", mv[:, 0:2], st[:, 0:12], reads=[stb], writes=[mvb])
            c.op(dve, "tensor_scalar", mv[:, 2:3], mv[:, 1:2], EPS, None, op0=ALU.add, reads=[mvb], writes=[mvb])
            c.op(act, "activation", mv[:, 2:3], mv[:, 2:3], AF.Sqrt, reads=[mvb], writes=[mvb])
            c.op(dve, "reciprocal", mv[:, 2:3], mv[:, 2:3], reads=[mvb], writes=[mvb])
            c.op(dve, "scalar_tensor_tensor", mv[:, 3:4], mv[:, 0:1], -1.0, mv[:, 2:3], op0=ALU.mult, op1=ALU.mult,
                 reads=[mvb], writes=[mvb])
            c.op(act, "activation", y, y, AF.Identity, bias=mv[:, 3:4], scale=mv[:, 2:3], reads=[yb, mvb], writes=[yb])
            c.op(dve, "tensor_tensor", y, y, gB, op=ALU.mult, reads=[yb, gbuf], writes=[yb])
            c.op(pool, "tensor_tensor", outt, y, bB, op=ALU.add, reads=[yb, gbuf], writes=[outb])

        for L in range(nlayers):
            mixer, j = L % 3, L // 3
            if mixer == 0:
                Wqkv, Wo, NQ, NKV = w_qkv_a[j], w_o_a[j], 1024, 1024
            elif mixer == 1:
                Wqkv, Wo, NQ, NKV = w_qkv_b[j], w_o_b[j], 1024, 256
            else:
                Wqkv, Wo, NQ, NKV = w_qkv_c[j], w_o_c[j], 1024, 1024
            NQKV = NQ + 2 * NKV

            c.phase_reset(KEEP)
            Wt, Wb = c.alloc(8 * NQKV, MMDT)
            W3 = Wt.rearrange("p (c n) -> p c n", n=NQKV)
            Wq_pc = Wqkv.rearrange("(p c) n -> p c n", c=8)
            for cc in range(8):
                for n0 in range(0, NQKV, 1024):
                    n1 = min(NQKV, n0 + 1024)
                    c.dma(W3[:, cc, n0:n1], Wq_pc[:, cc, n0:n1], reads=[dIN], writes=[Wb], q=wq)
            xr_ring = [c.alloc(4 * 1024) for _ in range(2)]
            xT_ring = [c.alloc(8 * 512, MMDT) for _ in range(2)]
            stg_ring = [c.alloc(512) for _ in range(4)]
            sti = 0
            pso = 0
            for tb in range(T // 512):
                xr, xrb = xr_ring[tb % 2]
                xT, xTb = xT_ring[tb % 2]
                xr3 = xr.rearrange("p (j d) -> p j d", d=1024)
                xr4 = xr.rearrange("p (j q c) -> p j c q", q=128, c=8)
                xT3 = xT.rearrange("p (c t) -> p c t", t=512)
                c.dma(xr3, X[tb * 512:(tb + 1) * 512, :].rearrange("(j p) d -> p j d", p=128), reads=[dX], writes=[xrb])
                for cc in range(8):
                    pt, pb = PS[cc % 2]
                    for jj in range(4):
                        c.transpose(pt[:, jj * 128:(jj + 1) * 128], pb, xr4[:, jj, cc, :], [xrb], ident,
                                    signal=(jj == 3))
                    evac(xT3[:, cc, :], xTb, pt[:, :], pb)
                for fc in range((NQ + NKV) // 128):
                    pt, pb = PS[2 + pso % 4]; pso += 1
                    c.mm(pt[:, :], pb, [(W3[:, cc, fc * 128:(fc + 1) * 128], xT3[:, cc, :], [Wb, xTb]) for cc in range(8)])
                    sg, sgb = stg_ring[sti % 4]; sti += 1
                    evac(sg, sgb, pt[:, :], pb)
                    if fc < NQ // 128:
                        c.dma(QT[fc * 128:(fc + 1) * 128, tb * 512:(tb + 1) * 512], sg, reads=[sgb], writes=[dQT])
                    else:
                        f2 = fc - NQ // 128
                        c.dma(KT[f2 * 128:(f2 + 1) * 128, tb * 512:(tb + 1) * 512], sg, reads=[sgb], writes=[dKT])
                for jj in range(4):
                    for v0 in range(0, NKV, 512):
                        nv = min(512, NKV - v0)
                        pt, pb = PS[2 + pso % 4]; pso += 1
                        c.mm(pt[:, 0:nv], pb, [(xT3[:, cc, jj * 128:(jj + 1) * 128], W3[:, cc, NQ + NKV + v0:NQ + NKV + v0 + nv], [Wb, xTb])
                                               for cc in range(8)])
                        sg, sgb = stg_ring[sti % 4]; sti += 1
                        evac(sg[:, 0:nv], sgb, pt[:, 0:nv], pb)
                        r0 = tb * 512 + jj * 128
                        c.dma(V[r0:r0 + 128, v0:v0 + nv], sg[:, 0:nv], reads=[sgb], writes=[dV])
            if debug and ("QT%d" % L) in dbg:
                c.dma(dbg["QT%d" % L], QT, reads=[dQT], writes=[dIN])
                c.dma(dbg["KT%d" % L], KT, reads=[dKT], writes=[dIN])
                c.dma(dbg["V%d" % L], V, reads=[dV], writes=[dIN])

            c.phase_reset(KEEP)
            LA = 3
            ld_ring = []
            for _ in range(2):
                qt_, qb_ = c.alloc(SEQ, MMDT)
                kt_, kb_ = c.alloc(SEQ, MMDT)
                va_, vb_ = c.alloc(16 * 2 * 128, MMDT)
                ld_ring.append((qt_, qb_, kt_, kb_, va_, vb_))
                c.op(dve, "tensor_copy", va_.rearrange("p (a b) -> p a b", b=128), ones_f.unsqueeze(1).broadcast_to([128, 32, 128]),
                     reads=[b_const], writes=[vb_])
            ot_ring = [c.alloc(SEQ, MMDT) for _ in range(2)]
            tmp_ring = [c.alloc(512) for _ in range(3)]
            P_ring = [c.alloc(512, MMDT) for _ in range(LA + 3)]
            rec_ring = [c.alloc(512) for _ in range(2)]
            cnt = {}

            def nxt(ring, key):
                i = cnt.get(key, 0)
                cnt[key] = i + 1
                return ring[i % len(ring)]

            if mixer in (0, 1):
                bq_ring = [c.alloc(512) for _ in range(2)]
                nm = 4 if mixer == 0 else 5
                masks = [c.alloc(512) for _ in range(nm)]
                for i, (m, mb) in enumerate(masks):
                    c.dma(m, cmask[i if mixer == 0 else 4 + i], writes=[mb])
                bqm_ring = [[c.alloc(512) for _ in range(nm)] for _ in range(2)]
            if mixer == 0:
                comb_ring = [c.alloc(512) for _ in range(3)]
                bqB_ring = [c.alloc(512) for _ in range(2)]
                km, kmb = c.alloc(8)
                kmr, kmrb = c.alloc(8, MMDT)
                kmrep, kmrepb = c.alloc(8 * 128, MMDT)
                kmrep3 = kmrep.rearrange("p (n m) -> p n m", m=128)
                gsb_ring = [c.alloc(8) for _ in range(4)]
                top_ring = [c.alloc(12) for _ in range(4)]
                diag_ring = [c.alloc(128, MMDT) for _ in range(8)]
                thr_ring = [c.alloc(512) for _ in range(2)]
            elif mixer == 1:
                esk, eskb = c.alloc(NH)
                c.dma(esk, sinks_b[0:1, :].broadcast_to([128, NH]), reads=[dIN], writes=[eskb])
                c.op(act, "activation", esk, esk, AF.Exp, reads=[eskb], writes=[eskb])
            else:
                sp_ring = [c.alloc(512, MMDT) for _ in range(4)]
                e_ring = [c.alloc(512) for _ in range(2)]
                racc_ring = [c.alloc(512, MMDT) for _ in range(5)]
                t3_ring = [c.alloc(512) for _ in range(2)]

            units = [(s, hp) for s in range(NSEQ) for hp in range(8)]

            def load_unit(u):
                s, hp = units[u]
                qt_, qb_, kt_, kb_, va_, vb_ = ld_ring[u % 2]
                va4 = va_.rearrange("p (k h m) -> p k h m", h=2, m=128)
                ts0 = s * SEQ
                c.dma(qt_, QT[hp * 128:(hp + 1) * 128, ts0:ts0 + SEQ], reads=[dQT], writes=[qb_], q=wq)
                if mixer == 1:
                    g = hp // 2
                    for hh in range(2):
                        c.dma(kt_[hh * 64:(hh + 1) * 64, :], KT[g * 64:(g + 1) * 64, ts0:ts0 + SEQ], reads=[dKT], writes=[kb_], q=wq)
                        c.dma(va4[:, :, hh, 0:64], V[ts0:ts0 + SEQ, g * 64:(g + 1) * 64].rearrange("(k p) d -> p k d", p=128),
                              reads=[dV], writes=[vb_], q=wq)
                else:
                    c.dma(kt_, KT[hp * 128:(hp + 1) * 128, ts0:ts0 + SEQ], reads=[dKT], writes=[kb_], q=wq)
                    for hh in range(2):
                        f0 = hp * 128 + hh * 64
                        c.dma(va4[:, :, hh, 0:64], V[ts0:ts0 + SEQ, f0:f0 + 64].rearrange("(k p) d -> p k d", p=128),
                              reads=[dV], writes=[vb_], q=wq)

            def unit_prep(u):
                qt_, qb_, kt_, kb_, va_, vb_ = ld_ring[u % 2]
                c.op(dve, "tensor_reduce", km, kt_.bitcast(F32).rearrange("p (n k) -> p n k", k=256), axis=AX.X, op=ALU.add,
                     reads=[kb_], writes=[kmb])
                c.op(dve, "tensor_scalar", km, km, 1.0 / 256.0, None, op0=ALU.mult, reads=[kmb], writes=[kmb])
                c.op(dve, "tensor_copy", kmr, km, reads=[kmb], writes=[kmrb])
                for n in range(8):
                    c.op(dve, "tensor_scalar", kmrep3[:, n, :], ones_f, km[:, n:n + 1], None, op0=ALU.mult,
                         reads=[kmb, b_const], writes=[kmrepb])

            def head_prep(u, hh, st):
                s, hp = units[u]
                slope = SLOPES[hp * 2 + hh]
                bq, bqb = nxt(bq_ring, "bq")
                c.op(dve, "tensor_scalar", bq, cb_f, slope, None, op0=ALU.mult, reads=[b_const], writes=[bqb])
                bqm = bqm_ring[cnt.get("bqm", 0) % 2]
                cnt["bqm"] = cnt.get("bqm", 0) + 1
                for mi in range(nm):
                    c.op(dve, "tensor_tensor", bqm[mi][0], bq, masks[mi][0], op=ALU.add, reads=[bqb, masks[mi][1]], writes=[bqm[mi][1]])
                st["bq"], st["bqb"], st["bqm"] = bq, bqb, bqm
                if mixer == 0:
                    bqB, _ = nxt(bqB_ring, "bqB")
                    c.op(dve, "tensor_scalar", bqB, bq, -BIG, None, op0=ALU.add, reads=[bqb], writes=[bqb])
                    st["bqB"] = bqB

            def sel_prep1(u, hh, qc, st):
                qt_, qb_ = ld_ring[u % 2][0:2]
                hs = slice(hh * 64, (hh + 1) * 64)
                dgs = []
                for jq in range(4):
                    qtile = qc * 4 + jq
                    blk = qtile // 2
                    q128 = slice(qtile * 128, (qtile + 1) * 128)
                    pg, pgb = PS[7]
                    c.mm(pg[:, 0:8], pgb, [(qt_[hs, q128], kmr[hs, 0:8], [qb_, kmrb])])
                    gs, gsb = nxt(gsb_ring, "gsb")
                    c.op(dve, "memset", gs, -BIG, writes=[gsb])
                    c.op(dve, "tensor_copy", gs[:, 0:blk], pg[:, 0:blk], reads=[pgb], writes=[gsb])
                    tp, tpb = nxt(top_ring, "top")
                    c.op(dve, "max", tp[:, 0:8], gs, reads=[gsb], writes=[tpb])
                    c.op(dve, "tensor_tensor", tp[:, 8:9], tp[:, 2:3], tp[:, 3:4], op=ALU.add, reads=[tpb], writes=[tpb])
                    c.op(dve, "tensor_scalar", tp[:, 9:10], tp[:, 8:9], 0.5, None, op0=ALU.mult, reads=[tpb], writes=[tpb])
                    dg, dgb = nxt(diag_ring, "diag")
                    c.op(dve, "tensor_scalar", dg, ident, tp[:, 9:10], None, op0=ALU.mult, reads=[tpb, b_const], writes=[dgb])
                    dgs.append((dg, dgb))
                st[("dg", qc)] = dgs

            def sel_prep2(u, hh, qc, st):
                thr, thrb = nxt(thr_ring, "thr")
                pth, pthb = PS[7]
                for jq, (dg, dgb) in enumerate(st[("dg", qc)]):
                    c.mm(pth[:, jq * 128:(jq + 1) * 128], pthb, [(ones_r, dg, [dgb, b_const])])
                c.op(act, "copy", thr, pth[:, :], reads=[pthb], writes=[thrb])
                st[("thr", qc)] = (thr, thrb)

            tasks = []
            for u, (s, hp) in enumerate(units):
                qt_, qb_, kt_, kb_, va_, vb_ = ld_ring[u % 2]
                ot_, ob_ = ot_ring[u % 2]
                va4 = va_.rearrange("p (k h m) -> p k h m", h=2, m=128)
                ts0 = s * SEQ
                utasks = []
                for hh in range(2):
                    h = hp * 2 + hh
                    hs = slice(hh * 64, (hh + 1) * 64)
                    slope = SLOPES[h]
                    st = {}
                    for qc in range(4):
                        qs = slice(qc * 512, (qc + 1) * 512)
                        if mixer == 0:
                            kts = list(range(0, 4 * qc + 4))
                        elif mixer == 1:
                            kts = list(range(max(0, 4 * qc - 1), 4 * qc + 4))
                        else:
                            kts = list(range(4 * qc + 3, -1, -1))
                        grp = {"acc": None, "racc": None}
                        nk = len(kts)
                        for ki, kt in enumerate(kts):
                            ks = slice(kt * 128, (kt + 1) * 128)
                            first, last = (ki == 0), (ki == nk - 1)
                            pre = []
                            if hh == 0 and qc == 0 and ki == 0:
                                if u == 0:
                                    pre.append(lambda u=u: load_unit(u))
                                if mixer == 0:
                                    pre.append(lambda u=u: unit_prep(u))
                            if qc == 0 and ki == 0 and mixer in (0, 1):
                                pre.append(lambda u=u, hh=hh, st=st: head_prep(u, hh, st))
                            if mixer == 0 and qc in (1, 2):
                                if ki == 0:
                                    pre.append(lambda u=u, hh=hh, qc=qc, st=st: sel_prep1(u, hh, qc + 1, st))
                                if ki == nk // 2:
                                    pre.append(lambda u=u, hh=hh, qc=qc, st=st: sel_prep2(u, hh, qc + 1, st))
                            if hh == 0 and qc == 1 and ki == 0 and u + 1 < len(units):
                                pre.append(lambda u=u: load_unit(u + 1))

                            if mixer in (0, 1):
                                def A(pre=pre, grp=grp, st=st, kt=kt, ks=ks, qs=qs, qc=qc, hs=hs, first=first, qt_=qt_, qb_=qb_, kt_=kt_, kb_=kb_,
                                      slope=slope, tk=None):
                                    for f in pre:
                                        f()
                                    if first:
                                        grp["acc"] = PS[5 + cnt.get("acc", 0) % 2]
                                        cnt["acc"] = cnt.get("acc", 0) + 1
                                    bq, bqb, bqm = st["bq"], st["bqb"], st["bqm"]
                                    n = kt // 2
                                    bias2 = None
                                    if mixer == 0:
                                        mi = kt - 4 * qc
                                        if qc >= 2 and n <= 2 * qc:
                                            if kt % 2 == 0:
                                                thr, thrb = st[("thr", qc)]
                                                pg, pgb = PS[3 + cnt.get("G", 0) % 2]
                                                cnt["G"] = cnt.get("G", 0) + 1
                                                c.mm(pg[:, :], pgb, [(kmrep3[hs, n, :], qt_[hs, qs], [kmrepb, qb_])])
                                                cm, cmb = nxt(comb_ring, "comb")
                                                c.op(dve, "tensor_tensor", cm, pg[:, :], thr, op=ALU.is_ge, reads=[pgb, thrb], writes=[cmb])
                                                c.op(dve, "scalar_tensor_tensor", cm, cm, BIG, st["bqB"], op0=ALU.mult, op1=ALU.add,
                                                     reads=[cmb, bqb], writes=[cmb])
                                                st["comb"] = (cm, cmb)
                                            bias = st["comb"]
                                            if n == 2 * qc:
                                                bias2 = bqm[mi]
                                        else:
                                            bias = bqm[mi] if mi >= 0 else (bq, bqb)
                                    else:
                                        bias = bqm[kt - 4 * qc + 1]
                                    pS, pSb = PS[cnt.get("S", 0) % 3]
                                    cnt["S"] = cnt.get("S", 0) + 1
                                    c.mm(pS[:, :], pSb, [(kt_[hs, ks], qt_[hs, qs], [kb_, qb_])])
                                    tm, tmb = nxt(tmp_ring, "tmp")
                                    c.op(dve, "scalar_tensor_tensor", tm, pS[:, :], 0.125, bias[0], op0=ALU.mult, op1=ALU.add,
                                         reads=[pSb, bias[1]], writes=[tmb])
                                    if bias2 is not None:
                                        c.op(dve, "scalar_tensor_tensor", tm[:, 0:256], pS[:, 0:256], 0.125, bias2[0][:, 0:256], op0=ALU.mult, op1=ALU.add,
                                             reads=[pSb, bias2[1], tmb], writes=[tmb])
                                    Pt, Pb = nxt(P_ring, "P")
                                    c.op(act, "activation", Pt, tm, AF.Exp, bias=float(-slope * (qc * 512 - kt * 128)), scale=1.0,
                                         reads=[tmb], writes=[Pb])
                                    tk["P"] = (Pt, Pb)

                                def B(grp=grp, kt=kt, hh=hh, h=h, hs=hs, qs=qs, first=first, last=last, va4=va4, vb_=vb_, ot_=ot_, ob_=ob_,
                                      hp=hp, ts0=ts0, qc=qc, tk=None):
                                    acc, accb = grp["acc"]
                                    Pt, Pb = tk["P"]
                                    c.mm(acc[:, :], accb, [(va4[:, kt, hh, :], Pt, [vb_, Pb])], start=first, stop=last)
                                    if last:
                                        rc, rcb = nxt(rec_ring, "rec")
                                        if mixer == 1:
                                            c.op(act, "activation", rc[64:128, :], acc[64:128, :], AF.Ln, bias=esk[64:128, h:h + 1], scale=1.0,
                                                 reads=[accb, eskb], writes=[rcb])
                                        else:
                                            c.op(act, "activation", rc[64:128, :], acc[64:128, :], AF.Ln, reads=[accb], writes=[rcb])
                                        c.op(act, "activation", rc[64:128, :], rc[64:128, :], AF.Exp, scale=-1.0, reads=[rcb], writes=[rcb])
                                        c.op(dve, "tensor_tensor", ot_[hs, qs], acc[0:64, :], rc[64:128, :], op=ALU.mult, reads=[accb, rcb], writes=[ob_])
                                        if hh == 1 and qc == 3:
                                            c.dma(OT[hp * 128:(hp + 1) * 128, ts0:ts0 + SEQ], ot_.bitcast(F32), reads=[ob_], writes=[dOT])
                                tk = {}
                                utasks.append((lambda A=A, tk=tk: A(tk=tk), None, lambda B=B, tk=tk: B(tk=tk)))
                            else:
                                diag = kt >= 4 * qc
                                base = qc * 512 - kt * 128

                                def A1(pre=pre, grp=grp, kt=kt, ks=ks, qs=qs, hs=hs, first=first, last=last, diag=diag, base=base,
                                       qt_=qt_, qb_=qb_, kt_=kt_, kb_=kb_, tk=None):
                                    for f in pre:
                                        f()
                                    if first:
                                        grp["acc"] = PS[5 + cnt.get("acc", 0) % 2]
                                        cnt["acc"] = cnt.get("acc", 0) + 1
                                        grp["racc"] = None
                                    pS, pSb = PS[cnt.get("S", 0) % 3]
                                    cnt["S"] = cnt.get("S", 0) + 1
                                    c.mm(pS[:, :], pSb, [(kt_[hs, ks], qt_[hs, qs], [kb_, qb_])])
                                    et, etb = nxt(e_ring, "e")
                                    c.op(act, "activation", et, pS[:, :], AF.Exp, scale=0.125, reads=[pSb], writes=[etb])
                                    spt, spb = nxt(sp_ring, "sp")
                                    c.op(act, "activation", spt, et, AF.Ln, bias=1.0, scale=1.0, reads=[etb], writes=[spb])
                                    if diag:
                                        c.op(pool, "affine_select", spt, spt, pattern=[[1, 512]], compare_op=ALU.is_gt, fill=0.0,
                                             base=base, channel_multiplier=-1, reads=[spb], writes=[spb])
                                    tk["S"] = (pS, pSb)
                                    tk["sp"] = (spt, spb)
                                    tk["rprev"] = grp["racc"]
                                    if not last:
                                        rn, rnb = nxt(racc_ring, "racc")
                                        if grp["racc"] is None:
                                            c.op(pool, "tensor_copy", rn, spt, reads=[spb], writes=[rnb])
                                        else:
                                            c.op(pool, "tensor_tensor", rn, grp["racc"][0].bitcast(F32), spt.bitcast(F32), op=ALU.add,
                                                 reads=[grp["racc"][1], spb], writes=[rnb])
                                        grp["racc"] = (rn, rnb)

                                def A2(diag=diag, base=base, tk=None):
                                    pS, pSb = tk["S"]
                                    spt, spb = tk["sp"]
                                    pa, pab = PS[3 + cnt.get("pa", 0) % 2]
                                    cnt["pa"] = cnt.get("pa", 0) + 1
                                    parts = [(ustr_r, spt, [spb, b_const])]
                                    if tk["rprev"] is not None:
                                        parts.append((ones_r, tk["rprev"][0], [tk["rprev"][1], b_const]))
                                    c.mm(pa[:, :], pab, parts)
                                    tm, tmb = nxt(tmp_ring, "tmp")
                                    c.op(dve, "scalar_tensor_tensor", tm, pS[:, :], 0.125, spt.bitcast(F32), op0=ALU.mult, op1=ALU.subtract,
                                         reads=[pSb, spb], writes=[tmb])
                                    t3, t3b = nxt(t3_ring, "t3")
                                    c.op(dve, "tensor_tensor", t3, tm, pa[:, :], op=ALU.subtract, reads=[tmb, pab], writes=[t3b])
                                    Pt, Pb = nxt(P_ring, "P")
                                    c.op(act, "activation", Pt, t3, AF.Exp, reads=[t3b], writes=[Pb])
                                    if diag:
                                        c.op(pool, "affine_select", Pt, Pt, pattern=[[1, 512]], compare_op=ALU.is_gt, fill=0.0,
                                             base=base, channel_multiplier=-1, reads=[Pb], writes=[Pb])
                                    tk["P"] = (Pt, Pb)

                                def B(grp=grp, kt=kt, hh=hh, hs=hs, qs=qs, first=first, last=last, va4=va4, vb_=vb_, ot_=ot_, ob_=ob_,
                                      hp=hp, ts0=ts0, qc=qc, tk=None):
                                    acc, accb = grp["acc"]
                                    Pt, Pb = tk["P"]
                                    c.mm(acc[:, :], accb, [(va4[:, kt, hh, :], Pt, [vb_, Pb])], start=first, stop=last)
                                    if last:
                                        c.op(act, "copy", ot_[hs, qs], acc[0:64, :], reads=[accb], writes=[ob_])
                                        if hh == 1 and qc == 3:
                                            c.dma(OT[hp * 128:(hp + 1) * 128, ts0:ts0 + SEQ], ot_.bitcast(F32), reads=[ob_], writes=[dOT])
                                tk = {}
                                utasks.append((lambda A1=A1, tk=tk: A1(tk=tk), lambda A2=A2, tk=tk: A2(tk=tk), lambda B=B, tk=tk: B(tk=tk)))
                tasks.extend(utasks)
            lags = (0, 0, LA) if mixer in (0, 1) else (0, 1, 3)
            for t in range(len(tasks) + lags[-1]):
                for k in range(3):
                    i = t - lags[k]
                    if 0 <= i < len(tasks) and tasks[i][k] is not None:
                        tasks[i][k]()
            if debug and ("OT%d" % L) in dbg:
                c.dma(dbg["OT%d" % L], OT, reads=[dOT], writes=[dIN])

            c.phase_reset(KEEP)
            Wot, Wob = c.alloc(8 * D, MMDT)
            Wo3 = Wot.rearrange("p (c n) -> p c n", n=D)
            Wo_pc = Wo.rearrange("(p c) n -> p c n", c=8)
            for cc in range(8):
                c.dma(Wo3[:, cc, :], Wo_pc[:, cc, :], reads=[dIN], writes=[Wob], q=wq)
            Wr, Wrb = c.alloc(8 * 36)
            Wr3 = Wr.rearrange("p (c n) -> p c n", n=36)
            c.dma(Wr3, w_rt[L].rearrange("(p c) n -> p c n", c=8), reads=[dIN], writes=[Wrb])
            gB, gbuf = c.alloc(D)
            bB, _ = c.alloc(D)
            brt, _ = c.alloc(36)
            c.dma(gB, ln1_g[L:L + 1, :].broadcast_to([128, D]), reads=[dIN], writes=[gbuf])
            c.dma(bB, ln1_b[L:L + 1, :].broadcast_to([128, D]), reads=[dIN], writes=[gbuf])
            c.dma(brt, b_rt[L:L + 1, :].broadcast_to([128, 36]), reads=[dIN], writes=[gbuf])
            c.op(dve, "memset", cnt_b, 0.0, writes=[b_cnt])
            otb_ring = [c.alloc(8 * 128, MMDT) for _ in range(3)]
            xr_ring = [c.alloc(D) for _ in range(4)]
            x1_ring = [c.alloc(D) for _ in range(6)]
            x1T_ring = [c.alloc(8 * 128) for _ in range(2)]
            sc_ring = [{"st": c.alloc(12), "mv": c.alloc(4)} for _ in range(4)]
            rs_ring = [c.alloc(320) for _ in range(4)]
            for (rs, rsb) in rs_ring:
                c.op(dve, "memset", rs[:, 4:8], -BIG, writes=[rsb])

            def ln_a(y, yb, sc):
                st, stb = sc["st"]
                mv, mvb = sc["mv"]
                c.op(dve, "bn_stats", st[:, 0:6], y[:, 0:512], reads=[yb], writes=[stb])
                c.op(dve, "bn_stats", st[:, 6:12], y[:, 512:1024], reads=[yb], writes=[stb])
                c.op(dve, "bn_aggr", mv[:, 0:2], st[:, 0:12], reads=[stb], writes=[mvb])
                c.op(dve, "tensor_scalar", mv[:, 2:3], mv[:, 1:2], EPS, None, op0=ALU.add, reads=[mvb], writes=[mvb])
                c.op(act, "activation", mv[:, 2:3], mv[:, 2:3], AF.Sqrt, reads=[mvb], writes=[mvb])

            def ln_b(y, yb, sc):
                mv, mvb = sc["mv"]
                c.op(dve, "reciprocal", mv[:, 2:3], mv[:, 2:3], reads=[mvb], writes=[mvb])
                c.op(dve, "scalar_tensor_tensor", mv[:, 3:4], mv[:, 0:1], -1.0, mv[:, 2:3], op0=ALU.mult, op1=ALU.mult,
                     reads=[mvb], writes=[mvb])
                c.op(act, "activation", y, y, AF.Identity, bias=mv[:, 3:4], scale=mv[:, 2:3], reads=[yb, mvb], writes=[yb])

            def ln_c(y, yb, gB_, bB_, gbuf_, outt, outb):
                c.op(dve, "tensor_tensor", y, y, gB_, op=ALU.mult, reads=[yb, gbuf_], writes=[yb])
                c.op(pool, "tensor_tensor", outt, y, bB_, op=ALU.add, reads=[yb, gbuf_], writes=[outb])

            def p3_stages(tt):
                r0 = tt * 128
                otb, otbb = otb_ring[tt % 3]
                otb3 = otb.rearrange("p (c t) -> p c t", t=128)
                xr, xrb = xr_ring[tt % 4]
                y, yb = xr, xrb
                x1, x1b = x1_ring[tt % 6]
                x1T, x1Tb = x1T_ring[tt % 2]
                x1T3 = x1T.rearrange("p (c t) -> p c t", t=128)
                sc = sc_ring[tt % 4]
                rs, rsb = rs_ring[tt % 4]
                o = 40
                gm8 = rs[:, o:o + 8]; o += 8
                ngm = rs[:, o:o + 1]; o += 1
                ge = rs[:, o:o + 4]; o += 4
                gsum = rs[:, o:o + 1]; o += 1
                gp = rs[:, o:o + 1]; o += 1
                goh = rs[:, o:o + 4]; o += 4
                ml = rs[:, o:o + 32]; o += 32
                m8 = rs[:, o:o + 8]; o += 8
                sel = rs[:, o:o + 32]; o += 32
                sm = rs[:, o:o + 8]; o += 8
                ex = rs[:, o:o + 32]; o += 32
                wgt = rs[:, o:o + 32]; o += 32
                pos = rs[:, o:o + 32]; o += 32
                oh1 = rs[:, o:o + 32]; o += 32
                assert o <= 320
                R = dict(reads=[rsb], writes=[rsb])
                pl, plb = PS[4 + tt % 2]
                pp, ppb = PS[6 + tt % 2]

                def s0():
                    c.dma(otb3, OT[:, r0:r0 + 128].rearrange("(p c) t -> p c t", c=8), reads=[dOT], writes=[otbb], q=wq)
                    c.dma(xr, X[r0:r0 + 128, :], reads=[dX], writes=[xrb])

                def s1a():
                    for half in range(2):
                        pt, pb = PS[half]
                        c.mm(pt[:, :], pb, [(otb3[:, cc, :], Wo3[:, cc, half * 512:(half + 1) * 512], [otbb, Wob]) for cc in range(8)])
                        c.op(dve, "scalar_tensor_tensor", y[:, half * 512:(half + 1) * 512], xr[:, half * 512:(half + 1) * 512], ALPHA, pt[:, :],
                             op0=ALU.mult, op1=ALU.add, reads=[xrb, pb], writes=[yb])
                    ln_a(y, yb, sc)

                def s1b():
                    ln_b(y, yb, sc)

                def s1c():
                    ln_c(y, yb, gB, bB, gbuf, x1, x1b)
                    c.dma(X1[r0:r0 + 128, :], x1, reads=[x1b], writes=[dX1])

                def s2():
                    for half in range(2):
                        pt, pb = PS[2 + half]
                        for cc in range(4):
                            c4 = half * 4 + cc
                            c.transpose(pt[:, cc * 128:(cc + 1) * 128], pb, x1.rearrange("p (q c) -> p c q", c=8)[:, c4, :], [x1b], ident, signal=(cc == 3))
                        evac(x1T[:, half * 512:(half + 1) * 512], x1Tb, pt[:, :], pb)
                    c.mm(pl[:, 0:36], plb, [(x1T3[:, cc, :], Wr3[:, cc, :], [x1Tb, Wrb]) for cc in range(8)])

                def s3():
                    c.op(dve, "tensor_tensor", rs[:, 0:4], pl[:, 0:4], brt[:, 0:4], op=ALU.add, reads=[plb, gbuf], writes=[rsb])
                    c.op(dve, "tensor_tensor", rs[:, 8:40], pl[:, 4:36], brt[:, 4:36], op=ALU.add, reads=[plb, gbuf], writes=[rsb])
                    c.op(dve, "max", gm8, rs[:, 0:8], **R)
                    c.op(dve, "tensor_scalar", ngm, gm8[:, 0:1], -1.0, None, op0=ALU.mult, **R)
                    c.op(dve, "tensor_scalar", goh, rs[:, 0:4], gm8[:, 0:1], None, op0=ALU.is_ge, **R)
                    c.op(dve, "tensor_scalar", goh, goh, 1.0, BIG, op0=ALU.subtract, op1=ALU.mult, **R)
                    c.op(dve, "tensor_tensor", ml.rearrange("p (g e) -> p g e", e=8), rs[:, 8:40].rearrange("p (g e) -> p g e", e=8),
                         goh.unsqueeze(2).broadcast_to([128, 4, 8]), op=ALU.add, **R)
                    c.op(dve, "max", m8, ml, **R)
                    c.op(dve, "tensor_scalar", sel, ml, m8[:, 1:2], None, op0=ALU.is_ge, **R)
                    c.op(dve, "tensor_scalar", oh1, ml, m8[:, 0:1], None, op0=ALU.is_ge, **R)
                    c.op(dve, "tensor_tensor", sm[:, 0:1], m8[:, 1:2], m8[:, 0:1], op=ALU.subtract, **R)
                    c.op(dve, "tensor_scalar", sm[:, 1:2], m8[:, 0:1], -1.0, None, op0=ALU.mult, **R)
                    c.op(act, "activation", ge, rs[:, 0:4], AF.Exp, bias=ngm, scale=1.0, accum_out=gsum, **R)
                    c.op(act, "activation", sm[:, 2:3], sm[:, 0:1], AF.Exp, **R)
                    c.op(act, "activation", ex, ml, AF.Exp, bias=sm[:, 1:2], scale=1.0, **R)
                    c.mm(pp[:, 0:32], ppb, [(tri_f, sel, [b_const, rsb])])
                    c.mm(pp[:, 32:64], ppb, [(ones_f, sel, [b_const, rsb])])

                def s4():
                    c.op(dve, "reciprocal", gp, gsum, **R)
                    c.op(dve, "tensor_scalar", sm[:, 3:4], sm[:, 2:3], 1.0, None, op0=ALU.add, **R)
                    c.op(dve, "reciprocal", sm[:, 3:4], sm[:, 3:4], **R)
                    c.op(dve, "tensor_tensor", sm[:, 4:5], sm[:, 3:4], gp, op=ALU.mult, **R)
                    c.op(dve, "scalar_tensor_tensor", wgt, ex, sm[:, 4:5], sel, op0=ALU.mult, op1=ALU.mult, **R)
                    c.op(dve, "tensor_tensor", pos, pp[:, 0:32], cnt_b, op=ALU.add, reads=[ppb, b_cnt, rsb], writes=[rsb])
                    c.op(dve, "tensor_tensor", cnt_b, cnt_b, pp[:, 32:64], op=ALU.add, reads=[ppb, b_cnt], writes=[b_cnt])
                    c.op(dve, "scalar_tensor_tensor", pos, pos, float(CAP - 1), eoff, op0=ALU.min, op1=ALU.add, reads=[rsb, b_const], writes=[rsb])
                    c.op(dve, "tensor_tensor", sel, sel, oh1, op=ALU.subtract, **R)
                    c.op(dve, "tensor_tensor", ex, oh1, pos, op=ALU.mult, **R)
                    c.op(dve, "tensor_reduce", rt3[:, tt, 0:1], ex, axis=AX.X, op=ALU.add, reads=[rsb], writes=[b_rtab])
                    c.op(dve, "tensor_tensor", ex, sel, pos, op=ALU.mult, **R)
                    c.op(dve, "tensor_reduce", rt3[:, tt, 1:2], ex, axis=AX.X, op=ALU.add, reads=[rsb], writes=[b_rtab])
                    c.op(dve, "tensor_tensor", ex, oh1, wgt, op=ALU.mult, **R)
                    c.op(dve, "tensor_reduce", rt3[:, tt, 2:3], ex, axis=AX.X, op=ALU.add, reads=[rsb], writes=[b_rtab])
                    c.op(dve, "tensor_tensor", ex, sel, wgt, op=ALU.mult, **R)
                    c.op(dve, "tensor_reduce", rt3[:, tt, 3:4], ex, axis=AX.X, op=ALU.add, reads=[rsb], writes=[b_rtab])
                    c.op(dve, "tensor_copy", ridx3[:, tt, :], rt3[:, tt, 0:2], reads=[b_rtab], writes=[b_ridx])

                def s5():
                    for k in range(2):
                        c.dma(XS[:, :], x1, reads=[x1b, b_ridx], writes=[dXS], q=pool,
                              indirect=dict(out_offset=bass.IndirectOffsetOnAxis(ap=ridx3[:, tt, k:k + 1], axis=0), in_offset=None,
                                            bounds_check=bc_reg, oob_is_err=False))
                return (s0, s1a, s1b, s1c, s2, s3, s4, s5)

            p3t = [p3_stages(tt) for tt in range(T // 128)]
            nst = 8
            for t in range(len(p3t) + nst - 1):
                for k in range(nst - 1, -1, -1):
                    i = t - k
                    if 0 <= i < len(p3t):
                        p3t[i][k]()
            if debug and ("X1_%d" % L) in dbg:
                c.dma(dbg["X1_%d" % L], X1, reads=[dX1], writes=[dIN])
            if debug and ("rt%d" % L) in dbg:
                c.dma(dbg["rt%d" % L], rt, reads=[b_rtab], writes=[dIN])

            c.phase_reset(KEEP)
            w_ring = []
            for _ in range(2):
                w_ring.append((c.alloc(8 * DEXP, MMDT), c.alloc(8 * DEXP, MMDT), c.alloc(4 * D, MMDT)))
            xs_ring = [c.alloc(3 * D) for _ in range(2)]
            xsT_ring = [c.alloc(8 * CAP, MMDT) for _ in range(2)]
            hT_ring = [c.alloc(4 * CAP, MMDT) for _ in range(2)]
            sg_ring = [c.alloc(CAP) for _ in range(2)]
            ys_ring = [c.alloc(D) for _ in range(3)]
            NJ = CAP // 128
            ysi = 0
            sgi = 0

            def load_w(e):
                (wg, wgb), (wu, wub), (wd, wdb) = w_ring[e % 2]
                wg3 = wg.rearrange("p (c n) -> p c n", n=DEXP)
                wu3 = wu.rearrange("p (c n) -> p c n", n=DEXP)
                wd3 = wd.rearrange("p (c n) -> p c n", n=D)
                c.dma(wg, w_e_gate[L, e].rearrange("(p c) f -> p (c f)", c=8), reads=[dIN], writes=[wgb], q=wq)
                c.dma(wu, w_e_up[L, e].rearrange("(p c) f -> p (c f)", c=8), reads=[dIN], writes=[wub], q=wq)
                c.dma(wd, w_e_down[L, e].rearrange("(p c) n -> p (c n)", c=4), reads=[dIN], writes=[wdb], q=wq)

            def load_xs(e):
                xs, xsb = xs_ring[e % 2]
                c.dma(xs.rearrange("p (j d) -> p j d", d=D), XS[e * CAP:(e + 1) * CAP, :].rearrange("(j p) d -> p j d", p=128),
                      reads=[dXS], writes=[xsb])

            load_w(0)
            load_xs(0)
            for e in range(NEXP):
                if e + 1 < NEXP:
                    load_w(e + 1)
                    load_xs(e + 1)
                (wg, wgb), (wu, wub), (wd, wdb) = w_ring[e % 2]
                wg3 = wg.rearrange("p (c n) -> p c n", n=DEXP)
                wu3 = wu.rearrange("p (c n) -> p c n", n=DEXP)
                wd3 = wd.rearrange("p (c n) -> p c n", n=D)
                xs, xsb = xs_ring[e % 2]
                xs3 = xs.rearrange("p (j d) -> p j d", d=D)
                xs4 = xs.rearrange("p (j q c) -> p j c q", q=128, c=8)
                wg4 = wg.rearrange("p (c m k) -> p c k m", c=8, m=128, k=4)
                wu4 = wu.rearrange("p (c m k) -> p c k m", c=8, m=128, k=4)
                xsT, xsTb = xsT_ring[e % 2]
                xsT3 = xsT.rearrange("p (c t) -> p c t", t=CAP)
                hT, hTb = hT_ring[e % 2]
                hT3 = hT.rearrange("p (c t) -> p c t", t=CAP)
                for cc in range(8):
                    pt, pb = PS[cc % 2]
                    for jj in range(NJ):
                        c.transpose(pt[:, jj * 128:(jj + 1) * 128], pb, xs4[:, jj, cc, :], [xsb], ident, signal=(jj == NJ - 1))
                    evac(xsT3[:, cc, :], xsTb, pt[:, 0:CAP], pb)
                for fc in range(4):
                    pg, pgb = PS[2 + fc % 2]
                    pu, pub = PS[4 + fc % 2]
                    c.mm(pg[:, 0:CAP], pgb, [(wg4[:, cc, fc, :], xsT3[:, cc, :], [wgb, xsTb]) for cc in range(8)])
                    c.mm(pu[:, 0:CAP], pub, [(wu4[:, cc, fc, :], xsT3[:, cc, :], [wub, xsTb]) for cc in range(8)])
                    sg, sgb = sg_ring[sgi % 2]; sgi += 1
                    c.op(act, "activation", sg, pg[:, 0:CAP], AF.Silu, reads=[pgb], writes=[sgb])
                    c.op(dve, "tensor_tensor", hT3[:, fc, :], sg, pu[:, 0:CAP], op=ALU.mult, reads=[sgb, pub], writes=[hTb])
                for jj in range(NJ):
                    ys, ysb = ys_ring[ysi % 3]; ysi += 1
                    for half in range(2):
                        pt, pb = PS[6 + half]
                        c.mm(pt[:, :], pb, [(hT3[:, fc, jj * 128:(jj + 1) * 128], wd3[:, fc, half * 512:(half + 1) * 512], [hTb, wdb]) for fc in range(4)])
                        evac(ys[:, half * 512:(half + 1) * 512], ysb, pt[:, :], pb)
                    r0 = e * CAP + jj * 128
                    c.dma(YS[r0:r0 + 128, :], ys, reads=[ysb], writes=[dYS])

            c.phase_reset(KEEP)
            Wg_, Wgb_ = c.alloc(8 * D, MMDT)
            Wg3 = Wg_.rearrange("p (c n) -> p c n", n=D)
            Wg_pc = w_ple_gate[L].rearrange("(p c) n -> p c n", c=8)
            for cc in range(8):
                c.dma(Wg3[:, cc, :], Wg_pc[:, cc, :], reads=[dIN], writes=[Wgb_], q=wq)
            Wp_, Wpb_ = c.alloc(2 * D, MMDT)
            Wp3 = Wp_.rearrange("p (c n) -> p c n", n=D)
            c.dma(Wp_, w_ple_proj[L].rearrange("(p c) n -> p (c n)", c=2), reads=[dIN], writes=[Wpb_], q=wq)
            gB, gbuf = c.alloc(D)
            bB, _ = c.alloc(D)
            c.dma(gB, ln2_g[L:L + 1, :].broadcast_to([128, D]), reads=[dIN], writes=[gbuf])
            c.dma(bB, ln2_b[L:L + 1, :].broadcast_to([128, D]), reads=[dIN], writes=[gbuf])
            y1_ring = [c.alloc(D) for _ in range(4)]
            y2_ring = [c.alloc(D) for _ in range(2)]
            x1_ring = [c.alloc(D) for _ in range(2)]
            pr_ring = [c.alloc(PLE) for _ in range(6)]
            x2_ring = [c.alloc(D) for _ in range(3)]
            x2T_ring = [c.alloc(8 * 128, MMDT) for _ in range(2)]
            pT_ring = [c.alloc(2 * 128, MMDT) for _ in range(2)]
            sig_ring = [c.alloc(D) for _ in range(2)]
            sc_ring = [{"st": c.alloc(12), "mv": c.alloc(4)} for _ in range(4)]
            dst = out if L == DEPTH - 1 else X
            dstb = dIN if L == DEPTH - 1 else dX

            def p5_stages(tt):
                r0 = tt * 128
                y1, y1b = y1_ring[tt % 4]; y2, y2b = y2_ring[tt % 2]; x1, x1b = x1_ring[tt % 2]; pr, prb = pr_ring[tt % 6]
                y, yb = y1, y1b
                x2, x2b = x2_ring[tt % 3]; x2T, x2Tb = x2T_ring[tt % 2]; pT, pTb = pT_ring[tt % 2]
                sig, sigb = sig_ring[tt % 2]; sc = sc_ring[tt % 4]
                x2T3 = x2T.rearrange("p (c t) -> p c t", t=128)
                pT3 = pT.rearrange("p (c t) -> p c t", t=128)

                def s0():
                    for k, (yy, yyb) in enumerate(((y1, y1b), (y2, y2b))):
                        c.dma(yy, YS[:, :], reads=[dYS, b_ridx], writes=[yyb], q=pool,
                              indirect=dict(out_offset=None, in_offset=bass.IndirectOffsetOnAxis(ap=ridx3[:, tt, k:k + 1], axis=0),
                                            bounds_check=bc_reg, oob_is_err=False))
                    c.dma(x1, X1[r0:r0 + 128, :], reads=[dX1], writes=[x1b])
                    c.dma(pr, p_in[L, r0:r0 + 128, :], reads=[dIN], writes=[prb])

                def s1a():
                    c.op(dve, "tensor_scalar", y2, y2, rt3[:, tt, 3:4], None, op0=ALU.mult, reads=[y2b, b_rtab], writes=[y2b])
                    c.op(dve, "scalar_tensor_tensor", y1, y1, rt3[:, tt, 2:3], y2, op0=ALU.mult, op1=ALU.add, reads=[y1b, y2b, b_rtab], writes=[y1b])
                    c.op(dve, "scalar_tensor_tensor", y, x1, ALPHA, y1, op0=ALU.mult, op1=ALU.add, reads=[x1b, y1b], writes=[yb])
                    ln_a(y, yb, sc)

                def s1b():
                    ln_b(y, yb, sc)

                def s1c():
                    ln_c(y, yb, gB, bB, gbuf, x2, x2b)

                def s2():
                    for half in range(2):
                        pt, pb = PS[half]
                        for cc in range(4):
                            c4 = half * 4 + cc
                            c.transpose(pt[:, cc * 128:(cc + 1) * 128], pb, x2.rearrange("p (q c) -> p c q", c=8)[:, c4, :], [x2b], ident, signal=(cc == 3))
                        evac(x2T[:, half * 512:(half + 1) * 512], x2Tb, pt[:, :], pb)
                    pt, pb = PS[2]
                    for cc in range(2):
                        c.transpose(pt[:, cc * 128:(cc + 1) * 128], pb, pr.rearrange("p (q c) -> p c q", c=2)[:, cc, :], [prb], ident, signal=(cc == 1))
                    evac(pT, pTb, pt[:, 0:256], pb)

                def s3a():
                    for half in range(2):
                        hsl = slice(half * 512, (half + 1) * 512)
                        pg, pgb = PS[4 + half]
                        c.mm(pg[:, :], pgb, [(x2T3[:, cc, :], Wg3[:, cc, hsl], [x2Tb, Wgb_]) for cc in range(8)])
                        c.op(act, "activation", sig[:, hsl], pg[:, :], AF.Sigmoid, reads=[pgb], writes=[sigb])
                        pq, pqb = PS[6 + half]
                        c.mm(pq[:, :], pqb, [(pT3[:, cc, :], Wp3[:, cc, hsl], [pTb, Wpb_]) for cc in range(2)])

                def s3b():
                    for half in range(2):
                        hsl = slice(half * 512, (half + 1) * 512)
                        pq, pqb = PS[6 + half]
                        c.op(dve, "tensor_tensor", sig[:, hsl], sig[:, hsl], pq[:, :], op=ALU.mult, reads=[sigb, pqb], writes=[sigb])
                    c.op(pool, "tensor_tensor", sig, sig, x2, op=ALU.add, reads=[sigb, x2b], writes=[sigb])
                    c.dma(dst[r0:r0 + 128, :], sig, reads=[sigb], writes=[dstb])
                return (s0, s1a, s1b, s1c, s2, s3a, s3b)

            p5t = [p5_stages(tt) for tt in range(T // 128)]
            nst = 7
            for t in range(len(p5t) + nst - 1):
                for k in range(nst - 1, -1, -1):
                    i = t - k
                    if 0 <= i < len(p5t):
                        p5t[i][k]()
            if debug and ("X3_%d" % L) in dbg:
                c.barrier()
                c.dma(dbg["X3_%d" % L], dst, reads=[dstb], writes=[dIN])

        if nlayers < DEPTH:
            c.barrier()
            c.dma(out, X, reads=[dX], writes=[dIN])
        c.barrier()
    return nc


def _consts():
    cst = np.zeros((128, 6, 128), np.float32)
    k = np.arange(128)[:, None]
    m = np.arange(128)[None, :]
    cst[:, 0, :] = (k == m)
    cst[:, 1, :] = 1.0
    cst[:, 2, :] = (k < m)
    cst[:, 3, :] = (k > m)
    q = np.arange(512)[None, :]
    kk = np.arange(128)[:, None]
    cmask = np.zeros((9, 128, 512), np.float32)
    for i in range(4):
        cmask[i] = np.where((-128 * i + q - kk) >= 0, 0.0, -BIG)
    for i in range(5):
        dlt = 128 - 128 * i
        v = dlt + q - kk
        cmask[4 + i] = np.where((v >= 0) & (v < 128), 0.0, -BIG)
    cbase = (-(q - kk)).astype(np.float32)
    ceoff = np.tile((np.arange(NEXP) * CAP).astype(np.float32)[None, :], (128, 1))
    return cst, cmask, np.ascontiguousarray(cbase), np.ascontiguousarray(ceoff)


_NC_CACHE = {}


def kernel(**inputs):
    f = lambda a: np.ascontiguousarray(np.asarray(a, dtype=np.float32))
    x = f(inputs["x"]).reshape(NCORES, T, D)
    p = f(inputs["p"]).reshape(DEPTH, NCORES, T, PLE)
    cst, cmask, cbase, ceoff = _consts()
    w_rt = np.ascontiguousarray(np.concatenate([f(inputs["w_grp"]), f(inputs["w_exp"])], axis=2))
    b_rt = np.ascontiguousarray(np.concatenate([f(inputs["b_grp"]), f(inputs["b_exp"])], axis=1))
    shared = {k: f(inputs[k]) for k in ("w_qkv_a", "w_o_a", "w_qkv_b", "w_o_b", "sinks_b", "w_qkv_c", "w_o_c", "ln1_g", "ln1_b",
                                         "ln2_g", "ln2_b", "w_e_gate", "w_e_up", "w_e_down", "w_ple_gate", "w_ple_proj")}
    shared.update(w_rt=w_rt, b_rt=b_rt, cst=cst, cmask=cmask, cbase=cbase, ceoff=ceoff)
    if "nc" not in _NC_CACHE:
        _NC_CACHE["nc"] = build()
    nc = _NC_CACHE["nc"]
    in_maps = []
    for i in range(NCORES):
        m = dict(shared)
        m["x"] = x[i]
        m["p"] = np.ascontiguousarray(p[:, i])
        in_maps.append(m)
    res = run_bass_kernel_spmd(nc, in_maps, core_ids=list(range(NCORES)))
    o = np.stack([np.asarray(r["out"]) for r in res.results], axis=0)
    return o.reshape(16, SEQ, D).astype(np.float32)
```

```python
import numpy as np
import concourse.bass as bass
import concourse.mybir as mybir
from concourse.bass_utils import run_bass_kernel_spmd

F32 = mybir.dt.float32
F32R = mybir.dt.float32r
I32 = mybir.dt.int32
ALU = mybir.AluOpType
AF = mybir.ActivationFunctionType
AX = mybir.AxisListType

USE_F32R = True
MMDT = F32R if USE_F32R else F32

NCORES = 8
D = 1024
SEQ = 2048
NSEQ = 2
T = NSEQ * SEQ
DEPTH = 4
NH = 16
HD = 64
NEXP = 32
DEXP = 512
CAP = 384
PLE = 256
ALPHA = (2.0 * DEPTH) ** 0.25
EPS = 1e-5
BIG = 30000.0
SLOPES = [2.0 ** (-8.0 * (h + 1) / NH) for h in range(NH)]
XS_ROWS = NEXP * CAP + 128


class Eng:
    def __init__(self, name, h, sem):
        self.name, self.h, self.sem = name, h, sem
        self.count = 0
        self.known = {}


class Buf:
    __slots__ = ("name", "writers", "readers")

    def __init__(self, name):
        self.name = name
        self.writers = {}
        self.readers = {}


class Ctx:
    def __init__(self, nc, es):
        self.nc = nc
        self.es = es
        mk = lambda n, h: Eng(n, h, es.enter_context(nc.semaphore("s_" + n)))
        self.pe = mk("pe", nc.tensor)
        self.act = mk("act", nc.scalar)
        self.dve = mk("dve", nc.vector)
        self.pool = mk("pool", nc.gpsimd)
        self.sp = mk("sp", nc.sync)
        self.engs = [self.pe, self.act, self.dve, self.pool, self.sp]
        self.sems = {e.name: e.sem for e in self.engs}
        self.ndma = 40
        self.dsem = []
        for i in range(self.ndma):
            s = es.enter_context(nc.semaphore("s_dma%d" % i))
            self.dsem.append(s)
            self.sems["d%d" % i] = s
        self.dval = [0] * self.ndma
        self.dnext = 0
        self.FW, self.RW = 19100, 34048
        self.arena = es.enter_context(nc.sbuf_tensor("arena_f", [128, self.FW], F32))
        self.arena_r = es.enter_context(nc.sbuf_tensor("arena_r", [128, self.RW], MMDT))
        self.atop = 0
        self.rtop = 0
        self.psum = []
        for i in range(8):
            t = es.enter_context(nc.psum_tensor("psb%d" % i, [128, 512], F32))
            self.psum.append((t, Buf("ps%d" % i)))
        self.nbuf = 0
        self.future = []

    def alloc(self, ncols, dt=F32, name=None):
        if dt == MMDT and USE_F32R:
            a = self.arena_r[:, self.rtop:self.rtop + ncols]
            self.rtop += ncols
            assert self.rtop <= self.RW, "SBUF R arena overflow %d" % self.rtop
        else:
            a = self.arena[:, self.atop:self.atop + ncols]
            self.atop += ncols
            assert self.atop <= self.FW, "SBUF F arena overflow %d" % self.atop
            if dt != F32:
                a = a.bitcast(dt)
        self.nbuf += 1
        return a, Buf(name or ("b%d" % self.nbuf))

    def wait(self, E, key, val):
        if E.known.get(key, 0) >= val:
            return
        for F in self.engs:
            if F.name == key and val > F.count:
                self.future.append((E.name, key, val, F.count))
        E.h.wait_ge(self.sems[key], val)
        E.known[key] = val

    def _deps(self, E, reads, writes):
        evs = {}
        for b in reads:
            for k, v in b.writers.items():
                evs[k] = max(evs.get(k, 0), v)
        for b in writes:
            for k, v in b.readers.items():
                if k != E.name:
                    evs[k] = max(evs.get(k, 0), v)
            for k, v in b.writers.items():
                if k != E.name:
                    evs[k] = max(evs.get(k, 0), v)
        for k, v in evs.items():
            self.wait(E, k, v)

    def _record(self, key, val, reads, writes):
        for b in reads:
            b.readers[key] = max(b.readers.get(key, 0), val)
        for b in writes:
            if b.readers:
                b.readers = {}
                b.writers = {key: val}
            else:
                b.writers[key] = max(b.writers.get(key, 0), val)

    def op(self, E, fname, *args, reads=(), writes=(), signal=True, **kw):
        self._deps(E, reads, writes)
        ins = getattr(E.h, fname)(*args, **kw)
        if signal:
            E.count += 1
            ins.then_inc(E.sem, 1)
            self._record(E.name, E.count, reads, writes)
        else:
            self._record(E.name, E.count + 1, reads, writes)
        return ins

    def dma(self, out, in_, reads=(), writes=(), q=None, indirect=None, **kw):
        Q = q or self.sp
        self._deps(Q, reads, writes)
        i = self.dnext
        self.dnext = (self.dnext + 1) % self.ndma
        key = "d%d" % i
        self.wait(Q, key, self.dval[i])
        if indirect is None:
            ins = Q.h.dma_start(out=out, in_=in_, **kw)
        else:
            ins = Q.h.indirect_dma_start(out=out, in_=in_, **indirect)
        self.dval[i] += 16
        ins.then_inc(self.dsem[i], 16)
        self._record(key, self.dval[i], reads, writes)

    def barrier(self):
        for E in self.engs:
            for F in self.engs:
                if F is not E and F.count > 0:
                    self.wait(E, F.name, F.count)
            for i in range(self.ndma):
                if self.dval[i] > 0:
                    self.wait(E, "d%d" % i, self.dval[i])

    def phase_reset(self, keep):
        self.barrier()
        self.atop, self.rtop = keep

    def mm(self, out, outbuf, parts, start=True, stop=True):
        n = len(parts)
        for i, (l, r, rb) in enumerate(parts):
            last = (i == n - 1)
            self.op(self.pe, "matmul", out, l, r, start=(start and i == 0), stop=(stop and last),
                    reads=rb, writes=[outbuf], signal=last)

    def transpose(self, out, outbuf, in_, inbufs, ident, signal=True):
        self.op(self.pe, "transpose", out, in_, ident, reads=inbufs, writes=[outbuf], signal=signal)


def build(nlayers=DEPTH, debug=None):
    from contextlib import ExitStack
    nc = bass.Bass("TRN2", target_bir_lowering=False)
    dr = lambda n, s, kind="ExternalInput", dt=F32: nc.dram_tensor(n, list(s), dt, kind=kind).ap()
    x_in = dr("x", [T, D])
    p_in = dr("p", [DEPTH, T, PLE])
    w_qkv_a = dr("w_qkv_a", [2, D, 3 * D]); w_o_a = dr("w_o_a", [2, D, D])
    w_qkv_b = dr("w_qkv_b", [1, D, D + 512]); w_o_b = dr("w_o_b", [1, D, D])
    sinks_b = dr("sinks_b", [1, NH])
    w_qkv_c = dr("w_qkv_c", [1, D, 3 * D]); w_o_c = dr("w_o_c", [1, D, D])
    ln1_g = dr("ln1_g", [DEPTH, D]); ln1_b = dr("ln1_b", [DEPTH, D])
    ln2_g = dr("ln2_g", [DEPTH, D]); ln2_b = dr("ln2_b", [DEPTH, D])
    w_rt = dr("w_rt", [DEPTH, D, 36]); b_rt = dr("b_rt", [DEPTH, 36])
    w_e_gate = dr("w_e_gate", [DEPTH, NEXP, D, DEXP]); w_e_up = dr("w_e_up", [DEPTH, NEXP, D, DEXP])
    w_e_down = dr("w_e_down", [DEPTH, NEXP, DEXP, D])
    w_ple_gate = dr("w_ple_gate", [DEPTH, D, D]); w_ple_proj = dr("w_ple_proj", [DEPTH, PLE, D])
    cst = dr("cst", [128, 6, 128])
    cmask = dr("cmask", [9, 128, 512])
    cbase = dr("cbase", [128, 512])
    ceoff = dr("ceoff", [128, NEXP])
    out = dr("out", [T, D], kind="ExternalOutput")
    X = dr("Xs", [T, D], kind="Internal"); X1 = dr("X1s", [T, D], kind="Internal")
    QT = dr("QTs", [D, T], kind="Internal"); KT = dr("KTs", [D, T], kind="Internal")
    V = dr("Vs", [T, D], kind="Internal"); OT = dr("OTs", [D, T], kind="Internal")
    XS = dr("XSs", [XS_ROWS, D], kind="Internal"); YS = dr("YSs", [XS_ROWS, D], kind="Internal")
    dbg = {}
    if debug:
        for nme, shp in debug.items():
            dbg[nme] = dr("dbg_" + nme, shp, kind="ExternalOutput")
    dX, dX1, dQT, dKT, dV, dOT, dXS, dYS = [Buf(n) for n in ("X", "X1", "QT", "KT", "V", "OT", "XS", "YS")]
    dIN = Buf("in")

    with ExitStack() as es:
        c = Ctx(nc, es)
        pe, act, dve, pool, sp = c.pe, c.act, c.dve, c.pool, c.sp
        wq = pool if USE_F32R else sp
        PS = c.psum
        bc_reg = nc.gpsimd.to_reg(XS_ROWS - 1)

        ident, b_const = c.alloc(128)
        ones_r, _ = c.alloc(128, MMDT)
        tri_f, _ = c.alloc(128)
        ustr_r, _ = c.alloc(128, MMDT)
        ones_f, _ = c.alloc(128)
        cb_f, _ = c.alloc(512)
        eoff, _ = c.alloc(NEXP)
        rt, b_rtab = c.alloc(32 * 4)
        ridx, b_ridx = c.alloc(32 * 2, I32)
        cnt_b, b_cnt = c.alloc(NEXP)
        c.dma(ident, cst[:, 0, :], writes=[b_const])
        c.dma(ones_f, cst[:, 1, :], writes=[b_const])
        c.dma(tri_f, cst[:, 2, :], writes=[b_const])
        c.dma(ones_r, cst[:, 1, :], writes=[b_const], q=wq)
        c.dma(ustr_r, cst[:, 3, :], writes=[b_const], q=wq)
        c.dma(cb_f, cbase, writes=[b_const])
        c.dma(eoff, ceoff, writes=[b_const])
        KEEP = (c.atop, c.rtop)
        rt3 = rt.rearrange("p (t k) -> p t k", k=4)
        ridx3 = ridx.rearrange("p (t k) -> p t k", k=2)

        for i in range(8):
            c.dma(X[i * 512:(i + 1) * 512, :], x_in[i * 512:(i + 1) * 512, :], reads=[dIN], writes=[dX])

        state = {"ev": 0}

        def evac(out_ap, outb, in_ap, inb, extra_reads=()):
            state["ev"] ^= 1
            if state["ev"]:
                c.op(act, "copy", out_ap, in_ap, reads=[inb, *extra_reads], writes=[outb])
            else:
                c.op(dve, "tensor_copy", out_ap, in_ap, reads=[inb, *extra_reads], writes=[outb])

        def layer_norm(y, yb, gB, bB, gbuf, outt, outb, sc):
            st, stb = sc["st"]
            mv, mvb = sc["mv"]
            c.op(dve, "bn_stats", st[:, 0:6], y[:, 0:512], reads=[yb], writes=[stb])
            c.op(dve, "bn_stats", st[:, 6:12], y[:, 512:1024], reads=[yb], writes=[stb])
            c.op(dve, "bn_aggr", mv[:, 0:2], st[:, 0:12], reads=[stb], writes=[mvb])
            c.op(dve, "tensor_scalar", mv[:, 2:3], mv[:, 1:2], EPS, None, op0=ALU.add, reads=[mvb], writes=[mvb])
            c.op(act, "activation", mv[:, 2:3], mv[:, 2:3], AF.Sqrt, reads=[mvb], writes=[mvb])
            c.op(dve, "reciprocal", mv[:, 2:3], mv[:, 2:3], reads=[mvb], writes=[mvb])
            c.op(dve, "scalar_tensor_tensor", mv[:, 3:4], mv[:, 0:1], -1.0, mv[:, 2:3], op0=ALU.mult, op1=ALU.mult,
                 reads=[mvb], writes=[mvb])
            c.op(act, "activation", y, y, AF.Identity, bias=mv[:, 3:4], scale=mv[:, 2:3], reads=[yb, mvb], writes=[yb])
            c.op(dve, "tensor_tensor", y, y, gB, op=ALU.mult, reads=[yb, gbuf], writes=[yb])
            c.op(pool, "tensor_tensor", outt, y, bB, op=ALU.add, reads=[yb, gbuf], writes=[outb])

        for L in range(nlayers):
            mixer, j = L % 3, L // 3
            if mixer == 0:
                Wqkv, Wo, NQ, NKV = w_qkv_a[j], w_o_a[j], 1024, 1024
            elif mixer == 1:
                Wqkv, Wo, NQ, NKV = w_qkv_b[j], w_o_b[j], 1024, 256
            else:
                Wqkv, Wo, NQ, NKV = w_qkv_c[j], w_o_c[j], 1024, 1024
            NQKV = NQ + 2 * NKV

            c.phase_reset(KEEP)
            Wt, Wb = c.alloc(8 * NQKV, MMDT)
            W3 = Wt.rearrange("p (c n) -> p c n", n=NQKV)
            Wq_pc = Wqkv.rearrange("(p c) n -> p c n", c=8)
            for cc in range(8):
                for n0 in range(0, NQKV, 1024):
                    n1 = min(NQKV, n0 + 1024)
                    c.dma(W3[:, cc, n0:n1], Wq_pc[:, cc, n0:n1], reads=[dIN], writes=[Wb], q=wq)
            xr_ring = [c.alloc(4 * 1024) for _ in range(2)]
            xT_ring = [c.alloc(8 * 512, MMDT) for _ in range(2)]
            stg_ring = [c.alloc(512) for _ in range(4)]
            sti = 0
            pso = 0
            for tb in range(T // 512):
                xr, xrb = xr_ring[tb % 2]
                xT, xTb = xT_ring[tb % 2]
                xr3 = xr.rearrange("p (j d) -> p j d", d=1024)
                xr4 = xr.rearrange("p (j q c) -> p j c q", q=128, c=8)
                xT3 = xT.rearrange("p (c t) -> p c t", t=512)
                c.dma(xr3, X[tb * 512:(tb + 1) * 512, :].rearrange("(j p) d -> p j d", p=128), reads=[dX], writes=[xrb])
                for cc in range(8):
                    pt, pb = PS[cc % 2]
                    for jj in range(4):
                        c.transpose(pt[:, jj * 128:(jj + 1) * 128], pb, xr4[:, jj, cc, :], [xrb], ident,
                                    signal=(jj == 3))
                    evac(xT3[:, cc, :], xTb, pt[:, :], pb)
                for fc in range((NQ + NKV) // 128):
                    pt, pb = PS[2 + pso % 4]; pso += 1
                    c.mm(pt[:, :], pb, [(W3[:, cc, fc * 128:(fc + 1) * 128], xT3[:, cc, :], [Wb, xTb]) for cc in range(8)])
                    sg, sgb = stg_ring[sti % 4]; sti += 1
                    evac(sg, sgb, pt[:, :], pb)
                    if fc < NQ // 128:
                        c.dma(QT[fc * 128:(fc + 1) * 128, tb * 512:(tb + 1) * 512], sg, reads=[sgb], writes=[dQT])
                    else:
                        f2 = fc - NQ // 128
                        c.dma(KT[f2 * 128:(f2 + 1) * 128, tb * 512:(tb + 1) * 512], sg, reads=[sgb], writes=[dKT])
                for jj in range(4):
                    for v0 in range(0, NKV, 512):
                        nv = min(512, NKV - v0)
                        pt, pb = PS[2 + pso % 4]; pso += 1
                        c.mm(pt[:, 0:nv], pb, [(xT3[:, cc, jj * 128:(jj + 1) * 128], W3[:, cc, NQ + NKV + v0:NQ + NKV + v0 + nv], [Wb, xTb])
                                               for cc in range(8)])
                        sg, sgb = stg_ring[sti % 4]; sti += 1
                        evac(sg[:, 0:nv], sgb, pt[:, 0:nv], pb)
                        r0 = tb * 512 + jj * 128
                        c.dma(V[r0:r0 + 128, v0:v0 + nv], sg[:, 0:nv], reads=[sgb], writes=[dV])
            if debug and ("QT%d" % L) in dbg:
                c.dma(dbg["QT%d" % L], QT, reads=[dQT], writes=[dIN])
                c.dma(dbg["KT%d" % L], KT, reads=[dKT], writes=[dIN])
                c.dma(dbg["V%d" % L], V, reads=[dV], writes=[dIN])

            c.phase_reset(KEEP)
            LA = 3
            ld_ring = []
            for _ in range(2):
                qt_, qb_ = c.alloc(SEQ, MMDT)
                kt_, kb_ = c.alloc(SEQ, MMDT)
                va_, vb_ = c.alloc(16 * 2 * 128, MMDT)
                ld_ring.append((qt_, qb_, kt_, kb_, va_, vb_))
                c.op(dve, "tensor_copy", va_.rearrange("p (a b) -> p a b", b=128), ones_f.unsqueeze(1).broadcast_to([128, 32, 128]),
                     reads=[b_const], writes=[vb_])
            ot_ring = [c.alloc(SEQ, MMDT) for _ in range(2)]
            tmp_ring = [c.alloc(512) for _ in range(3)]
            P_ring = [c.alloc(512, MMDT) for _ in range(LA + 3)]
            rec_ring = [c.alloc(512) for _ in range(2)]
            cnt = {}

            def nxt(ring, key):
                i = cnt.get(key, 0)
                cnt[key] = i + 1
                return ring[i % len(ring)]

            if mixer in (0, 1):
                bq_ring = [c.alloc(512) for _ in range(2)]
                nm = 4 if mixer == 0 else 5
                masks = [c.alloc(512) for _ in range(nm)]
                for i, (m, mb) in enumerate(masks):
                    c.dma(m, cmask[i if mixer == 0 else 4 + i], writes=[mb])
                bqm_ring = [[c.alloc(512) for _ in range(nm)] for _ in range(2)]
            if mixer == 0:
                comb_ring = [c.alloc(512) for _ in range(3)]
                bqB_ring = [c.alloc(512) for _ in range(2)]
                km, kmb = c.alloc(8)
                kmr, kmrb = c.alloc(8, MMDT)
                kmrep, kmrepb = c.alloc(8 * 128, MMDT)
                kmrep3 = kmrep.rearrange("p (n m) -> p n m", m=128)
                gsb_ring = [c.alloc(8) for _ in range(4)]
                top_ring = [c.alloc(12) for _ in range(4)]
                diag_ring = [c.alloc(128, MMDT) for _ in range(8)]
                thr_ring = [c.alloc(512) for _ in range(2)]
            elif mixer == 1:
                esk, eskb = c.alloc(NH)
                c.dma(esk, sinks_b[0:1, :].broadcast_to([128, NH]), reads=[dIN], writes=[eskb])
                c.op(act, "activation", esk, esk, AF.Exp, reads=[eskb], writes=[eskb])
            else:
                sp_ring = [c.alloc(512, MMDT) for _ in range(4)]
                e_ring = [c.alloc(512) for _ in range(2)]
                racc_ring = [c.alloc(512, MMDT) for _ in range(5)]
                t3_ring = [c.alloc(512) for _ in range(2)]

            units = [(s, hp) for s in range(NSEQ) for hp in range(8)]

            def load_unit(u):
                s, hp = units[u]
                qt_, qb_, kt_, kb_, va_, vb_ = ld_ring[u % 2]
                va4 = va_.rearrange("p (k h m) -> p k h m", h=2, m=128)
                ts0 = s * SEQ
                c.dma(qt_, QT[hp * 128:(hp + 1) * 128, ts0:ts0 + SEQ], reads=[dQT], writes=[qb_], q=wq)
                if mixer == 1:
                    g = hp // 2
                    for hh in range(2):
                        c.dma(kt_[hh * 64:(hh + 1) * 64, :], KT[g * 64:(g + 1) * 64, ts0:ts0 + SEQ], reads=[dKT], writes=[kb_], q=wq)
                        c.dma(va4[:, :, hh, 0:64], V[ts0:ts0 + SEQ, g * 64:(g + 1) * 64].rearrange("(k p) d -> p k d", p=128),
                              reads=[dV], writes=[vb_], q=wq)
                else:
                    c.dma(kt_, KT[hp * 128:(hp + 1) * 128, ts0:ts0 + SEQ], reads=[dKT], writes=[kb_], q=wq)
                    for hh in range(2):
                        f0 = hp * 128 + hh * 64
                        c.dma(va4[:, :, hh, 0:64], V[ts0:ts0 + SEQ, f0:f0 + 64].rearrange("(k p) d -> p k d", p=128),
                              reads=[dV], writes=[vb_], q=wq)

            def unit_prep(u):
                qt_, qb_, kt_, kb_, va_, vb_ = ld_ring[u % 2]
                c.op(dve, "tensor_reduce", km, kt_.bitcast(F32).rearrange("p (n k) -> p n k", k=256), axis=AX.X, op=ALU.add,
                     reads=[kb_], writes=[kmb])
                c.op(dve, "tensor_scalar", km, km, 1.0 / 256.0, None, op0=ALU.mult, reads=[kmb], writes=[kmb])
                c.op(dve, "tensor_copy", kmr, km, reads=[kmb], writes=[kmrb])
                for n in range(8):
                    c.op(dve, "tensor_scalar", kmrep3[:, n, :], ones_f, km[:, n:n + 1], None, op0=ALU.mult,
                         reads=[kmb, b_const], writes=[kmrepb])

            def head_prep(u, hh, st):
                s, hp = units[u]
                slope = SLOPES[hp * 2 + hh]
                bq, bqb = nxt(bq_ring, "bq")
                c.op(dve, "tensor_scalar", bq, cb_f, slope, None, op0=ALU.mult, reads=[b_const], writes=[bqb])
                bqm = bqm_ring[cnt.get("bqm", 0) % 2]
                cnt["bqm"] = cnt.get("bqm", 0) + 1
                for mi in range(nm):
                    c.op(dve, "tensor_tensor", bqm[mi][0], bq, masks[mi][0], op=ALU.add, reads=[bqb, masks[mi][1]], writes=[bqm[mi][1]])
                st["bq"], st["bqb"], st["bqm"] = bq, bqb, bqm
                if mixer == 0:
                    bqB, _ = nxt(bqB_ring, "bqB")
                    c.op(dve, "tensor_scalar", bqB, bq, -BIG, None, op0=ALU.add, reads=[bqb], writes=[bqb])
                    st["bqB"] = bqB

            def sel_prep1(u, hh, qc, st):
                qt_, qb_ = ld_ring[u % 2][0:2]
                hs = slice(hh * 64, (hh + 1) * 64)
                dgs = []
                for jq in range(4):
                    qtile = qc * 4 + jq
                    blk = qtile // 2
                    q128 = slice(qtile * 128, (qtile + 1) * 128)
                    pg, pgb = PS[7]
                    c.mm(pg[:, 0:8], pgb, [(qt_[hs, q128], kmr[hs, 0:8], [qb_, kmrb])])
                    gs, gsb = nxt(gsb_ring, "gsb")
                    c.op(dve, "memset", gs, -BIG, writes=[gsb])
                    c.op(dve, "tensor_copy", gs[:, 0:blk], pg[:, 0:blk], reads=[pgb], writes=[gsb])
                    tp, tpb = nxt(top_ring, "top")
                    c.op(dve, "max", tp[:, 0:8], gs, reads=[gsb], writes=[tpb])
                    c.op(dve, "tensor_tensor", tp[:, 8:9], tp[:, 2:3], tp[:, 3:4], op=ALU.add, reads=[tpb], writes=[tpb])
                    c.op(dve, "tensor_scalar", tp[:, 9:10], tp[:, 8:9], 0.5, None, op0=ALU.mult, reads=[tpb], writes=[tpb])
                    dg, dgb = nxt(diag_ring, "diag")
                    c.op(dve, "tensor_scalar", dg, ident, tp[:, 9:10], None, op0=ALU.mult, reads=[tpb, b_const], writes=[dgb])
                    dgs.append((dg, dgb))
                st[("dg", qc)] = dgs

            def sel_prep2(u, hh, qc, st):
                thr, thrb = nxt(thr_ring, "thr")
                pth, pthb = PS[7]
                for jq, (dg, dgb) in enumerate(st[("dg", qc)]):
                    c.mm(pth[:, jq * 128:(jq + 1) * 128], pthb, [(ones_r, dg, [dgb, b_const])])
                c.op(act, "copy", thr, pth[:, :], reads=[pthb], writes=[thrb])
                st[("thr", qc)] = (thr, thrb)

            tasks = []
            for u, (s, hp) in enumerate(units):
                qt_, qb_, kt_, kb_, va_, vb_ = ld_ring[u % 2]
                ot_, ob_ = ot_ring[u % 2]
                va4 = va_.rearrange("p (k h m) -> p k h m", h=2, m=128)
                ts0 = s * SEQ
                utasks = []
                for hh in range(2):
                    h = hp * 2 + hh
                    hs = slice(hh * 64, (hh + 1) * 64)
                    slope = SLOPES[h]
                    st = {}
                    for qc in range(4):
                        qs = slice(qc * 512, (qc + 1) * 512)
                        if mixer == 0:
                            kts = list(range(0, 4 * qc + 4))
                        elif mixer == 1:
                            kts = list(range(max(0, 4 * qc - 1), 4 * qc + 4))
                        else:
                            kts = list(range(4 * qc + 3, -1, -1))
                        grp = {"acc": None, "racc": None}
                        nk = len(kts)
                        for ki, kt in enumerate(kts):
                            ks = slice(kt * 128, (kt + 1) * 128)
                            first, last = (ki == 0), (ki == nk - 1)
                            pre = []
                            if hh == 0 and qc == 0 and ki == 0:
                                if u == 0:
                                    pre.append(lambda u=u: load_unit(u))
                                if mixer == 0:
                                    pre.append(lambda u=u: unit_prep(u))
                            if qc == 0 and ki == 0 and mixer in (0, 1):
                                pre.append(lambda u=u, hh=hh, st=st: head_prep(u, hh, st))
                            if mixer == 0 and qc in (1, 2):
                                if ki == 0:
                                    pre.append(lambda u=u, hh=hh, qc=qc, st=st: sel_prep1(u, hh, qc + 1, st))
                                if ki == nk // 2:
                                    pre.append(lambda u=u, hh=hh, qc=qc, st=st: sel_prep2(u, hh, qc + 1, st))
                            if hh == 0 and qc == 1 and ki == 0 and u + 1 < len(units):
                                pre.append(lambda u=u: load_unit(u + 1))

                            if mixer in (0, 1):
                                def A(pre=pre, grp=grp, st=st, kt=kt, ks=ks, qs=qs, qc=qc, hs=hs, first=first, qt_=qt_, qb_=qb_, kt_=kt_, kb_=kb_,
                                      slope=slope, tk=None):
                                    for f in pre:
                                        f()
                                    if first:
                                        grp["acc"] = PS[5 + cnt.get("acc", 0) % 2]
                                        cnt["acc"] = cnt.get("acc", 0) + 1
                                    bq, bqb, bqm = st["bq"], st["bqb"], st["bqm"]
                                    n = kt // 2
                                    bias2 = None
                                    if mixer == 0:
                                        c0 = max(0, (kt - 4 * qc) * 128)
                                        c1 = 512
                                    else:
                                        ii = kt - 4 * qc + 1
                                        c0 = max(0, 128 * (ii - 1))
                                        c1 = min(512, 128 * (ii + 1))
                                    tk["cs"] = (c0, c1)
                                    if mixer == 0:
                                        mi = kt - 4 * qc
                                        if qc >= 2 and n <= 2 * qc:
                                            if kt % 2 == 0:
                                                thr, thrb = st[("thr", qc)]
                                                pg, pgb = PS[3 + cnt.get("G", 0) % 2]
                                                cnt["G"] = cnt.get("G", 0) + 1
                                                c.mm(pg[:, :], pgb, [(kmrep3[hs, n, :], qt_[hs, qs], [kmrepb, qb_])])
                                                cm, cmb = nxt(comb_ring, "comb")
                                                c.op(dve, "tensor_tensor", cm, pg[:, :], thr, op=ALU.is_ge, reads=[pgb, thrb], writes=[cmb])
                                                c.op(dve, "scalar_tensor_tensor", cm, cm, BIG, st["bqB"], op0=ALU.mult, op1=ALU.add,
                                                     reads=[cmb, bqb], writes=[cmb])
                                                st["comb"] = (cm, cmb)
                                            bias = st["comb"]
                                            if n == 2 * qc:
                                                bias2 = bqm[mi]
                                        else:
                                            bias = bqm[mi] if mi >= 0 else (bq, bqb)
                                    else:
                                        bias = bqm[kt - 4 * qc + 1]
                                    pS, pSb = PS[cnt.get("S", 0) % 3]
                                    cnt["S"] = cnt.get("S", 0) + 1
                                    qs0 = qs.start
                                    c.mm(pS[:, c0:c1], pSb, [(kt_[hs, ks], qt_[hs, qs0 + c0:qs0 + c1], [kb_, qb_])])
                                    tm, tmb = nxt(tmp_ring, "tmp")
                                    c.op(dve, "scalar_tensor_tensor", tm[:, c0:c1], pS[:, c0:c1], 0.125, bias[0][:, c0:c1], op0=ALU.mult, op1=ALU.add,
                                         reads=[pSb, bias[1]], writes=[tmb])
                                    if bias2 is not None and c0 < 256:
                                        c.op(dve, "scalar_tensor_tensor", tm[:, c0:256], pS[:, c0:256], 0.125, bias2[0][:, c0:256], op0=ALU.mult, op1=ALU.add,
                                             reads=[pSb, bias2[1], tmb], writes=[tmb])
                                    Pt, Pb = nxt(P_ring, "P")
                                    c.op(act, "activation", Pt[:, c0:c1], tm[:, c0:c1], AF.Exp, bias=float(-slope * (qc * 512 - kt * 128)), scale=1.0,
                                         reads=[tmb], writes=[Pb])
                                    tk["P"] = (Pt, Pb)

                                def B(grp=grp, kt=kt, hh=hh, h=h, hs=hs, qs=qs, first=first, last=last, va4=va4, vb_=vb_, ot_=ot_, ob_=ob_,
                                      hp=hp, ts0=ts0, qc=qc, tk=None):
                                    acc, accb = grp["acc"]
                                    Pt, Pb = tk["P"]
                                    c0, c1 = tk["cs"]
                                    c.mm(acc[:, c0:c1], accb, [(va4[:, kt, hh, :], Pt[:, c0:c1], [vb_, Pb])], start=first, stop=last)
                                    if last:
                                        rc, rcb = nxt(rec_ring, "rec")
                                        if mixer == 1:
                                            c.op(act, "activation", rc[64:128, :], acc[64:128, :], AF.Ln, bias=esk[64:128, h:h + 1], scale=1.0,
                                                 reads=[accb, eskb], writes=[rcb])
                                        else:
                                            c.op(act, "activation", rc[64:128, :], acc[64:128, :], AF.Ln, reads=[accb], writes=[rcb])
                                        c.op(act, "activation", rc[64:128, :], rc[64:128, :], AF.Exp, scale=-1.0, reads=[rcb], writes=[rcb])
                                        c.op(dve, "tensor_tensor", ot_[hs, qs], acc[0:64, :], rc[64:128, :], op=ALU.mult, reads=[accb, rcb], writes=[ob_])
                                        if hh == 1 and qc == 3:
                                            c.dma(OT[hp * 128:(hp + 1) * 128, ts0:ts0 + SEQ], ot_.bitcast(F32), reads=[ob_], writes=[dOT])
                                tk = {}
                                utasks.append((lambda A=A, tk=tk: A(tk=tk), None, lambda B=B, tk=tk: B(tk=tk)))
                            else:
                                diag = kt >= 4 * qc
                                base = qc * 512 - kt * 128

                                def A1(pre=pre, grp=grp, kt=kt, ks=ks, qs=qs, hs=hs, first=first, last=last, diag=diag, base=base,
                                       qt_=qt_, qb_=qb_, kt_=kt_, kb_=kb_, tk=None):
                                    for f in pre:
                                        f()
                                    if first:
                                        grp["acc"] = PS[5 + cnt.get("acc", 0) % 2]
                                        cnt["acc"] = cnt.get("acc", 0) + 1
                                        grp["racc"] = None
                                    pS, pSb = PS[cnt.get("S", 0) % 3]
                                    cnt["S"] = cnt.get("S", 0) + 1
                                    c.mm(pS[:, :], pSb, [(kt_[hs, ks], qt_[hs, qs], [kb_, qb_])])
                                    et, etb = nxt(e_ring, "e")
                                    c.op(act, "activation", et, pS[:, :], AF.Exp, scale=0.125, reads=[pSb], writes=[etb])
                                    spt, spb = nxt(sp_ring, "sp")
                                    c.op(act, "activation", spt, et, AF.Ln, bias=1.0, scale=1.0, reads=[etb], writes=[spb])
                                    if diag:
                                        c.op(pool, "affine_select", spt, spt, pattern=[[1, 512]], compare_op=ALU.is_gt, fill=0.0,
                                             base=base, channel_multiplier=-1, reads=[spb], writes=[spb])
                                    tk["S"] = (pS, pSb)
                                    tk["sp"] = (spt, spb)
                                    tk["rprev"] = grp["racc"]
                                    if not last:
                                        rn, rnb = nxt(racc_ring, "racc")
                                        if grp["racc"] is None:
                                            c.op(pool, "tensor_copy", rn, spt, reads=[spb], writes=[rnb])
                                        else:
                                            c.op(pool, "tensor_tensor", rn, grp["racc"][0].bitcast(F32), spt.bitcast(F32), op=ALU.add,
                                                 reads=[grp["racc"][1], spb], writes=[rnb])
                                        grp["racc"] = (rn, rnb)

                                def A2(diag=diag, base=base, tk=None):
                                    pS, pSb = tk["S"]
                                    spt, spb = tk["sp"]
                                    pa, pab = PS[3 + cnt.get("pa", 0) % 2]
                                    cnt["pa"] = cnt.get("pa", 0) + 1
                                    parts = [(ustr_r, spt, [spb, b_const])]
                                    if tk["rprev"] is not None:
                                        parts.append((ones_r, tk["rprev"][0], [tk["rprev"][1], b_const]))
                                    c.mm(pa[:, :], pab, parts)
                                    tm, tmb = nxt(tmp_ring, "tmp")
                                    c.op(dve, "scalar_tensor_tensor", tm, pS[:, :], 0.125, spt.bitcast(F32), op0=ALU.mult, op1=ALU.subtract,
                                         reads=[pSb, spb], writes=[tmb])
                                    t3, t3b = nxt(t3_ring, "t3")
                                    c.op(dve, "tensor_tensor", t3, tm, pa[:, :], op=ALU.subtract, reads=[tmb, pab], writes=[t3b])
                                    Pt, Pb = nxt(P_ring, "P")
                                    c.op(act, "activation", Pt, t3, AF.Exp, reads=[t3b], writes=[Pb])
                                    if diag:
                                        c.op(pool, "affine_select", Pt, Pt, pattern=[[1, 512]], compare_op=ALU.is_gt, fill=0.0,
                                             base=base, channel_multiplier=-1, reads=[Pb], writes=[Pb])
                                    tk["P"] = (Pt, Pb)

                                def B(grp=grp, kt=kt, hh=hh, hs=hs, qs=qs, first=first, last=last, va4=va4, vb_=vb_, ot_=ot_, ob_=ob_,
                                      hp=hp, ts0=ts0, qc=qc, tk=None):
                                    acc, accb = grp["acc"]
                                    Pt, Pb = tk["P"]
                                    c.mm(acc[:, :], accb, [(va4[:, kt, hh, :], Pt, [vb_, Pb])], start=first, stop=last)
                                    if last:
                                        c.op(act, "copy", ot_[hs, qs], acc[0:64, :], reads=[accb], writes=[ob_])
                                        if hh == 1 and qc == 3:
                                            c.dma(OT[hp * 128:(hp + 1) * 128, ts0:ts0 + SEQ], ot_.bitcast(F32), reads=[ob_], writes=[dOT])
                                tk = {}
                                utasks.append((lambda A1=A1, tk=tk: A1(tk=tk), lambda A2=A2, tk=tk: A2(tk=tk), lambda B=B, tk=tk: B(tk=tk)))
                tasks.extend(utasks)
            lags = (0, 0, LA) if mixer in (0, 1) else (0, 1, 3)
            for t in range(len(tasks) + lags[-1]):
                for k in range(3):
                    i = t - lags[k]
                    if 0 <= i < len(tasks) and tasks[i][k] is not None:
                        tasks[i][k]()
            if debug and ("OT%d" % L) in dbg:
                c.dma(dbg["OT%d" % L], OT, reads=[dOT], writes=[dIN])

            c.phase_reset(KEEP)
            Wot, Wob = c.alloc(8 * D, MMDT)
            Wo3 = Wot.rearrange("p (c n) -> p c n", n=D)
            Wo_pc = Wo.rearrange("(p c) n -> p c n", c=8)
            for cc in range(8):
                c.dma(Wo3[:, cc, :], Wo_pc[:, cc, :], reads=[dIN], writes=[Wob], q=wq)
            Wr, Wrb = c.alloc(8 * 36)
            Wr3 = Wr.rearrange("p (c n) -> p c n", n=36)
            c.dma(Wr3, w_rt[L].rearrange("(p c) n -> p c n", c=8), reads=[dIN], writes=[Wrb])
            gB, gbuf = c.alloc(D)
            bB, _ = c.alloc(D)
            brt, _ = c.alloc(36)
            c.dma(gB, ln1_g[L:L + 1, :].broadcast_to([128, D]), reads=[dIN], writes=[gbuf])
            c.dma(bB, ln1_b[L:L + 1, :].broadcast_to([128, D]), reads=[dIN], writes=[gbuf])
            c.dma(brt, b_rt[L:L + 1, :].broadcast_to([128, 36]), reads=[dIN], writes=[gbuf])
            c.op(dve, "memset", cnt_b, 0.0, writes=[b_cnt])
            otb_ring = [c.alloc(8 * 128, MMDT) for _ in range(3)]
            xr_ring = [c.alloc(D) for _ in range(4)]
            x1_ring = [c.alloc(D) for _ in range(6)]
            x1T_ring = [c.alloc(8 * 128) for _ in range(2)]
            sc_ring = [{"st": c.alloc(12), "mv": c.alloc(4)} for _ in range(4)]
            rs_ring = [c.alloc(320) for _ in range(4)]
            for (rs, rsb) in rs_ring:
                c.op(dve, "memset", rs[:, 4:8], -BIG, writes=[rsb])

            def ln_a(y, yb, sc):
                st, stb = sc["st"]
                mv, mvb = sc["mv"]
                c.op(dve, "bn_stats", st[:, 0:6], y[:, 0:512], reads=[yb], writes=[stb])
                c.op(dve, "bn_stats", st[:, 6:12], y[:, 512:1024], reads=[yb], writes=[stb])
                c.op(dve, "bn_aggr", mv[:, 0:2], st[:, 0:12], reads=[stb], writes=[mvb])
                c.op(dve, "tensor_scalar", mv[:, 2:3], mv[:, 1:2], EPS, None, op0=ALU.add, reads=[mvb], writes=[mvb])
                c.op(act, "activation", mv[:, 2:3], mv[:, 2:3], AF.Sqrt, reads=[mvb], writes=[mvb])

            def ln_b(y, yb, sc):
                mv, mvb = sc["mv"]
                c.op(dve, "reciprocal", mv[:, 2:3], mv[:, 2:3], reads=[mvb], writes=[mvb])
                c.op(dve, "scalar_tensor_tensor", mv[:, 3:4], mv[:, 0:1], -1.0, mv[:, 2:3], op0=ALU.mult, op1=ALU.mult,
                     reads=[mvb], writes=[mvb])
                c.op(act, "activation", y, y, AF.Identity, bias=mv[:, 3:4], scale=mv[:, 2:3], reads=[yb, mvb], writes=[yb])

            def ln_c(y, yb, gB_, bB_, gbuf_, outt, outb):
                c.op(dve, "tensor_tensor", y, y, gB_, op=ALU.mult, reads=[yb, gbuf_], writes=[yb])
                c.op(pool, "tensor_tensor", outt, y, bB_, op=ALU.add, reads=[yb, gbuf_], writes=[outb])

            def p3_stages(tt):
                r0 = tt * 128
                otb, otbb = otb_ring[tt % 3]
                otb3 = otb.rearrange("p (c t) -> p c t", t=128)
                xr, xrb = xr_ring[tt % 4]
                y, yb = xr, xrb
                x1, x1b = x1_ring[tt % 6]
                x1T, x1Tb = x1T_ring[tt % 2]
                x1T3 = x1T.rearrange("p (c t) -> p c t", t=128)
                sc = sc_ring[tt % 4]
                rs, rsb = rs_ring[tt % 4]
                o = 40
                gm8 = rs[:, o:o + 8]; o += 8
                ngm = rs[:, o:o + 1]; o += 1
                ge = rs[:, o:o + 4]; o += 4
                gsum = rs[:, o:o + 1]; o += 1
                gp = rs[:, o:o + 1]; o += 1
                goh = rs[:, o:o + 4]; o += 4
                ml = rs[:, o:o + 32]; o += 32
                m8 = rs[:, o:o + 8]; o += 8
                sel = rs[:, o:o + 32]; o += 32
                sm = rs[:, o:o + 8]; o += 8
                ex = rs[:, o:o + 32]; o += 32
                wgt = rs[:, o:o + 32]; o += 32
                pos = rs[:, o:o + 32]; o += 32
                oh1 = rs[:, o:o + 32]; o += 32
                assert o <= 320
                R = dict(reads=[rsb], writes=[rsb])
                pl, plb = PS[4 + tt % 2]
                pp, ppb = PS[6 + tt % 2]

                def s0():
                    c.dma(otb3, OT[:, r0:r0 + 128].rearrange("(p c) t -> p c t", c=8), reads=[dOT], writes=[otbb], q=wq)
                    c.dma(xr, X[r0:r0 + 128, :], reads=[dX], writes=[xrb])

                def s1a():
                    for half in range(2):
                        pt, pb = PS[half]
                        c.mm(pt[:, :], pb, [(otb3[:, cc, :], Wo3[:, cc, half * 512:(half + 1) * 512], [otbb, Wob]) for cc in range(8)])
                        c.op(dve, "scalar_tensor_tensor", y[:, half * 512:(half + 1) * 512], xr[:, half * 512:(half + 1) * 512], ALPHA, pt[:, :],
                             op0=ALU.mult, op1=ALU.add, reads=[xrb, pb], writes=[yb])
                    ln_a(y, yb, sc)

                def s1b():
                    ln_b(y, yb, sc)

                def s1c():
                    ln_c(y, yb, gB, bB, gbuf, x1, x1b)
                    c.dma(X1[r0:r0 + 128, :], x1, reads=[x1b], writes=[dX1])

                def s2():
                    for half in range(2):
                        pt, pb = PS[2 + half]
                        for cc in range(4):
                            c4 = half * 4 + cc
                            c.transpose(pt[:, cc * 128:(cc + 1) * 128], pb, x1.rearrange("p (q c) -> p c q", c=8)[:, c4, :], [x1b], ident, signal=(cc == 3))
                        evac(x1T[:, half * 512:(half + 1) * 512], x1Tb, pt[:, :], pb)

                def s2b():
                    c.mm(pl[:, 0:36], plb, [(x1T3[:, cc, :], Wr3[:, cc, :], [x1Tb, Wrb]) for cc in range(8)])

                def s3():
                    yield
                    c.op(dve, "tensor_tensor", rs[:, 0:4], pl[:, 0:4], brt[:, 0:4], op=ALU.add, reads=[plb, gbuf], writes=[rsb])
                    yield
                    c.op(dve, "tensor_tensor", rs[:, 8:40], pl[:, 4:36], brt[:, 4:36], op=ALU.add, reads=[plb, gbuf], writes=[rsb])
                    yield
                    c.op(dve, "max", gm8, rs[:, 0:8], **R)
                    yield
                    c.op(dve, "tensor_scalar", ngm, gm8[:, 0:1], -1.0, None, op0=ALU.mult, **R)
                    yield
                    c.op(dve, "tensor_scalar", goh, rs[:, 0:4], gm8[:, 0:1], None, op0=ALU.is_ge, **R)
                    yield
                    c.op(dve, "tensor_scalar", goh, goh, 1.0, BIG, op0=ALU.subtract, op1=ALU.mult, **R)
                    yield
                    c.op(dve, "tensor_tensor", ml.rearrange("p (g e) -> p g e", e=8), rs[:, 8:40].rearrange("p (g e) -> p g e", e=8),
                         goh.unsqueeze(2).broadcast_to([128, 4, 8]), op=ALU.add, **R)
                    yield
                    c.op(dve, "max", m8, ml, **R)
                    yield
                    c.op(dve, "tensor_scalar", sel, ml, m8[:, 1:2], None, op0=ALU.is_ge, **R)
                    yield
                    c.op(dve, "tensor_scalar", oh1, ml, m8[:, 0:1], None, op0=ALU.is_ge, **R)
                    yield
                    c.op(dve, "tensor_tensor", sm[:, 0:1], m8[:, 1:2], m8[:, 0:1], op=ALU.subtract, **R)
                    yield
                    c.op(dve, "tensor_scalar", sm[:, 1:2], m8[:, 0:1], -1.0, None, op0=ALU.mult, **R)
                    yield
                    c.op(act, "activation", ge, rs[:, 0:4], AF.Exp, bias=ngm, scale=1.0, accum_out=gsum, **R)
                    yield
                    c.op(act, "activation", sm[:, 2:3], sm[:, 0:1], AF.Exp, **R)
                    yield
                    c.op(act, "activation", ex, ml, AF.Exp, bias=sm[:, 1:2], scale=1.0, **R)
                    yield

                def s4pre():
                    c.mm(pp[:, 0:32], ppb, [(tri_f, sel, [b_const, rsb])])
                    c.mm(pp[:, 32:64], ppb, [(ones_f, sel, [b_const, rsb])])

                def s4():
                    c.op(dve, "reciprocal", gp, gsum, **R)
                    yield
                    c.op(dve, "tensor_scalar", sm[:, 3:4], sm[:, 2:3], 1.0, None, op0=ALU.add, **R)
                    yield
                    c.op(dve, "reciprocal", sm[:, 3:4], sm[:, 3:4], **R)
                    yield
                    c.op(dve, "tensor_tensor", sm[:, 4:5], sm[:, 3:4], gp, op=ALU.mult, **R)
                    yield
                    c.op(dve, "scalar_tensor_tensor", wgt, ex, sm[:, 4:5], sel, op0=ALU.mult, op1=ALU.mult, **R)
                    yield
                    c.op(dve, "tensor_tensor", pos, pp[:, 0:32], cnt_b, op=ALU.add, reads=[ppb, b_cnt, rsb], writes=[rsb])
                    yield
                    c.op(dve, "tensor_tensor", cnt_b, cnt_b, pp[:, 32:64], op=ALU.add, reads=[ppb, b_cnt], writes=[b_cnt])
                    yield
                    c.op(dve, "scalar_tensor_tensor", pos, pos, float(CAP - 1), eoff, op0=ALU.min, op1=ALU.add, reads=[rsb, b_const], writes=[rsb])
                    yield
                    c.op(dve, "tensor_tensor", sel, sel, oh1, op=ALU.subtract, **R)
                    yield
                    c.op(dve, "tensor_tensor", ex, oh1, pos, op=ALU.mult, **R)
                    yield
                    c.op(dve, "tensor_reduce", rt3[:, tt, 0:1], ex, axis=AX.X, op=ALU.add, reads=[rsb], writes=[b_rtab])
                    yield
                    c.op(dve, "tensor_tensor", ex, sel, pos, op=ALU.mult, **R)
                    yield
                    c.op(dve, "tensor_reduce", rt3[:, tt, 1:2], ex, axis=AX.X, op=ALU.add, reads=[rsb], writes=[b_rtab])
                    yield
                    c.op(dve, "tensor_tensor", ex, oh1, wgt, op=ALU.mult, **R)
                    yield
                    c.op(dve, "tensor_reduce", rt3[:, tt, 2:3], ex, axis=AX.X, op=ALU.add, reads=[rsb], writes=[b_rtab])
                    yield
                    c.op(dve, "tensor_tensor", ex, sel, wgt, op=ALU.mult, **R)
                    yield
                    c.op(dve, "tensor_reduce", rt3[:, tt, 3:4], ex, axis=AX.X, op=ALU.add, reads=[rsb], writes=[b_rtab])
                    yield
                    c.op(dve, "tensor_copy", ridx3[:, tt, :], rt3[:, tt, 0:2], reads=[b_rtab], writes=[b_ridx])
                    yield

                def s5():
                    for k in range(2):
                        c.dma(XS[:, :], x1, reads=[x1b, b_ridx], writes=[dXS], q=pool,
                              indirect=dict(out_offset=bass.IndirectOffsetOnAxis(ap=ridx3[:, tt, k:k + 1], axis=0), in_offset=None,
                                            bounds_check=bc_reg, oob_is_err=False))
                return dict(s0=s0, s1a=s1a, s1b=s1b, s1c=s1c, s2a=s2, s2b=s2b, s3=s3, s4pre=s4pre, s4=s4, s5=s5)

            p3t = [p3_stages(tt) for tt in range(T // 128)]
            lag3 = dict(s0=0, s1a=1, s1b=2, s1c=3, s2a=4, s2b=4, s3=5, s4pre=6, s4=6, s5=7)
            n3 = len(p3t)

            def run3(name, t):
                i = t - lag3[name]
                if 0 <= i < n3:
                    return p3t[i][name]()
                return None
            for t in range(n3 + 7):
                for nm_ in ("s1b", "s1c", "s4pre", "s2a", "s1a"):
                    run3(nm_, t)
                gens = [g for g in (run3("s4", t), run3("s3", t)) if g is not None]
                while gens:
                    for g in list(gens):
                        try:
                            next(g)
                        except StopIteration:
                            gens.remove(g)
                for nm_ in ("s2b", "s5", "s0"):
                    run3(nm_, t)
            if debug and ("X1_%d" % L) in dbg:
                c.dma(dbg["X1_%d" % L], X1, reads=[dX1], writes=[dIN])
            if debug and ("rt%d" % L) in dbg:
                c.dma(dbg["rt%d" % L], rt, reads=[b_rtab], writes=[dIN])

            c.phase_reset(KEEP)
            w_ring = []
            for _ in range(2):
                w_ring.append((c.alloc(8 * DEXP, MMDT), c.alloc(8 * DEXP, MMDT), c.alloc(4 * D, MMDT)))
            xs_ring = [c.alloc(3 * D) for _ in range(2)]
            xsT_ring = [c.alloc(8 * CAP, MMDT) for _ in range(2)]
            hT_ring = [c.alloc(4 * CAP, MMDT) for _ in range(2)]
            sg_ring = [c.alloc(CAP) for _ in range(2)]
            ys_ring = [c.alloc(D) for _ in range(3)]
            NJ = CAP // 128
            ysi = 0
            sgi = 0

            def load_w(e):
                (wg, wgb), (wu, wub), (wd, wdb) = w_ring[e % 2]
                wg3 = wg.rearrange("p (c n) -> p c n", n=DEXP)
                wu3 = wu.rearrange("p (c n) -> p c n", n=DEXP)
                wd3 = wd.rearrange("p (c n) -> p c n", n=D)
                c.dma(wg, w_e_gate[L, e].rearrange("(p c) f -> p (c f)", c=8), reads=[dIN], writes=[wgb], q=wq)
                c.dma(wu, w_e_up[L, e].rearrange("(p c) f -> p (c f)", c=8), reads=[dIN], writes=[wub], q=wq)
                c.dma(wd, w_e_down[L, e].rearrange("(p c) n -> p (c n)", c=4), reads=[dIN], writes=[wdb], q=wq)

            def load_xs(e):
                xs, xsb = xs_ring[e % 2]
                c.dma(xs.rearrange("p (j d) -> p j d", d=D), XS[e * CAP:(e + 1) * CAP, :].rearrange("(j p) d -> p j d", p=128),
                      reads=[dXS], writes=[xsb])

            load_w(0)
            load_xs(0)
            for e in range(NEXP):
                if e + 1 < NEXP:
                    load_w(e + 1)
                    load_xs(e + 1)
                (wg, wgb), (wu, wub), (wd, wdb) = w_ring[e % 2]
                wg3 = wg.rearrange("p (c n) -> p c n", n=DEXP)
                wu3 = wu.rearrange("p (c n) -> p c n", n=DEXP)
                wd3 = wd.rearrange("p (c n) -> p c n", n=D)
                xs, xsb = xs_ring[e % 2]
                xs3 = xs.rearrange("p (j d) -> p j d", d=D)
                xs4 = xs.rearrange("p (j q c) -> p j c q", q=128, c=8)
                wg4 = wg.rearrange("p (c m k) -> p c k m", c=8, m=128, k=4)
                wu4 = wu.rearrange("p (c m k) -> p c k m", c=8, m=128, k=4)
                xsT, xsTb = xsT_ring[e % 2]
                xsT3 = xsT.rearrange("p (c t) -> p c t", t=CAP)
                hT, hTb = hT_ring[e % 2]
                hT3 = hT.rearrange("p (c t) -> p c t", t=CAP)
                for cc in range(8):
                    pt, pb = PS[cc % 2]
                    for jj in range(NJ):
                        c.transpose(pt[:, jj * 128:(jj + 1) * 128], pb, xs4[:, jj, cc, :], [xsb], ident, signal=(jj == NJ - 1))
                    evac(xsT3[:, cc, :], xsTb, pt[:, 0:CAP], pb)
                for fc in range(4):
                    pg, pgb = PS[2 + fc % 2]
                    pu, pub = PS[4 + fc % 2]
                    c.mm(pg[:, 0:CAP], pgb, [(wg4[:, cc, fc, :], xsT3[:, cc, :], [wgb, xsTb]) for cc in range(8)])
                    c.mm(pu[:, 0:CAP], pub, [(wu4[:, cc, fc, :], xsT3[:, cc, :], [wub, xsTb]) for cc in range(8)])
                    sg, sgb = sg_ring[sgi % 2]; sgi += 1
                    c.op(act, "activation", sg, pg[:, 0:CAP], AF.Silu, reads=[pgb], writes=[sgb])
                    c.op(dve, "tensor_tensor", hT3[:, fc, :], sg, pu[:, 0:CAP], op=ALU.mult, reads=[sgb, pub], writes=[hTb])
                for jj in range(NJ):
                    ys, ysb = ys_ring[ysi % 3]; ysi += 1
                    for half in range(2):
                        pt, pb = PS[6 + half]
                        c.mm(pt[:, :], pb, [(hT3[:, fc, jj * 128:(jj + 1) * 128], wd3[:, fc, half * 512:(half + 1) * 512], [hTb, wdb]) for fc in range(4)])
                        evac(ys[:, half * 512:(half + 1) * 512], ysb, pt[:, :], pb)
                    r0 = e * CAP + jj * 128
                    c.dma(YS[r0:r0 + 128, :], ys, reads=[ysb], writes=[dYS])

            c.phase_reset(KEEP)
            Wg_, Wgb_ = c.alloc(8 * D, MMDT)
            Wg3 = Wg_.rearrange("p (c n) -> p c n", n=D)
            Wg_pc = w_ple_gate[L].rearrange("(p c) n -> p c n", c=8)
            for cc in range(8):
                c.dma(Wg3[:, cc, :], Wg_pc[:, cc, :], reads=[dIN], writes=[Wgb_], q=wq)
            Wp_, Wpb_ = c.alloc(2 * D, MMDT)
            Wp3 = Wp_.rearrange("p (c n) -> p c n", n=D)
            c.dma(Wp_, w_ple_proj[L].rearrange("(p c) n -> p (c n)", c=2), reads=[dIN], writes=[Wpb_], q=wq)
            gB, gbuf = c.alloc(D)
            bB, _ = c.alloc(D)
            c.dma(gB, ln2_g[L:L + 1, :].broadcast_to([128, D]), reads=[dIN], writes=[gbuf])
            c.dma(bB, ln2_b[L:L + 1, :].broadcast_to([128, D]), reads=[dIN], writes=[gbuf])
            y1_ring = [c.alloc(D) for _ in range(4)]
            y2_ring = [c.alloc(D) for _ in range(2)]
            x1_ring = [c.alloc(D) for _ in range(2)]
            pr_ring = [c.alloc(PLE) for _ in range(6)]
            x2_ring = [c.alloc(D) for _ in range(3)]
            x2T_ring = [c.alloc(8 * 128, MMDT) for _ in range(2)]
            pT_ring = [c.alloc(2 * 128, MMDT) for _ in range(2)]
            sig_ring = [c.alloc(D) for _ in range(2)]
            sc_ring = [{"st": c.alloc(12), "mv": c.alloc(4)} for _ in range(4)]
            dst = out if L == DEPTH - 1 else X
            dstb = dIN if L == DEPTH - 1 else dX

            def p5_stages(tt):
                r0 = tt * 128
                y1, y1b = y1_ring[tt % 4]; y2, y2b = y2_ring[tt % 2]; x1, x1b = x1_ring[tt % 2]; pr, prb = pr_ring[tt % 6]
                y, yb = y1, y1b
                x2, x2b = x2_ring[tt % 3]; x2T, x2Tb = x2T_ring[tt % 2]; pT, pTb = pT_ring[tt % 2]
                sig, sigb = sig_ring[tt % 2]; sc = sc_ring[tt % 4]
                x2T3 = x2T.rearrange("p (c t) -> p c t", t=128)
                pT3 = pT.rearrange("p (c t) -> p c t", t=128)

                def s0():
                    for k, (yy, yyb) in enumerate(((y1, y1b), (y2, y2b))):
                        c.dma(yy, YS[:, :], reads=[dYS, b_ridx], writes=[yyb], q=pool,
                              indirect=dict(out_offset=None, in_offset=bass.IndirectOffsetOnAxis(ap=ridx3[:, tt, k:k + 1], axis=0),
                                            bounds_check=bc_reg, oob_is_err=False))
                    c.dma(x1, X1[r0:r0 + 128, :], reads=[dX1], writes=[x1b])
                    c.dma(pr, p_in[L, r0:r0 + 128, :], reads=[dIN], writes=[prb])

                def s1a():
                    c.op(dve, "tensor_scalar", y2, y2, rt3[:, tt, 3:4], None, op0=ALU.mult, reads=[y2b, b_rtab], writes=[y2b])
                    c.op(dve, "scalar_tensor_tensor", y1, y1, rt3[:, tt, 2:3], y2, op0=ALU.mult, op1=ALU.add, reads=[y1b, y2b, b_rtab], writes=[y1b])
                    c.op(dve, "scalar_tensor_tensor", y, x1, ALPHA, y1, op0=ALU.mult, op1=ALU.add, reads=[x1b, y1b], writes=[yb])
                    ln_a(y, yb, sc)

                def s1b():
                    ln_b(y, yb, sc)

                def s1c():
                    ln_c(y, yb, gB, bB, gbuf, x2, x2b)

                def s2():
                    for half in range(2):
                        pt, pb = PS[half]
                        for cc in range(4):
                            c4 = half * 4 + cc
                            c.transpose(pt[:, cc * 128:(cc + 1) * 128], pb, x2.rearrange("p (q c) -> p c q", c=8)[:, c4, :], [x2b], ident, signal=(cc == 3))
                        evac(x2T[:, half * 512:(half + 1) * 512], x2Tb, pt[:, :], pb)
                    pt, pb = PS[2]
                    for cc in range(2):
                        c.transpose(pt[:, cc * 128:(cc + 1) * 128], pb, pr.rearrange("p (q c) -> p c q", c=2)[:, cc, :], [prb], ident, signal=(cc == 1))
                    evac(pT, pTb, pt[:, 0:256], pb)

                def s3a():
                    for half in range(2):
                        hsl = slice(half * 512, (half + 1) * 512)
                        pg, pgb = PS[4 + half]
                        c.mm(pg[:, :], pgb, [(x2T3[:, cc, :], Wg3[:, cc, hsl], [x2Tb, Wgb_]) for cc in range(8)])
                        c.op(act, "activation", sig[:, hsl], pg[:, :], AF.Sigmoid, reads=[pgb], writes=[sigb])
                        pq, pqb = PS[6 + half]
                        c.mm(pq[:, :], pqb, [(pT3[:, cc, :], Wp3[:, cc, hsl], [pTb, Wpb_]) for cc in range(2)])

                def s3b():
                    for half in range(2):
                        hsl = slice(half * 512, (half + 1) * 512)
                        pq, pqb = PS[6 + half]
                        c.op(dve, "tensor_tensor", sig[:, hsl], sig[:, hsl], pq[:, :], op=ALU.mult, reads=[sigb, pqb], writes=[sigb])
                    c.op(pool, "tensor_tensor", sig, sig, x2, op=ALU.add, reads=[sigb, x2b], writes=[sigb])
                    c.dma(dst[r0:r0 + 128, :], sig, reads=[sigb], writes=[dstb])
                return (s0, s1a, s1b, s1c, s2, s3a, s3b)

            p5t = [p5_stages(tt) for tt in range(T // 128)]
            nst = 7
            for t in range(len(p5t) + nst - 1):
                for k in (6, 2, 3, 4, 5, 1, 0):
                    i = t - k
                    if 0 <= i < len(p5t):
                        p5t[i][k]()
            if debug and ("X3_%d" % L) in dbg:
                c.barrier()
                c.dma(dbg["X3_%d" % L], dst, reads=[dstb], writes=[dIN])

        if nlayers < DEPTH:
            c.barrier()
            c.dma(out, X, reads=[dX], writes=[dIN])
        c.barrier()
        if c.future:
            print('FUTURE WAITS', len(c.future), c.future[:10])
    return nc


def _consts():
    cst = np.zeros((128, 6, 128), np.float32)
    k = np.arange(128)[:, None]
    m = np.arange(128)[None, :]
    cst[:, 0, :] = (k == m)
    cst[:, 1, :] = 1.0
    cst[:, 2, :] = (k < m)
    cst[:, 3, :] = (k > m)
    q = np.arange(512)[None, :]
    kk = np.arange(128)[:, None]
    cmask = np.zeros((9, 128, 512), np.float32)
    for i in range(4):
        cmask[i] = np.where((-128 * i + q - kk) >= 0, 0.0, -BIG)
    for i in range(5):
        dlt = 128 - 128 * i
        v = dlt + q - kk
        cmask[4 + i] = np.where((v >= 0) & (v < 128), 0.0, -BIG)
    cbase = (-(q - kk)).astype(np.float32)
    ceoff = np.tile((np.arange(NEXP) * CAP).astype(np.float32)[None, :], (128, 1))
    return cst, cmask, np.ascontiguousarray(cbase), np.ascontiguousarray(ceoff)


_NC_CACHE = {}


def kernel(**inputs):
    f = lambda a: np.ascontiguousarray(np.asarray(a, dtype=np.float32))
    x = f(inputs["x"]).reshape(NCORES, T, D)
    p = f(inputs["p"]).reshape(DEPTH, NCORES, T, PLE)
    cst, cmask, cbase, ceoff = _consts()
    w_rt = np.ascontiguousarray(np.concatenate([f(inputs["w_grp"]), f(inputs["w_exp"])], axis=2))
    b_rt = np.ascontiguousarray(np.concatenate([f(inputs["b_grp"]), f(inputs["b_exp"])], axis=1))
    shared = {k: f(inputs[k]) for k in ("w_qkv_a", "w_o_a", "w_qkv_b", "w_o_b", "sinks_b", "w_qkv_c", "w_o_c", "ln1_g", "ln1_b",
                                         "ln2_g", "ln2_b", "w_e_gate", "w_e_up", "w_e_down", "w_ple_gate", "w_ple_proj")}
    shared.update(w_rt=w_rt, b_rt=b_rt, cst=cst, cmask=cmask, cbase=cbase, ceoff=ceoff)
    if "nc" not in _NC_CACHE:
        _NC_CACHE["nc"] = build()
    nc = _NC_CACHE["nc"]
    in_maps = []
    for i in range(NCORES):
        m = dict(shared)
        m["x"] = x[i]
        m["p"] = np.ascontiguousarray(p[:, i])
        in_maps.append(m)
    res = run_bass_kernel_spmd(nc, in_maps, core_ids=list(range(NCORES)))
    o = np.stack([np.asarray(r["out"]) for r in res.results], axis=0)
    return o.reshape(16, SEQ, D).astype(np.float32)
```

```python
import numpy as np
import concourse.bass as bass
import concourse.mybir as mybir
from concourse.bass_utils import run_bass_kernel_spmd

F32 = mybir.dt.float32
F32R = mybir.dt.float32r
I32 = mybir.dt.int32
ALU = mybir.AluOpType
AF = mybir.ActivationFunctionType
AX = mybir.AxisListType

USE_F32R = True
MMDT = F32R if USE_F32R else F32

NCORES = 8
D = 1024
SEQ = 2048
NSEQ = 2
T = NSEQ * SEQ
DEPTH = 4
NH = 16
HD = 64
NEXP = 32
DEXP = 512
CAP = 384
PLE = 256
ALPHA = (2.0 * DEPTH) ** 0.25
EPS = 1e-5
BIG = 30000.0
SLOPES = [2.0 ** (-8.0 * (h + 1) / NH) for h in range(NH)]
XS_ROWS = NEXP * CAP + 128


class Eng:
    def __init__(self, name, h, sem):
        self.name, self.h, self.sem = name, h, sem
        self.count = 0
        self.known = {}


class Buf:
    __slots__ = ("name", "writers", "readers")

    def __init__(self, name):
        self.name = name
        self.writers = {}
        self.readers = {}


class Ctx:
    def __init__(self, nc, es):
        self.nc = nc
        self.es = es
        mk = lambda n, h: Eng(n, h, es.enter_context(nc.semaphore("s_" + n)))
        self.pe = mk("pe", nc.tensor)
        self.act = mk("act", nc.scalar)
        self.dve = mk("dve", nc.vector)
        self.pool = mk("pool", nc.gpsimd)
        self.sp = mk("sp", nc.sync)
        self.engs = [self.pe, self.act, self.dve, self.pool, self.sp]
        self.sems = {e.name: e.sem for e in self.engs}
        self.ndma = 40
        self.dsem = []
        for i in range(self.ndma):
            s = es.enter_context(nc.semaphore("s_dma%d" % i))
            self.dsem.append(s)
            self.sems["d%d" % i] = s
        self.dval = [0] * self.ndma
        self.dnext = 0
        self.FW, self.RW = 19100, 34048
        self.arena = es.enter_context(nc.sbuf_tensor("arena_f", [128, self.FW], F32))
        self.arena_r = es.enter_context(nc.sbuf_tensor("arena_r", [128, self.RW], MMDT))
        self.atop = 0
        self.rtop = 0
        self.psum = []
        for i in range(8):
            t = es.enter_context(nc.psum_tensor("psb%d" % i, [128, 512], F32))
            self.psum.append((t, Buf("ps%d" % i)))
        self.nbuf = 0
        self.future = []

    def alloc(self, ncols, dt=F32, name=None):
        if dt == MMDT and USE_F32R:
            a = self.arena_r[:, self.rtop:self.rtop + ncols]
            self.rtop += ncols
            assert self.rtop <= self.RW, "SBUF R arena overflow %d" % self.rtop
        else:
            a = self.arena[:, self.atop:self.atop + ncols]
            self.atop += ncols
            assert self.atop <= self.FW, "SBUF F arena overflow %d" % self.atop
            if dt != F32:
                a = a.bitcast(dt)
        self.nbuf += 1
        return a, Buf(name or ("b%d" % self.nbuf))

    def wait(self, E, key, val):
        if E.known.get(key, 0) >= val:
            return
        for F in self.engs:
            if F.name == key and val > F.count:
                self.future.append((E.name, key, val, F.count))
        E.h.wait_ge(self.sems[key], val)
        E.known[key] = val

    def _deps(self, E, reads, writes):
        evs = {}
        for b in reads:
            for k, v in b.writers.items():
                evs[k] = max(evs.get(k, 0), v)
        for b in writes:
            for k, v in b.readers.items():
                if k != E.name:
                    evs[k] = max(evs.get(k, 0), v)
            for k, v in b.writers.items():
                if k != E.name:
                    evs[k] = max(evs.get(k, 0), v)
        for k, v in evs.items():
            self.wait(E, k, v)

    def _record(self, key, val, reads, writes):
        for b in reads:
            b.readers[key] = max(b.readers.get(key, 0), val)
        for b in writes:
            if b.readers:
                b.readers = {}
                b.writers = {key: val}
            else:
                b.writers[key] = max(b.writers.get(key, 0), val)

    def op(self, E, fname, *args, reads=(), writes=(), signal=True, **kw):
        self._deps(E, reads, writes)
        ins = getattr(E.h, fname)(*args, **kw)
        if signal:
            E.count += 1
            ins.then_inc(E.sem, 1)
            self._record(E.name, E.count, reads, writes)
        else:
            self._record(E.name, E.count + 1, reads, writes)
        return ins

    def dma(self, out, in_, reads=(), writes=(), q=None, indirect=None, **kw):
        Q = q or self.sp
        self._deps(Q, reads, writes)
        i = self.dnext
        self.dnext = (self.dnext + 1) % self.ndma
        key = "d%d" % i
        self.wait(Q, key, self.dval[i])
        if indirect is None:
            ins = Q.h.dma_start(out=out, in_=in_, **kw)
        else:
            ins = Q.h.indirect_dma_start(out=out, in_=in_, **indirect)
        self.dval[i] += 16
        ins.then_inc(self.dsem[i], 16)
        self._record(key, self.dval[i], reads, writes)

    def barrier(self):
        for E in self.engs:
            for F in self.engs:
                if F is not E and F.count > 0:
                    self.wait(E, F.name, F.count)
            for i in range(self.ndma):
                if self.dval[i] > 0:
                    self.wait(E, "d%d" % i, self.dval[i])

    def phase_reset(self, keep):
        self.barrier()
        self.atop, self.rtop = keep

    def mm(self, out, outbuf, parts, start=True, stop=True):
        n = len(parts)
        for i, (l, r, rb) in enumerate(parts):
            last = (i == n - 1)
            self.op(self.pe, "matmul", out, l, r, start=(start and i == 0), stop=(stop and last),
                    reads=rb, writes=[outbuf], signal=last)

    def transpose(self, out, outbuf, in_, inbufs, ident, signal=True):
        self.op(self.pe, "transpose", out, in_, ident, reads=inbufs, writes=[outbuf], signal=signal)


def build(nlayers=DEPTH, debug=None):
    from contextlib import ExitStack
    nc = bass.Bass("TRN2", target_bir_lowering=False)
    dr = lambda n, s, kind="ExternalInput", dt=F32: nc.dram_tensor(n, list(s), dt, kind=kind).ap()
    x_in = dr("x", [T, D])
    p_in = dr("p", [DEPTH, T, PLE])
    w_qkv_a = dr("w_qkv_a", [2, D, 3 * D]); w_o_a = dr("w_o_a", [2, D, D])
    w_qkv_b = dr("w_qkv_b", [1, D, D + 512]); w_o_b = dr("w_o_b", [1, D, D])
    sinks_b = dr("sinks_b", [1, NH])
    w_qkv_c = dr("w_qkv_c", [1, D, 3 * D]); w_o_c = dr("w_o_c", [1, D, D])
    ln1_g = dr("ln1_g", [DEPTH, D]); ln1_b = dr("ln1_b", [DEPTH, D])
    ln2_g = dr("ln2_g", [DEPTH, D]); ln2_b = dr("ln2_b", [DEPTH, D])
    w_rt = dr("w_rt", [DEPTH, D, 36]); b_rt = dr("b_rt", [DEPTH, 36])
    w_e_gate = dr("w_e_gate", [DEPTH, NEXP, D, DEXP]); w_e_up = dr("w_e_up", [DEPTH, NEXP, D, DEXP])
    w_e_down = dr("w_e_down", [DEPTH, NEXP, DEXP, D])
    w_ple_gate = dr("w_ple_gate", [DEPTH, D, D]); w_ple_proj = dr("w_ple_proj", [DEPTH, PLE, D])
    cst = dr("cst", [128, 6, 128])
    cmask = dr("cmask", [9, 128, 512])
    cbase = dr("cbase", [128, 512])
    ceoff = dr("ceoff", [128, NEXP])
    out = dr("out", [T, D], kind="ExternalOutput")
    X = dr("Xs", [T, D], kind="Internal"); X1 = dr("X1s", [T, D], kind="Internal")
    QT = dr("QTs", [D, T], kind="Internal"); KT = dr("KTs", [D, T], kind="Internal")
    V = dr("Vs", [T, D], kind="Internal"); OT = dr("OTs", [D, T], kind="Internal")
    XS = dr("XSs", [XS_ROWS, D], kind="Internal"); YS = dr("YSs", [XS_ROWS, D], kind="Internal")
    dbg = {}
    if debug:
        for nme, shp in debug.items():
            dbg[nme] = dr("dbg_" + nme, shp, kind="ExternalOutput")
    dX, dX1, dQT, dKT, dV, dOT, dXS, dYS = [Buf(n) for n in ("X", "X1", "QT", "KT", "V", "OT", "XS", "YS")]
    dIN = Buf("in")

    with ExitStack() as es:
        c = Ctx(nc, es)
        pe, act, dve, pool, sp = c.pe, c.act, c.dve, c.pool, c.sp
        wq = pool if USE_F32R else sp
        PS = c.psum
        bc_reg = nc.gpsimd.to_reg(XS_ROWS - 1)

        ident, b_const = c.alloc(128)
        ones_r, _ = c.alloc(128, MMDT)
        tri_f, _ = c.alloc(128)
        ustr_r, _ = c.alloc(128, MMDT)
        ones_f, _ = c.alloc(128)
        cb_f, _ = c.alloc(512)
        eoff, _ = c.alloc(NEXP)
        rt, b_rtab = c.alloc(32 * 4)
        ridx, b_ridx = c.alloc(32 * 2, I32)
        cnt_b, b_cnt = c.alloc(NEXP)
        c.dma(ident, cst[:, 0, :], writes=[b_const])
        c.dma(ones_f, cst[:, 1, :], writes=[b_const])
        c.dma(tri_f, cst[:, 2, :], writes=[b_const])
        c.dma(ones_r, cst[:, 1, :], writes=[b_const], q=wq)
        c.dma(ustr_r, cst[:, 3, :], writes=[b_const], q=wq)
        c.dma(cb_f, cbase, writes=[b_const])
        c.dma(eoff, ceoff, writes=[b_const])
        KEEP = (c.atop, c.rtop)
        rt3 = rt.rearrange("p (t k) -> p t k", k=4)
        ridx3 = ridx.rearrange("p (t k) -> p t k", k=2)

        for i in range(8):
            c.dma(X[i * 512:(i + 1) * 512, :], x_in[i * 512:(i + 1) * 512, :], reads=[dIN], writes=[dX])

        state = {"ev": 0}

        def evac(out_ap, outb, in_ap, inb, extra_reads=()):
            state["ev"] ^= 1
            if state["ev"]:
                c.op(act, "copy", out_ap, in_ap, reads=[inb, *extra_reads], writes=[outb])
            else:
                c.op(dve, "tensor_copy", out_ap, in_ap, reads=[inb, *extra_reads], writes=[outb])

        def layer_norm(y, yb, gB, bB, gbuf, outt, outb, sc):
            st, stb = sc["st"]
            mv, mvb = sc["mv"]
            c.op(dve, "bn_stats", st[:, 0:6], y[:, 0:512], reads=[yb], writes=[stb])
            c.op(dve, "bn_stats", st[:, 6:12], y[:, 512:1024], reads=[yb], writes=[stb])
            c.op(dve, "bn_aggr", mv[:, 0:2], st[:, 0:12], reads=[stb], writes=[mvb])
            c.op(dve, "tensor_scalar", mv[:, 2:3], mv[:, 1:2], EPS, None, op0=ALU.add, reads=[mvb], writes=[mvb])
            c.op(act, "activation", mv[:, 2:3], mv[:, 2:3], AF.Sqrt, reads=[mvb], writes=[mvb])
            c.op(dve, "reciprocal", mv[:, 2:3], mv[:, 2:3], reads=[mvb], writes=[mvb])
            c.op(dve, "scalar_tensor_tensor", mv[:, 3:4], mv[:, 0:1], -1.0, mv[:, 2:3], op0=ALU.mult, op1=ALU.mult,
                 reads=[mvb], writes=[mvb])
            c.op(act, "activation", y, y, AF.Identity, bias=mv[:, 3:4], scale=mv[:, 2:3], reads=[yb, mvb], writes=[yb])
            c.op(dve, "tensor_tensor", y, y, gB, op=ALU.mult, reads=[yb, gbuf], writes=[yb])
            c.op(pool, "tensor_tensor", outt, y, bB, op=ALU.add, reads=[yb, gbuf], writes=[outb])

        for L in range(nlayers):
            mixer, j = L % 3, L // 3
            if mixer == 0:
                Wqkv, Wo, NQ, NKV = w_qkv_a[j], w_o_a[j], 1024, 1024
            elif mixer == 1:
                Wqkv, Wo, NQ, NKV = w_qkv_b[j], w_o_b[j], 1024, 256
            else:
                Wqkv, Wo, NQ, NKV = w_qkv_c[j], w_o_c[j], 1024, 1024
            NQKV = NQ + 2 * NKV

            c.phase_reset(KEEP)
            Wt, Wb = c.alloc(8 * NQKV, MMDT)
            W3 = Wt.rearrange("p (c n) -> p c n", n=NQKV)
            Wq_pc = Wqkv.rearrange("(p c) n -> p c n", c=8)
            for cc in range(8):
                for n0 in range(0, NQKV, 1024):
                    n1 = min(NQKV, n0 + 1024)
                    c.dma(W3[:, cc, n0:n1], Wq_pc[:, cc, n0:n1], reads=[dIN], writes=[Wb], q=wq)
            xr_ring = [c.alloc(4 * 1024) for _ in range(2)]
            xT_ring = [c.alloc(8 * 512, MMDT) for _ in range(2)]
            stg_ring = [c.alloc(512) for _ in range(4)]
            sti = 0
            pso = 0
            for tb in range(T // 512):
                xr, xrb = xr_ring[tb % 2]
                xT, xTb = xT_ring[tb % 2]
                xr3 = xr.rearrange("p (j d) -> p j d", d=1024)
                xr4 = xr.rearrange("p (j q c) -> p j c q", q=128, c=8)
                xT3 = xT.rearrange("p (c t) -> p c t", t=512)
                c.dma(xr3, X[tb * 512:(tb + 1) * 512, :].rearrange("(j p) d -> p j d", p=128), reads=[dX], writes=[xrb])
                for cc in range(8):
                    pt, pb = PS[cc % 2]
                    for jj in range(4):
                        c.transpose(pt[:, jj * 128:(jj + 1) * 128], pb, xr4[:, jj, cc, :], [xrb], ident,
                                    signal=(jj == 3))
                    evac(xT3[:, cc, :], xTb, pt[:, :], pb)
                for fc in range((NQ + NKV) // 128):
                    pt, pb = PS[2 + pso % 4]; pso += 1
                    c.mm(pt[:, :], pb, [(W3[:, cc, fc * 128:(fc + 1) * 128], xT3[:, cc, :], [Wb, xTb]) for cc in range(8)])
                    sg, sgb = stg_ring[sti % 4]; sti += 1
                    evac(sg, sgb, pt[:, :], pb)
                    if fc < NQ // 128:
                        c.dma(QT[fc * 128:(fc + 1) * 128, tb * 512:(tb + 1) * 512], sg, reads=[sgb], writes=[dQT])
                    else:
                        f2 = fc - NQ // 128
                        c.dma(KT[f2 * 128:(f2 + 1) * 128, tb * 512:(tb + 1) * 512], sg, reads=[sgb], writes=[dKT])
                for jj in range(4):
                    for v0 in range(0, NKV, 512):
                        nv = min(512, NKV - v0)
                        pt, pb = PS[2 + pso % 4]; pso += 1
                        c.mm(pt[:, 0:nv], pb, [(xT3[:, cc, jj * 128:(jj + 1) * 128], W3[:, cc, NQ + NKV + v0:NQ + NKV + v0 + nv], [Wb, xTb])
                                               for cc in range(8)])
                        sg, sgb = stg_ring[sti % 4]; sti += 1
                        evac(sg[:, 0:nv], sgb, pt[:, 0:nv], pb)
                        r0 = tb * 512 + jj * 128
                        c.dma(V[r0:r0 + 128, v0:v0 + nv], sg[:, 0:nv], reads=[sgb], writes=[dV])
            if debug and ("QT%d" % L) in dbg:
                c.dma(dbg["QT%d" % L], QT, reads=[dQT], writes=[dIN])
                c.dma(dbg["KT%d" % L], KT, reads=[dKT], writes=[dIN])
                c.dma(dbg["V%d" % L], V, reads=[dV], writes=[dIN])

            c.phase_reset(KEEP)
            LA = 3
            ld_ring = []
            for _ in range(2):
                qt_, qb_ = c.alloc(SEQ, MMDT)
                kt_, kb_ = c.alloc(SEQ, MMDT)
                va_, vb_ = c.alloc(16 * 2 * 128, MMDT)
                ld_ring.append((qt_, qb_, kt_, kb_, va_, vb_))
                c.op(dve, "tensor_copy", va_.rearrange("p (a b) -> p a b", b=128), ones_f.unsqueeze(1).broadcast_to([128, 32, 128]),
                     reads=[b_const], writes=[vb_])
            ot_ring = [c.alloc(SEQ, MMDT) for _ in range(2)]
            tmp_ring = [c.alloc(512) for _ in range(4)]
            P_ring = [c.alloc(512, MMDT) for _ in range(LA + 3)]
            rec_ring = [c.alloc(512) for _ in range(2)]
            cnt = {}

            def nxt(ring, key):
                i = cnt.get(key, 0)
                cnt[key] = i + 1
                return ring[i % len(ring)]

            if mixer in (0, 1):
                bq_ring = [c.alloc(512) for _ in range(2)]
                nm = 4 if mixer == 0 else 5
                masks = [c.alloc(512) for _ in range(nm)]
                for i, (m, mb) in enumerate(masks):
                    c.dma(m, cmask[i if mixer == 0 else 4 + i], writes=[mb])
                bqm_ring = [[c.alloc(512) for _ in range(nm)] for _ in range(2)]
            if mixer == 0:
                comb_ring = [c.alloc(512) for _ in range(3)]
                bqB_ring = [c.alloc(512) for _ in range(2)]
                km, kmb = c.alloc(8)
                kmr, kmrb = c.alloc(8, MMDT)
                kmrep, kmrepb = c.alloc(8 * 128, MMDT)
                kmrep3 = kmrep.rearrange("p (n m) -> p n m", m=128)
                gsb_ring = [c.alloc(8) for _ in range(4)]
                top_ring = [c.alloc(12) for _ in range(4)]
                diag_ring = [c.alloc(128, MMDT) for _ in range(8)]
                thr_ring = [c.alloc(512) for _ in range(2)]
            elif mixer == 1:
                esk, eskb = c.alloc(NH)
                c.dma(esk, sinks_b[0:1, :].broadcast_to([128, NH]), reads=[dIN], writes=[eskb])
                c.op(act, "activation", esk, esk, AF.Exp, reads=[eskb], writes=[eskb])
            else:
                sp_ring = [c.alloc(512, MMDT) for _ in range(4)]
                e_ring = [c.alloc(512) for _ in range(2)]
                racc_ring = [c.alloc(512, MMDT) for _ in range(5)]
                t3_ring = [c.alloc(512) for _ in range(2)]

            units = [(s, hp) for s in range(NSEQ) for hp in range(8)]

            def load_unit(u):
                s, hp = units[u]
                qt_, qb_, kt_, kb_, va_, vb_ = ld_ring[u % 2]
                va4 = va_.rearrange("p (k h m) -> p k h m", h=2, m=128)
                ts0 = s * SEQ
                c.dma(qt_, QT[hp * 128:(hp + 1) * 128, ts0:ts0 + SEQ], reads=[dQT], writes=[qb_], q=wq)
                if mixer == 1:
                    g = hp // 2
                    for hh in range(2):
                        c.dma(kt_[hh * 64:(hh + 1) * 64, :], KT[g * 64:(g + 1) * 64, ts0:ts0 + SEQ], reads=[dKT], writes=[kb_], q=wq)
                        c.dma(va4[:, :, hh, 0:64], V[ts0:ts0 + SEQ, g * 64:(g + 1) * 64].rearrange("(k p) d -> p k d", p=128),
                              reads=[dV], writes=[vb_], q=wq)
                else:
                    c.dma(kt_, KT[hp * 128:(hp + 1) * 128, ts0:ts0 + SEQ], reads=[dKT], writes=[kb_], q=wq)
                    for hh in range(2):
                        f0 = hp * 128 + hh * 64
                        c.dma(va4[:, :, hh, 0:64], V[ts0:ts0 + SEQ, f0:f0 + 64].rearrange("(k p) d -> p k d", p=128),
                              reads=[dV], writes=[vb_], q=wq)

            def unit_prep(u):
                qt_, qb_, kt_, kb_, va_, vb_ = ld_ring[u % 2]
                c.op(dve, "tensor_reduce", km, kt_.bitcast(F32).rearrange("p (n k) -> p n k", k=256), axis=AX.X, op=ALU.add,
                     reads=[kb_], writes=[kmb])
                c.op(dve, "tensor_scalar", km, km, 1.0 / 256.0, None, op0=ALU.mult, reads=[kmb], writes=[kmb])
                c.op(dve, "tensor_copy", kmr, km, reads=[kmb], writes=[kmrb])
                for n in range(8):
                    c.op(dve, "tensor_scalar", kmrep3[:, n, :], ones_f, km[:, n:n + 1], None, op0=ALU.mult,
                         reads=[kmb, b_const], writes=[kmrepb])

            def head_prep(u, hh, st):
                s, hp = units[u]
                slope = SLOPES[hp * 2 + hh]
                bq, bqb = nxt(bq_ring, "bq")
                c.op(dve, "tensor_scalar", bq, cb_f, slope, None, op0=ALU.mult, reads=[b_const], writes=[bqb])
                bqm = bqm_ring[cnt.get("bqm", 0) % 2]
                cnt["bqm"] = cnt.get("bqm", 0) + 1
                for mi in range(nm):
                    c.op(dve, "tensor_tensor", bqm[mi][0], bq, masks[mi][0], op=ALU.add, reads=[bqb, masks[mi][1]], writes=[bqm[mi][1]])
                st["bq"], st["bqb"], st["bqm"] = bq, bqb, bqm
                if mixer == 0:
                    bqB, _ = nxt(bqB_ring, "bqB")
                    c.op(dve, "tensor_scalar", bqB, bq, -BIG, None, op0=ALU.add, reads=[bqb], writes=[bqb])
                    st["bqB"] = bqB

            def sel_prep1(u, hh, qc, st):
                qt_, qb_ = ld_ring[u % 2][0:2]
                hs = slice(hh * 64, (hh + 1) * 64)
                dgs = []
                for jq in range(4):
                    qtile = qc * 4 + jq
                    blk = qtile // 2
                    q128 = slice(qtile * 128, (qtile + 1) * 128)
                    pg, pgb = PS[7]
                    c.mm(pg[:, 0:8], pgb, [(qt_[hs, q128], kmr[hs, 0:8], [qb_, kmrb])])
                    gs, gsb = nxt(gsb_ring, "gsb")
                    c.op(dve, "memset", gs, -BIG, writes=[gsb])
                    c.op(dve, "tensor_copy", gs[:, 0:blk], pg[:, 0:blk], reads=[pgb], writes=[gsb])
                    tp, tpb = nxt(top_ring, "top")
                    c.op(dve, "max", tp[:, 0:8], gs, reads=[gsb], writes=[tpb])
                    c.op(dve, "tensor_tensor", tp[:, 8:9], tp[:, 2:3], tp[:, 3:4], op=ALU.add, reads=[tpb], writes=[tpb])
                    c.op(dve, "tensor_scalar", tp[:, 9:10], tp[:, 8:9], 0.5, None, op0=ALU.mult, reads=[tpb], writes=[tpb])
                    dg, dgb = nxt(diag_ring, "diag")
                    c.op(dve, "tensor_scalar", dg, ident, tp[:, 9:10], None, op0=ALU.mult, reads=[tpb, b_const], writes=[dgb])
                    dgs.append((dg, dgb))
                st[("dg", qc)] = dgs

            def sel_prep2(u, hh, qc, st):
                thr, thrb = nxt(thr_ring, "thr")
                pth, pthb = PS[7]
                for jq, (dg, dgb) in enumerate(st[("dg", qc)]):
                    c.mm(pth[:, jq * 128:(jq + 1) * 128], pthb, [(ones_r, dg, [dgb, b_const])])
                c.op(act, "copy", thr, pth[:, :], reads=[pthb], writes=[thrb])
                st[("thr", qc)] = (thr, thrb)

            tasks = []
            for u, (s, hp) in enumerate(units):
                qt_, qb_, kt_, kb_, va_, vb_ = ld_ring[u % 2]
                ot_, ob_ = ot_ring[u % 2]
                va4 = va_.rearrange("p (k h m) -> p k h m", h=2, m=128)
                ts0 = s * SEQ
                utasks = []
                for hh in range(2):
                    h = hp * 2 + hh
                    hs = slice(hh * 64, (hh + 1) * 64)
                    slope = SLOPES[h]
                    st = {}
                    for qc in range(4):
                        qs = slice(qc * 512, (qc + 1) * 512)
                        if mixer == 0:
                            kts = list(range(0, 4 * qc + 4))
                        elif mixer == 1:
                            kts = list(range(max(0, 4 * qc - 1), 4 * qc + 4))
                        else:
                            kts = list(range(4 * qc + 3, -1, -1))
                        grp = {"acc": None, "racc": None}
                        nk = len(kts)
                        for ki, kt in enumerate(kts):
                            ks = slice(kt * 128, (kt + 1) * 128)
                            first, last = (ki == 0), (ki == nk - 1)
                            pre = []
                            if hh == 0 and qc == 0 and ki == 0:
                                if u == 0:
                                    pre.append(lambda u=u: load_unit(u))
                                if mixer == 0:
                                    pre.append(lambda u=u: unit_prep(u))
                            if qc == 0 and ki == 0 and mixer in (0, 1):
                                pre.append(lambda u=u, hh=hh, st=st: head_prep(u, hh, st))
                            if mixer == 0 and qc in (1, 2):
                                if ki == 0:
                                    pre.append(lambda u=u, hh=hh, qc=qc, st=st: sel_prep1(u, hh, qc + 1, st))
                                if ki == nk // 2:
                                    pre.append(lambda u=u, hh=hh, qc=qc, st=st: sel_prep2(u, hh, qc + 1, st))
                            if hh == 0 and qc == 1 and ki == 0 and u + 1 < len(units):
                                pre.append(lambda u=u: load_unit(u + 1))

                            if mixer in (0, 1):
                                def A(pre=pre, grp=grp, st=st, kt=kt, ks=ks, qs=qs, qc=qc, hs=hs, first=first, qt_=qt_, qb_=qb_, kt_=kt_, kb_=kb_,
                                      slope=slope, tk=None):
                                    for f in pre:
                                        f()
                                    if first:
                                        grp["acc"] = PS[5 + cnt.get("acc", 0) % 2]
                                        cnt["acc"] = cnt.get("acc", 0) + 1
                                    bq, bqb, bqm = st["bq"], st["bqb"], st["bqm"]
                                    n = kt // 2
                                    bias2 = None
                                    if mixer == 0:
                                        c0 = max(0, (kt - 4 * qc) * 128)
                                        c1 = 512
                                    else:
                                        ii = kt - 4 * qc + 1
                                        c0 = max(0, 128 * (ii - 1))
                                        c1 = min(512, 128 * (ii + 1))
                                    tk["cs"] = (c0, c1)
                                    if mixer == 0:
                                        mi = kt - 4 * qc
                                        if qc >= 2 and n <= 2 * qc:
                                            if kt % 2 == 0:
                                                thr, thrb = st[("thr", qc)]
                                                pg, pgb = PS[3 + cnt.get("G", 0) % 2]
                                                cnt["G"] = cnt.get("G", 0) + 1
                                                c.mm(pg[:, :], pgb, [(kmrep3[hs, n, :], qt_[hs, qs], [kmrepb, qb_])])
                                                cm, cmb = nxt(comb_ring, "comb")
                                                c.op(dve, "tensor_tensor", cm, pg[:, :], thr, op=ALU.is_ge, reads=[pgb, thrb], writes=[cmb])
                                                c.op(dve, "scalar_tensor_tensor", cm, cm, BIG, st["bqB"], op0=ALU.mult, op1=ALU.add,
                                                     reads=[cmb, bqb], writes=[cmb])
                                                st["comb"] = (cm, cmb)
                                            bias = st["comb"]
                                            if n == 2 * qc:
                                                bias2 = bqm[mi]
                                        else:
                                            bias = bqm[mi] if mi >= 0 else (bq, bqb)
                                    else:
                                        bias = bqm[kt - 4 * qc + 1]
                                    pS, pSb = PS[cnt.get("S", 0) % 3]
                                    cnt["S"] = cnt.get("S", 0) + 1
                                    qs0 = qs.start
                                    c.mm(pS[:, c0:c1], pSb, [(kt_[hs, ks], qt_[hs, qs0 + c0:qs0 + c1], [kb_, qb_])])
                                    tm, tmb = nxt(tmp_ring, "tmp")
                                    cnt["tile"] = cnt.get("tile", 0) + 1
                                    if bias2 is None and cnt["tile"] % 4 == 0:
                                        c.op(act, "activation", tm[:, c0:c1], pS[:, c0:c1], AF.Identity, scale=0.125, reads=[pSb], writes=[tmb])
                                        c.op(pool, "tensor_tensor", tm[:, c0:c1], tm[:, c0:c1], bias[0][:, c0:c1], op=ALU.add,
                                             reads=[tmb, bias[1]], writes=[tmb])
                                    else:
                                        c.op(dve, "scalar_tensor_tensor", tm[:, c0:c1], pS[:, c0:c1], 0.125, bias[0][:, c0:c1], op0=ALU.mult, op1=ALU.add,
                                             reads=[pSb, bias[1]], writes=[tmb])
                                    if bias2 is not None and c0 < 256:
                                        c.op(dve, "scalar_tensor_tensor", tm[:, c0:256], pS[:, c0:256], 0.125, bias2[0][:, c0:256], op0=ALU.mult, op1=ALU.add,
                                             reads=[pSb, bias2[1], tmb], writes=[tmb])
                                    Pt, Pb = nxt(P_ring, "P")
                                    c.op(act, "activation", Pt[:, c0:c1], tm[:, c0:c1], AF.Exp, bias=float(-slope * (qc * 512 - kt * 128)), scale=1.0,
                                         reads=[tmb], writes=[Pb])
                                    tk["P"] = (Pt, Pb)

                                def B(grp=grp, kt=kt, hh=hh, h=h, hs=hs, qs=qs, first=first, last=last, va4=va4, vb_=vb_, ot_=ot_, ob_=ob_,
                                      hp=hp, ts0=ts0, qc=qc, tk=None):
                                    acc, accb = grp["acc"]
                                    Pt, Pb = tk["P"]
                                    c0, c1 = tk["cs"]
                                    c.mm(acc[:, c0:c1], accb, [(va4[:, kt, hh, :], Pt[:, c0:c1], [vb_, Pb])], start=first, stop=last)
                                    if last:
                                        rc, rcb = nxt(rec_ring, "rec")
                                        if mixer == 1:
                                            c.op(act, "activation", rc[64:128, :], acc[64:128, :], AF.Ln, bias=esk[64:128, h:h + 1], scale=1.0,
                                                 reads=[accb, eskb], writes=[rcb])
                                        else:
                                            c.op(act, "activation", rc[64:128, :], acc[64:128, :], AF.Ln, reads=[accb], writes=[rcb])
                                        c.op(act, "activation", rc[64:128, :], rc[64:128, :], AF.Exp, scale=-1.0, reads=[rcb], writes=[rcb])
                                        c.op(dve, "tensor_tensor", ot_[hs, qs], acc[0:64, :], rc[64:128, :], op=ALU.mult, reads=[accb, rcb], writes=[ob_])
                                        if hh == 1 and qc == 3:
                                            c.dma(OT[hp * 128:(hp + 1) * 128, ts0:ts0 + SEQ], ot_.bitcast(F32), reads=[ob_], writes=[dOT])
                                tk = {}
                                utasks.append((lambda A=A, tk=tk: A(tk=tk), None, lambda B=B, tk=tk: B(tk=tk)))
                            else:
                                diag = kt >= 4 * qc
                                base = qc * 512 - kt * 128

                                def A1(pre=pre, grp=grp, kt=kt, ks=ks, qs=qs, hs=hs, first=first, last=last, diag=diag, base=base,
                                       qt_=qt_, qb_=qb_, kt_=kt_, kb_=kb_, tk=None):
                                    for f in pre:
                                        f()
                                    if first:
                                        grp["acc"] = PS[5 + cnt.get("acc", 0) % 2]
                                        cnt["acc"] = cnt.get("acc", 0) + 1
                                        grp["racc"] = None
                                    pS, pSb = PS[cnt.get("S", 0) % 3]
                                    cnt["S"] = cnt.get("S", 0) + 1
                                    c.mm(pS[:, :], pSb, [(kt_[hs, ks], qt_[hs, qs], [kb_, qb_])])
                                    et, etb = nxt(e_ring, "e")
                                    c.op(act, "activation", et, pS[:, :], AF.Exp, scale=0.125, reads=[pSb], writes=[etb])
                                    spt, spb = nxt(sp_ring, "sp")
                                    c.op(act, "activation", spt, et, AF.Ln, bias=1.0, scale=1.0, reads=[etb], writes=[spb])
                                    if diag:
                                        c.op(pool, "affine_select", spt, spt, pattern=[[1, 512]], compare_op=ALU.is_gt, fill=0.0,
                                             base=base, channel_multiplier=-1, reads=[spb], writes=[spb])
                                    tk["S"] = (pS, pSb)
                                    tk["sp"] = (spt, spb)
                                    tk["rprev"] = grp["racc"]
                                    if not last:
                                        rn, rnb = nxt(racc_ring, "racc")
                                        if grp["racc"] is None:
                                            c.op(pool, "tensor_copy", rn, spt, reads=[spb], writes=[rnb])
                                        else:
                                            c.op(pool, "tensor_tensor", rn, grp["racc"][0].bitcast(F32), spt.bitcast(F32), op=ALU.add,
                                                 reads=[grp["racc"][1], spb], writes=[rnb])
                                        grp["racc"] = (rn, rnb)

                                def A2(diag=diag, base=base, tk=None):
                                    pS, pSb = tk["S"]
                                    spt, spb = tk["sp"]
                                    pa, pab = PS[3 + cnt.get("pa", 0) % 2]
                                    cnt["pa"] = cnt.get("pa", 0) + 1
                                    parts = [(ustr_r, spt, [spb, b_const])]
                                    if tk["rprev"] is not None:
                                        parts.append((ones_r, tk["rprev"][0], [tk["rprev"][1], b_const]))
                                    c.mm(pa[:, :], pab, parts)
                                    tm, tmb = nxt(tmp_ring, "tmp")
                                    c.op(dve, "scalar_tensor_tensor", tm, pS[:, :], 0.125, spt.bitcast(F32), op0=ALU.mult, op1=ALU.subtract,
                                         reads=[pSb, spb], writes=[tmb])
                                    t3, t3b = nxt(t3_ring, "t3")
                                    c.op(dve, "tensor_tensor", t3, tm, pa[:, :], op=ALU.subtract, reads=[tmb, pab], writes=[t3b])
                                    Pt, Pb = nxt(P_ring, "P")
                                    c.op(act, "activation", Pt, t3, AF.Exp, reads=[t3b], writes=[Pb])
                                    if diag:
                                        c.op(pool, "affine_select", Pt, Pt, pattern=[[1, 512]], compare_op=ALU.is_gt, fill=0.0,
                                             base=base, channel_multiplier=-1, reads=[Pb], writes=[Pb])
                                    tk["P"] = (Pt, Pb)

                                def B(grp=grp, kt=kt, hh=hh, hs=hs, qs=qs, first=first, last=last, va4=va4, vb_=vb_, ot_=ot_, ob_=ob_,
                                      hp=hp, ts0=ts0, qc=qc, tk=None):
                                    acc, accb = grp["acc"]
                                    Pt, Pb = tk["P"]
                                    c.mm(acc[:, :], accb, [(va4[:, kt, hh, :], Pt, [vb_, Pb])], start=first, stop=last)
                                    if last:
                                        c.op(act, "copy", ot_[hs, qs], acc[0:64, :], reads=[accb], writes=[ob_])
                                        if hh == 1 and qc == 3:
                                            c.dma(OT[hp * 128:(hp + 1) * 128, ts0:ts0 + SEQ], ot_.bitcast(F32), reads=[ob_], writes=[dOT])
                                tk = {}
                                utasks.append((lambda A1=A1, tk=tk: A1(tk=tk), lambda A2=A2, tk=tk: A2(tk=tk), lambda B=B, tk=tk: B(tk=tk)))
                tasks.extend(utasks)
            lags = (0, 0, LA) if mixer in (0, 1) else (0, 1, 3)
            for t in range(len(tasks) + lags[-1]):
                for k in range(3):
                    i = t - lags[k]
                    if 0 <= i < len(tasks) and tasks[i][k] is not None:
                        tasks[i][k]()
            if debug and ("OT%d" % L) in dbg:
                c.dma(dbg["OT%d" % L], OT, reads=[dOT], writes=[dIN])

            c.phase_reset(KEEP)
            Wot, Wob = c.alloc(8 * D, MMDT)
            Wo3 = Wot.rearrange("p (c n) -> p c n", n=D)
            Wo_pc = Wo.rearrange("(p c) n -> p c n", c=8)
            for cc in range(8):
                c.dma(Wo3[:, cc, :], Wo_pc[:, cc, :], reads=[dIN], writes=[Wob], q=wq)
            Wr, Wrb = c.alloc(8 * 36)
            Wr3 = Wr.rearrange("p (c n) -> p c n", n=36)
            c.dma(Wr3, w_rt[L].rearrange("(p c) n -> p c n", c=8), reads=[dIN], writes=[Wrb])
            gB, gbuf = c.alloc(D)
            bB, _ = c.alloc(D)
            brt, _ = c.alloc(36)
            c.dma(gB, ln1_g[L:L + 1, :].broadcast_to([128, D]), reads=[dIN], writes=[gbuf])
            c.dma(bB, ln1_b[L:L + 1, :].broadcast_to([128, D]), reads=[dIN], writes=[gbuf])
            c.dma(brt, b_rt[L:L + 1, :].broadcast_to([128, 36]), reads=[dIN], writes=[gbuf])
            c.op(dve, "memset", cnt_b, 0.0, writes=[b_cnt])
            otb_ring = [c.alloc(8 * 128, MMDT) for _ in range(3)]
            xr_ring = [c.alloc(D) for _ in range(4)]
            x1_ring = [c.alloc(D) for _ in range(6)]
            x1T_ring = [c.alloc(8 * 128) for _ in range(2)]
            sc_ring = [{"st": c.alloc(12), "mv": c.alloc(4)} for _ in range(4)]
            rs_ring = [c.alloc(320) for _ in range(4)]
            for (rs, rsb) in rs_ring:
                c.op(dve, "memset", rs[:, 4:8], -BIG, writes=[rsb])

            def ln_a(y, yb, sc):
                st, stb = sc["st"]
                mv, mvb = sc["mv"]
                c.op(dve, "bn_stats", st[:, 0:6], y[:, 0:512], reads=[yb], writes=[stb])
                c.op(dve, "bn_stats", st[:, 6:12], y[:, 512:1024], reads=[yb], writes=[stb])
                c.op(dve, "bn_aggr", mv[:, 0:2], st[:, 0:12], reads=[stb], writes=[mvb])
                c.op(dve, "tensor_scalar", mv[:, 2:3], mv[:, 1:2], EPS, None, op0=ALU.add, reads=[mvb], writes=[mvb])
                c.op(act, "activation", mv[:, 2:3], mv[:, 2:3], AF.Sqrt, reads=[mvb], writes=[mvb])

            def ln_b(y, yb, sc):
                mv, mvb = sc["mv"]
                c.op(dve, "reciprocal", mv[:, 2:3], mv[:, 2:3], reads=[mvb], writes=[mvb])
                c.op(dve, "scalar_tensor_tensor", mv[:, 3:4], mv[:, 0:1], -1.0, mv[:, 2:3], op0=ALU.mult, op1=ALU.mult,
                     reads=[mvb], writes=[mvb])
                c.op(act, "activation", y, y, AF.Identity, bias=mv[:, 3:4], scale=mv[:, 2:3], reads=[yb, mvb], writes=[yb])

            def ln_c(y, yb, gB_, bB_, gbuf_, outt, outb):
                c.op(dve, "tensor_tensor", y, y, gB_, op=ALU.mult, reads=[yb, gbuf_], writes=[yb])
                c.op(pool, "tensor_tensor", outt, y, bB_, op=ALU.add, reads=[yb, gbuf_], writes=[outb])

            def p3_stages(tt):
                r0 = tt * 128
                otb, otbb = otb_ring[tt % 3]
                otb3 = otb.rearrange("p (c t) -> p c t", t=128)
                xr, xrb = xr_ring[tt % 4]
                y, yb = xr, xrb
                x1, x1b = x1_ring[tt % 6]
                x1T, x1Tb = x1T_ring[tt % 2]
                x1T3 = x1T.rearrange("p (c t) -> p c t", t=128)
                sc = sc_ring[tt % 4]
                rs, rsb = rs_ring[tt % 4]
                o = 40
                gm8 = rs[:, o:o + 8]; o += 8
                ngm = rs[:, o:o + 1]; o += 1
                ge = rs[:, o:o + 4]; o += 4
                gsum = rs[:, o:o + 1]; o += 1
                gp = rs[:, o:o + 1]; o += 1
                goh = rs[:, o:o + 4]; o += 4
                ml = rs[:, o:o + 32]; o += 32
                m8 = rs[:, o:o + 8]; o += 8
                sel = rs[:, o:o + 32]; o += 32
                sm = rs[:, o:o + 8]; o += 8
                ex = rs[:, o:o + 32]; o += 32
                wgt = rs[:, o:o + 32]; o += 32
                pos = rs[:, o:o + 32]; o += 32
                oh1 = rs[:, o:o + 32]; o += 32
                assert o <= 320
                R = dict(reads=[rsb], writes=[rsb])
                pl, plb = PS[4 + tt % 2]
                pp, ppb = PS[6 + tt % 2]

                def s0():
                    c.dma(otb3, OT[:, r0:r0 + 128].rearrange("(p c) t -> p c t", c=8), reads=[dOT], writes=[otbb], q=wq)
                    c.dma(xr, X[r0:r0 + 128, :], reads=[dX], writes=[xrb])

                def s1a():
                    for half in range(2):
                        pt, pb = PS[half]
                        c.mm(pt[:, :], pb, [(otb3[:, cc, :], Wo3[:, cc, half * 512:(half + 1) * 512], [otbb, Wob]) for cc in range(8)])
                        c.op(dve, "scalar_tensor_tensor", y[:, half * 512:(half + 1) * 512], xr[:, half * 512:(half + 1) * 512], ALPHA, pt[:, :],
                             op0=ALU.mult, op1=ALU.add, reads=[xrb, pb], writes=[yb])
                    ln_a(y, yb, sc)

                def s1b():
                    ln_b(y, yb, sc)

                def s1c():
                    ln_c(y, yb, gB, bB, gbuf, x1, x1b)
                    c.dma(X1[r0:r0 + 128, :], x1, reads=[x1b], writes=[dX1])

                def s2():
                    for half in range(2):
                        pt, pb = PS[2 + half]
                        for cc in range(4):
                            c4 = half * 4 + cc
                            c.transpose(pt[:, cc * 128:(cc + 1) * 128], pb, x1.rearrange("p (q c) -> p c q", c=8)[:, c4, :], [x1b], ident, signal=(cc == 3))
                        evac(x1T[:, half * 512:(half + 1) * 512], x1Tb, pt[:, :], pb)

                def s2b():
                    c.mm(pl[:, 0:36], plb, [(x1T3[:, cc, :], Wr3[:, cc, :], [x1Tb, Wrb]) for cc in range(8)])

                def s3():
                    yield
                    c.op(dve, "tensor_tensor", rs[:, 0:4], pl[:, 0:4], brt[:, 0:4], op=ALU.add, reads=[plb, gbuf], writes=[rsb])
                    yield
                    c.op(dve, "tensor_tensor", rs[:, 8:40], pl[:, 4:36], brt[:, 4:36], op=ALU.add, reads=[plb, gbuf], writes=[rsb])
                    yield
                    c.op(dve, "max", gm8, rs[:, 0:8], **R)
                    yield
                    c.op(dve, "tensor_scalar", ngm, gm8[:, 0:1], -1.0, None, op0=ALU.mult, **R)
                    yield
                    c.op(dve, "tensor_scalar", goh, rs[:, 0:4], gm8[:, 0:1], None, op0=ALU.is_ge, **R)
                    yield
                    c.op(dve, "tensor_scalar", goh, goh, 1.0, BIG, op0=ALU.subtract, op1=ALU.mult, **R)
                    yield
                    c.op(dve, "tensor_tensor", ml.rearrange("p (g e) -> p g e", e=8), rs[:, 8:40].rearrange("p (g e) -> p g e", e=8),
                         goh.unsqueeze(2).broadcast_to([128, 4, 8]), op=ALU.add, **R)
                    yield
                    c.op(dve, "max", m8, ml, **R)
                    yield
                    c.op(dve, "tensor_scalar", sel, ml, m8[:, 1:2], None, op0=ALU.is_ge, **R)
                    yield
                    c.op(dve, "tensor_scalar", oh1, ml, m8[:, 0:1], None, op0=ALU.is_ge, **R)
                    yield
                    c.op(dve, "tensor_tensor", sm[:, 0:1], m8[:, 1:2], m8[:, 0:1], op=ALU.subtract, **R)
                    yield
                    c.op(dve, "tensor_scalar", sm[:, 1:2], m8[:, 0:1], -1.0, None, op0=ALU.mult, **R)
                    yield
                    c.op(act, "activation", ge, rs[:, 0:4], AF.Exp, bias=ngm, scale=1.0, accum_out=gsum, **R)
                    yield
                    c.op(act, "activation", sm[:, 2:3], sm[:, 0:1], AF.Exp, **R)
                    yield
                    c.op(act, "activation", ex, ml, AF.Exp, bias=sm[:, 1:2], scale=1.0, **R)
                    yield

                def s4pre():
                    c.mm(pp[:, 0:32], ppb, [(tri_f, sel, [b_const, rsb])])
                    c.mm(pp[:, 32:64], ppb, [(ones_f, sel, [b_const, rsb])])

                def s4():
                    c.op(dve, "reciprocal", gp, gsum, **R)
                    yield
                    c.op(dve, "tensor_scalar", sm[:, 3:4], sm[:, 2:3], 1.0, None, op0=ALU.add, **R)
                    yield
                    c.op(dve, "reciprocal", sm[:, 3:4], sm[:, 3:4], **R)
                    yield
                    c.op(dve, "tensor_tensor", sm[:, 4:5], sm[:, 3:4], gp, op=ALU.mult, **R)
                    yield
                    c.op(dve, "scalar_tensor_tensor", wgt, ex, sm[:, 4:5], sel, op0=ALU.mult, op1=ALU.mult, **R)
                    yield
                    c.op(dve, "tensor_tensor", pos, pp[:, 0:32], cnt_b, op=ALU.add, reads=[ppb, b_cnt, rsb], writes=[rsb])
                    yield
                    c.op(dve, "tensor_tensor", cnt_b, cnt_b, pp[:, 32:64], op=ALU.add, reads=[ppb, b_cnt], writes=[b_cnt])
                    yield
                    c.op(dve, "scalar_tensor_tensor", pos, pos, float(CAP - 1), eoff, op0=ALU.min, op1=ALU.add, reads=[rsb, b_const], writes=[rsb])
                    yield
                    c.op(dve, "tensor_tensor", sel, sel, oh1, op=ALU.subtract, **R)
                    yield
                    c.op(dve, "tensor_tensor", ex, oh1, pos, op=ALU.mult, **R)
                    yield
                    c.op(dve, "tensor_reduce", rt3[:, tt, 0:1], ex, axis=AX.X, op=ALU.add, reads=[rsb], writes=[b_rtab])
                    yield
                    c.op(dve, "tensor_tensor", ex, sel, pos, op=ALU.mult, **R)
                    yield
                    c.op(dve, "tensor_reduce", rt3[:, tt, 1:2], ex, axis=AX.X, op=ALU.add, reads=[rsb], writes=[b_rtab])
                    yield
                    c.op(dve, "tensor_tensor", ex, oh1, wgt, op=ALU.mult, **R)
                    yield
                    c.op(dve, "tensor_reduce", rt3[:, tt, 2:3], ex, axis=AX.X, op=ALU.add, reads=[rsb], writes=[b_rtab])
                    yield
                    c.op(dve, "tensor_tensor", ex, sel, wgt, op=ALU.mult, **R)
                    yield
                    c.op(dve, "tensor_reduce", rt3[:, tt, 3:4], ex, axis=AX.X, op=ALU.add, reads=[rsb], writes=[b_rtab])
                    yield
                    c.op(dve, "tensor_copy", ridx3[:, tt, :], rt3[:, tt, 0:2], reads=[b_rtab], writes=[b_ridx])
                    yield

                def s5():
                    for k in range(2):
                        c.dma(XS[:, :], x1, reads=[x1b, b_ridx], writes=[dXS], q=pool,
                              indirect=dict(out_offset=bass.IndirectOffsetOnAxis(ap=ridx3[:, tt, k:k + 1], axis=0), in_offset=None,
                                            bounds_check=bc_reg, oob_is_err=False))
                return dict(s0=s0, s1a=s1a, s1b=s1b, s1c=s1c, s2a=s2, s2b=s2b, s3=s3, s4pre=s4pre, s4=s4, s5=s5)

            p3t = [p3_stages(tt) for tt in range(T // 128)]
            lag3 = dict(s0=0, s1a=1, s1b=2, s1c=3, s2a=4, s2b=4, s3=5, s4pre=6, s4=6, s5=7)
            n3 = len(p3t)

            def run3(name, t):
                i = t - lag3[name]
                if 0 <= i < n3:
                    return p3t[i][name]()
                return None
            for t in range(n3 + 7):
                for nm_ in ("s0", "s1b", "s1c", "s4pre", "s2a", "s1a"):
                    run3(nm_, t)
                gens = [g for g in (run3("s4", t), run3("s3", t)) if g is not None]
                while gens:
                    for g in list(gens):
                        try:
                            next(g)
                        except StopIteration:
                            gens.remove(g)
                for nm_ in ("s2b", "s5"):
                    run3(nm_, t)
            if debug and ("X1_%d" % L) in dbg:
                c.dma(dbg["X1_%d" % L], X1, reads=[dX1], writes=[dIN])
            if debug and ("rt%d" % L) in dbg:
                c.dma(dbg["rt%d" % L], rt, reads=[b_rtab], writes=[dIN])

            c.phase_reset(KEEP)
            w_ring = []
            for _ in range(2):
                w_ring.append((c.alloc(8 * DEXP, MMDT), c.alloc(8 * DEXP, MMDT), c.alloc(4 * D, MMDT)))
            xs_ring = [c.alloc(3 * D) for _ in range(2)]
            xsT_ring = [c.alloc(8 * CAP, MMDT) for _ in range(2)]
            hT_ring = [c.alloc(4 * CAP, MMDT) for _ in range(2)]
            sg_ring = [c.alloc(CAP) for _ in range(2)]
            ys_ring = [c.alloc(D) for _ in range(3)]
            NJ = CAP // 128
            ysi = 0
            sgi = 0

            def load_w(e):
                (wg, wgb), (wu, wub), (wd, wdb) = w_ring[e % 2]
                wg3 = wg.rearrange("p (c n) -> p c n", n=DEXP)
                wu3 = wu.rearrange("p (c n) -> p c n", n=DEXP)
                wd3 = wd.rearrange("p (c n) -> p c n", n=D)
                c.dma(wg, w_e_gate[L, e].rearrange("(p c) f -> p (c f)", c=8), reads=[dIN], writes=[wgb], q=wq)
                c.dma(wu, w_e_up[L, e].rearrange("(p c) f -> p (c f)", c=8), reads=[dIN], writes=[wub], q=wq)
                c.dma(wd, w_e_down[L, e].rearrange("(p c) n -> p (c n)", c=4), reads=[dIN], writes=[wdb], q=wq)

            def load_xs(e):
                xs, xsb = xs_ring[e % 2]
                c.dma(xs.rearrange("p (j d) -> p j d", d=D), XS[e * CAP:(e + 1) * CAP, :].rearrange("(j p) d -> p j d", p=128),
                      reads=[dXS], writes=[xsb])

            load_w(0)
            load_xs(0)
            for e in range(NEXP):
                if e + 1 < NEXP:
                    load_w(e + 1)
                    load_xs(e + 1)
                (wg, wgb), (wu, wub), (wd, wdb) = w_ring[e % 2]
                wg3 = wg.rearrange("p (c n) -> p c n", n=DEXP)
                wu3 = wu.rearrange("p (c n) -> p c n", n=DEXP)
                wd3 = wd.rearrange("p (c n) -> p c n", n=D)
                xs, xsb = xs_ring[e % 2]
                xs3 = xs.rearrange("p (j d) -> p j d", d=D)
                xs4 = xs.rearrange("p (j q c) -> p j c q", q=128, c=8)
                wg4 = wg.rearrange("p (c m k) -> p c k m", c=8, m=128, k=4)
                wu4 = wu.rearrange("p (c m k) -> p c k m", c=8, m=128, k=4)
                xsT, xsTb = xsT_ring[e % 2]
                xsT3 = xsT.rearrange("p (c t) -> p c t", t=CAP)
                hT, hTb = hT_ring[e % 2]
                hT3 = hT.rearrange("p (c t) -> p c t", t=CAP)
                for cc in range(8):
                    pt, pb = PS[cc % 2]
                    for jj in range(NJ):
                        c.transpose(pt[:, jj * 128:(jj + 1) * 128], pb, xs4[:, jj, cc, :], [xsb], ident, signal=(jj == NJ - 1))
                    evac(xsT3[:, cc, :], xsTb, pt[:, 0:CAP], pb)
                for fc in range(4):
                    pg, pgb = PS[2 + fc % 2]
                    pu, pub = PS[4 + fc % 2]
                    c.mm(pg[:, 0:CAP], pgb, [(wg4[:, cc, fc, :], xsT3[:, cc, :], [wgb, xsTb]) for cc in range(8)])
                    c.mm(pu[:, 0:CAP], pub, [(wu4[:, cc, fc, :], xsT3[:, cc, :], [wub, xsTb]) for cc in range(8)])
                    sg, sgb = sg_ring[sgi % 2]; sgi += 1
                    c.op(act, "activation", sg, pg[:, 0:CAP], AF.Silu, reads=[pgb], writes=[sgb])
                    c.op(dve, "tensor_tensor", hT3[:, fc, :], sg, pu[:, 0:CAP], op=ALU.mult, reads=[sgb, pub], writes=[hTb])
                for jj in range(NJ):
                    ys, ysb = ys_ring[ysi % 3]; ysi += 1
                    for half in range(2):
                        pt, pb = PS[6 + half]
                        c.mm(pt[:, :], pb, [(hT3[:, fc, jj * 128:(jj + 1) * 128], wd3[:, fc, half * 512:(half + 1) * 512], [hTb, wdb]) for fc in range(4)])
                        evac(ys[:, half * 512:(half + 1) * 512], ysb, pt[:, :], pb)
                    r0 = e * CAP + jj * 128
                    c.dma(YS[r0:r0 + 128, :], ys, reads=[ysb], writes=[dYS])

            c.phase_reset(KEEP)
            Wg_, Wgb_ = c.alloc(8 * D, MMDT)
            Wg3 = Wg_.rearrange("p (c n) -> p c n", n=D)
            Wg_pc = w_ple_gate[L].rearrange("(p c) n -> p c n", c=8)
            for cc in range(8):
                c.dma(Wg3[:, cc, :], Wg_pc[:, cc, :], reads=[dIN], writes=[Wgb_], q=wq)
            Wp_, Wpb_ = c.alloc(2 * D, MMDT)
            Wp3 = Wp_.rearrange("p (c n) -> p c n", n=D)
            c.dma(Wp_, w_ple_proj[L].rearrange("(p c) n -> p (c n)", c=2), reads=[dIN], writes=[Wpb_], q=wq)
            gB, gbuf = c.alloc(D)
            bB, _ = c.alloc(D)
            c.dma(gB, ln2_g[L:L + 1, :].broadcast_to([128, D]), reads=[dIN], writes=[gbuf])
            c.dma(bB, ln2_b[L:L + 1, :].broadcast_to([128, D]), reads=[dIN], writes=[gbuf])
            y1_ring = [c.alloc(D) for _ in range(4)]
            y2_ring = [c.alloc(D) for _ in range(2)]
            x1_ring = [c.alloc(D) for _ in range(2)]
            pr_ring = [c.alloc(PLE) for _ in range(6)]
            x2_ring = [c.alloc(D) for _ in range(3)]
            x2T_ring = [c.alloc(8 * 128, MMDT) for _ in range(2)]
            pT_ring = [c.alloc(2 * 128, MMDT) for _ in range(2)]
            sig_ring = [c.alloc(D) for _ in range(2)]
            sc_ring = [{"st": c.alloc(12), "mv": c.alloc(4)} for _ in range(4)]
            dst = out if L == DEPTH - 1 else X
            dstb = dIN if L == DEPTH - 1 else dX

            def p5_stages(tt):
                r0 = tt * 128
                y1, y1b = y1_ring[tt % 4]; y2, y2b = y2_ring[tt % 2]; x1, x1b = x1_ring[tt % 2]; pr, prb = pr_ring[tt % 6]
                y, yb = y1, y1b
                x2, x2b = x2_ring[tt % 3]; x2T, x2Tb = x2T_ring[tt % 2]; pT, pTb = pT_ring[tt % 2]
                sig, sigb = sig_ring[tt % 2]; sc = sc_ring[tt % 4]
                x2T3 = x2T.rearrange("p (c t) -> p c t", t=128)
                pT3 = pT.rearrange("p (c t) -> p c t", t=128)

                def s0():
                    for k, (yy, yyb) in enumerate(((y1, y1b), (y2, y2b))):
                        c.dma(yy, YS[:, :], reads=[dYS, b_ridx], writes=[yyb], q=pool,
                              indirect=dict(out_offset=None, in_offset=bass.IndirectOffsetOnAxis(ap=ridx3[:, tt, k:k + 1], axis=0),
                                            bounds_check=bc_reg, oob_is_err=False))
                    c.dma(x1, X1[r0:r0 + 128, :], reads=[dX1], writes=[x1b])
                    c.dma(pr, p_in[L, r0:r0 + 128, :], reads=[dIN], writes=[prb])

                def s1a():
                    c.op(dve, "tensor_scalar", y2, y2, rt3[:, tt, 3:4], None, op0=ALU.mult, reads=[y2b, b_rtab], writes=[y2b])
                    c.op(dve, "scalar_tensor_tensor", y1, y1, rt3[:, tt, 2:3], y2, op0=ALU.mult, op1=ALU.add, reads=[y1b, y2b, b_rtab], writes=[y1b])
                    c.op(dve, "scalar_tensor_tensor", y, x1, ALPHA, y1, op0=ALU.mult, op1=ALU.add, reads=[x1b, y1b], writes=[yb])
                    ln_a(y, yb, sc)

                def s1b():
                    ln_b(y, yb, sc)

                def s1c():
                    ln_c(y, yb, gB, bB, gbuf, x2, x2b)

                def s2():
                    for half in range(2):
                        pt, pb = PS[half]
                        for cc in range(4):
                            c4 = half * 4 + cc
                            c.transpose(pt[:, cc * 128:(cc + 1) * 128], pb, x2.rearrange("p (q c) -> p c q", c=8)[:, c4, :], [x2b], ident, signal=(cc == 3))
                        evac(x2T[:, half * 512:(half + 1) * 512], x2Tb, pt[:, :], pb)
                    pt, pb = PS[2]
                    for cc in range(2):
                        c.transpose(pt[:, cc * 128:(cc + 1) * 128], pb, pr.rearrange("p (q c) -> p c q", c=2)[:, cc, :], [prb], ident, signal=(cc == 1))
                    evac(pT, pTb, pt[:, 0:256], pb)

                def s3a():
                    for half in range(2):
                        hsl = slice(half * 512, (half + 1) * 512)
                        pg, pgb = PS[4 + half]
                        c.mm(pg[:, :], pgb, [(x2T3[:, cc, :], Wg3[:, cc, hsl], [x2Tb, Wgb_]) for cc in range(8)])
                        c.op(act, "activation", sig[:, hsl], pg[:, :], AF.Sigmoid, reads=[pgb], writes=[sigb])
                        pq, pqb = PS[6 + half]
                        c.mm(pq[:, :], pqb, [(pT3[:, cc, :], Wp3[:, cc, hsl], [pTb, Wpb_]) for cc in range(2)])

                def s3b():
                    for half in range(2):
                        hsl = slice(half * 512, (half + 1) * 512)
                        pq, pqb = PS[6 + half]
                        c.op(dve, "tensor_tensor", sig[:, hsl], sig[:, hsl], pq[:, :], op=ALU.mult, reads=[sigb, pqb], writes=[sigb])
                    c.op(pool, "tensor_tensor", sig, sig, x2, op=ALU.add, reads=[sigb, x2b], writes=[sigb])
                    c.dma(dst[r0:r0 + 128, :], sig, reads=[sigb], writes=[dstb])
                return (s0, s1a, s1b, s1c, s2, s3a, s3b)

            p5t = [p5_stages(tt) for tt in range(T // 128)]
            nst = 7
            for t in range(len(p5t) + nst - 1):
                for k in (0, 6, 2, 3, 4, 5, 1):
                    i = t - k
                    if 0 <= i < len(p5t):
                        p5t[i][k]()
            if debug and ("X3_%d" % L) in dbg:
                c.barrier()
                c.dma(dbg["X3_%d" % L], dst, reads=[dstb], writes=[dIN])

        if nlayers < DEPTH:
            c.barrier()
            c.dma(out, X, reads=[dX], writes=[dIN])
        c.barrier()
        if c.future:
            print('FUTURE WAITS', len(c.future), c.future[:10])
    return nc


def _consts():
    cst = np.zeros((128, 6, 128), np.float32)
    k = np.arange(128)[:, None]
    m = np.arange(128)[None, :]
    cst[:, 0, :] = (k == m)
    cst[:, 1, :] = 1.0
    cst[:, 2, :] = (k < m)
    cst[:, 3, :] = (k > m)
    q = np.arange(512)[None, :]
    kk = np.arange(128)[:, None]
    cmask = np.zeros((9, 128, 512), np.float32)
    for i in range(4):
        cmask[i] = np.where((-128 * i + q - kk) >= 0, 0.0, -BIG)
    for i in range(5):
        dlt = 128 - 128 * i
        v = dlt + q - kk
        cmask[4 + i] = np.where((v >= 0) & (v < 128), 0.0, -BIG)
    cbase = (-(q - kk)).astype(np.float32)
    ceoff = np.tile((np.arange(NEXP) * CAP).astype(np.float32)[None, :], (128, 1))
    return cst, cmask, np.ascontiguousarray(cbase), np.ascontiguousarray(ceoff)


_NC_CACHE = {}


def kernel(**inputs):
    f = lambda a: np.ascontiguousarray(np.asarray(a, dtype=np.float32))
    x = f(inputs["x"]).reshape(NCORES, T, D)
    p = f(inputs["p"]).reshape(DEPTH, NCORES, T, PLE)
    cst, cmask, cbase, ceoff = _consts()
    w_rt = np.ascontiguousarray(np.concatenate([f(inputs["w_grp"]), f(inputs["w_exp"])], axis=2))
    b_rt = np.ascontiguousarray(np.concatenate([f(inputs["b_grp"]), f(inputs["b_exp"])], axis=1))
    shared = {k: f(inputs[k]) for k in ("w_qkv_a", "w_o_a", "w_qkv_b", "w_o_b", "sinks_b", "w_qkv_c", "w_o_c", "ln1_g", "ln1_b",
                                         "ln2_g", "ln2_b", "w_e_gate", "w_e_up", "w_e_down", "w_ple_gate", "w_ple_proj")}
    shared.update(w_rt=w_rt, b_rt=b_rt, cst=cst, cmask=cmask, cbase=cbase, ceoff=ceoff)
    if "nc" not in _NC_CACHE:
        _NC_CACHE["nc"] = build()
    nc = _NC_CACHE["nc"]
    in_maps = []
    for i in range(NCORES):
        m = dict(shared)
        m["x"] = x[i]
        m["p"] = np.ascontiguousarray(p[:, i])
        in_maps.append(m)
    res = run_bass_kernel_spmd(nc, in_maps, core_ids=list(range(NCORES)))
    o = np.stack([np.asarray(r["out"]) for r in res.results], axis=0)
    return o.reshape(16, SEQ, D).astype(np.float32)
```
